# Optimizing a Trainium2 kernel written in Bass

```python
import jax, jax.numpy as jnp
from jax import lax
import numpy as np

D_MODEL = 1024
BATCH = 8
SEQ = 4096
DEPTH = 1

EPS = 1e-6
GRID_W = 64
D_MIX = D_MODEL

GLA_HEADS = 4
GLA_DK = 64
GLA_DV = 128
GLA_KEY_WIDTH = GLA_HEADS * GLA_DK
GLA_WIDTH = GLA_HEADS * GLA_DV
GLA_GATE_RANK = 16
GLA_GATE_NORM = 16.0
GLA_CHUNK = 64

ATT_HEADS = 8
ATT_KV_HEADS = 2
ATT_HEAD_DIM = 64
ATT_WIDTH = ATT_HEADS * ATT_HEAD_DIM
ATT_KV_WIDTH = ATT_KV_HEADS * ATT_HEAD_DIM
Q_BLOCK = 128
ROPE_THETA = 10000.0

N_GROUPS = 8
EXPERTS_PER_GROUP = 8
N_EXPERTS = N_GROUPS * EXPERTS_PER_GROUP
TOP_K = 2
D_EXPERT = 512
MOE_BLOCK = 128

IN_SIZES = (GLA_KEY_WIDTH, GLA_KEY_WIDTH, GLA_WIDTH, GLA_WIDTH,
            GLA_GATE_RANK, GLA_GATE_RANK,
            ATT_WIDTH, ATT_KV_WIDTH, ATT_KV_WIDTH)
D_IN = 2336

kernel_name = "hymba_gla_axialgqa_hmoe_encoder"


def rms_norm(x, gain):
    x32 = x.astype(jnp.float32)
    y = x32 * lax.rsqrt(jnp.mean(x32 * x32, axis=-1, keepdims=True) + EPS)
    return y.astype(x.dtype) * gain


def gla_chunked(q, k, v, log_a):
    B, H, T, dk = q.shape
    dv = v.shape[-1]
    C = GLA_CHUNK
    N = T // C
    f32 = jnp.float32
    qf = q.astype(f32).reshape(B, H, N, C, dk)
    kf = k.astype(f32).reshape(B, H, N, C, dk)
    vf = v.astype(f32).reshape(B, H, N, C, dv)
    b = jnp.cumsum(log_a.astype(f32).reshape(B, H, N, C, dk), axis=3)
    b_ref = b[:, :, :, C // 2 - 1:C // 2, :]
    b_last = b[:, :, :, -1:, :]
    scores = jnp.einsum('bhncd,bhnsd->bhncs', qf * jnp.exp(b - b_ref), kf * jnp.exp(b_ref - b))
    mask = jnp.tril(jnp.ones((C, C), dtype=bool))
    scores = jnp.where(mask, scores, 0.0)
    o_intra = jnp.einsum('bhncs,bhnse->bhnce', scores, vf)
    chunk_kv = jnp.einsum('bhncd,bhnce->bhnde', kf * jnp.exp(b_last - b), vf)
    chunk_decay = jnp.exp(b_last[:, :, :, 0, :])

    def step(state, inp):
        decay, kv = inp
        return decay[..., None] * state + kv, state

    init = jnp.zeros((B, H, dk, dv), f32)
    _, states = lax.scan(step, init, (jnp.moveaxis(chunk_decay, 2, 0), jnp.moveaxis(chunk_kv, 2, 0)))
    states = jnp.moveaxis(states, 0, 2)
    o_inter = jnp.einsum('bhncd,bhnde->bhnce', qf * jnp.exp(b), states)
    return (o_intra + o_inter).reshape(B, H, T, dv).astype(v.dtype)


def gla_group(q, k, v, g, z_fwd, z_bwd, up_f, up_f_bias, up_b, up_b_bias, out_gain):
    B, T, _ = q.shape

    def heads(t, d):
        return t.reshape(B, T, GLA_HEADS, d).transpose(0, 2, 1, 3)

    qh = heads(q, GLA_DK) * (GLA_DK ** -0.5)
    kh = heads(k, GLA_DK)
    vh = heads(v, GLA_DV)
    la_f = heads(jax.nn.log_sigmoid((z_fwd @ up_f + up_f_bias).astype(jnp.float32)) / GLA_GATE_NORM, GLA_DK)
    la_b = heads(jax.nn.log_sigmoid((z_bwd @ up_b + up_b_bias).astype(jnp.float32)) / GLA_GATE_NORM, GLA_DK)
    o_f = gla_chunked(qh, kh, vh, la_f)
    flip = lambda t: jnp.flip(t, axis=2)
    o_b = flip(gla_chunked(flip(qh), flip(kh), flip(vh), flip(la_b)))
    o = rms_norm(o_f + o_b, out_gain)
    o = o.transpose(0, 2, 1, 3).reshape(B, T, GLA_WIDTH)
    return o * jax.nn.silu(g)


def axial_rope_tables(T):
    rows = T // GRID_W
    row = jnp.repeat(jnp.arange(rows), GRID_W).astype(jnp.float32)
    col = jnp.tile(jnp.arange(GRID_W), rows).astype(jnp.float32)
    axis_dim = ATT_HEAD_DIM // 2
    inv_freq = ROPE_THETA ** (-jnp.arange(0, axis_dim, 2, dtype=jnp.float32) / axis_dim)
    ang = jnp.concatenate([row[:, None] * inv_freq, col[:, None] * inv_freq], axis=-1)
    return jnp.cos(ang), jnp.sin(ang)


def apply_axial_rope(x, cos, sin):
    xr = x.astype(jnp.float32).reshape(*x.shape[:-1], -1, 2)
    x0, x1 = xr[..., 0], xr[..., 1]
    out = jnp.stack([x0 * cos - x1 * sin, x0 * sin + x1 * cos], axis=-1)
    return out.reshape(x.shape).astype(x.dtype)


def gqa_group(q, k, v, q_gain, k_gain, out_gain):
    B, T, _ = q.shape
    dh = ATT_HEAD_DIM
    G = ATT_HEADS // ATT_KV_HEADS
    qh = rms_norm(q.reshape(B, T, ATT_HEADS, dh).transpose(0, 2, 1, 3), q_gain)
    kh = rms_norm(k.reshape(B, T, ATT_KV_HEADS, dh).transpose(0, 2, 1, 3), k_gain)
    vh = v.reshape(B, T, ATT_KV_HEADS, dh).transpose(0, 2, 1, 3)
    cos, sin = axial_rope_tables(T)
    qh = apply_axial_rope(qh, cos, sin) * (dh ** -0.5)
    kh = apply_axial_rope(kh, cos, sin)
    nb = T // Q_BLOCK
    q_blocks = qh.reshape(B, ATT_KV_HEADS, G, nb, Q_BLOCK, dh).transpose(3, 0, 1, 2, 4, 5)

    def attend(qb):
        s = jnp.einsum('bkgqd,bksd->bkgqs', qb, kh).astype(jnp.float32)
        p = jax.nn.softmax(s, axis=-1).astype(vh.dtype)
        return jnp.einsum('bkgqs,bksd->bkgqd', p, vh)

    o = lax.map(attend, q_blocks)
    o = o.transpose(1, 0, 4, 2, 3, 5).reshape(B, T, ATT_WIDTH)
    return rms_norm(o, out_gain)


def hier_moe(x, w_group, b_group, w_expert, b_expert, w_gate, w_up, w_down):
    B, T, D = x.shape
    xt = x.reshape(-1, D)
    N = xt.shape[0]
    g_logits = (xt @ w_group + b_group).astype(jnp.float32)
    g_prob = jax.nn.softmax(g_logits, axis=-1)
    g_idx = jnp.argmax(g_logits, axis=-1)
    g_w = jnp.take_along_axis(g_prob, g_idx[:, None], axis=-1)
    e_logits = (xt @ w_expert + b_expert).astype(jnp.float32).reshape(N, N_GROUPS, EXPERTS_PER_GROUP)
    e_logits = jnp.take_along_axis(e_logits, g_idx[:, None, None], axis=1)[:, 0]
    top_val, top_idx = lax.top_k(e_logits, TOP_K)
    gate = g_w * jax.nn.softmax(top_val, axis=-1)
    expert_id = g_idx[:, None] * EXPERTS_PER_GROUP + top_idx

    M = N * TOP_K
    flat_e = expert_id.reshape(M).astype(jnp.int32)
    flat_tok = jnp.repeat(jnp.arange(N, dtype=jnp.int32), TOP_K)
    flat_gate = gate.reshape(M).astype(x.dtype)
    order = jnp.argsort(flat_e)
    se = flat_e[order]
    counts = jnp.bincount(flat_e, length=N_EXPERTS)
    starts = jnp.cumsum(counts) - counts
    padded = (counts + MOE_BLOCK - 1) // MOE_BLOCK * MOE_BLOCK
    pad_ends = jnp.cumsum(padded)
    pad_starts = pad_ends - padded
    dest = pad_starts[se] + (jnp.arange(M, dtype=jnp.int32) - starts[se])
    P = M + N_EXPERTS * MOE_BLOCK
    slot_tok = jnp.zeros((P,), jnp.int32).at[dest].set(flat_tok[order])
    slot_gate = jnp.zeros((P,), x.dtype).at[dest].set(flat_gate[order])
    nblk = P // MOE_BLOCK
    blk_start = jnp.arange(nblk, dtype=jnp.int32) * MOE_BLOCK
    blk_expert = jnp.minimum(jnp.searchsorted(pad_ends, blk_start, side='right'), N_EXPERTS - 1)
    xs = xt[slot_tok].reshape(nblk, MOE_BLOCK, D)

    def run_block(args):
        xb, e = args
        h = jax.nn.silu(xb @ w_gate[e]) * (xb @ w_up[e])
        return h @ w_down[e]

    ys = lax.map(run_block, (xs, blk_expert)).reshape(P, D)
    y = jnp.zeros((N, D), x.dtype).at[slot_tok].add(ys * slot_gate[:, None])
    return y.reshape(B, T, D)


def setup_inputs(seed: int = 0) -> dict:
    key = jax.random.key(seed)
    ks = jax.random.split(key, 24)
    f32 = jnp.float32
    L = DEPTH

    def nrm(k, shape, scale):
        return jax.random.normal(k, shape, f32) * scale

    def gain(k, shape):
        return 1.0 + 0.02 * jax.random.normal(k, shape, f32)

    return {
        "x": nrm(ks[0], (BATCH, SEQ, D_MODEL), 1.0),
        "norm1_gain": gain(ks[1], (L, D_MODEL)),
        "w_in": nrm(ks[2], (L, D_MODEL, D_IN), D_MODEL ** -0.5),
        "gla_up_fwd": nrm(ks[3], (L, GLA_GATE_RANK, GLA_KEY_WIDTH), GLA_GATE_RANK ** -0.5),
        "gla_up_fwd_bias": nrm(ks[4], (L, GLA_KEY_WIDTH), 0.1),
        "gla_up_bwd": nrm(ks[5], (L, GLA_GATE_RANK, GLA_KEY_WIDTH), GLA_GATE_RANK ** -0.5),
        "gla_up_bwd_bias": nrm(ks[6], (L, GLA_KEY_WIDTH), 0.1),
        "gla_out_gain": gain(ks[7], (L, GLA_DV)),
        "q_norm_gain": gain(ks[8], (L, ATT_HEAD_DIM)),
        "k_norm_gain": gain(ks[9], (L, ATT_HEAD_DIM)),
        "att_out_gain": gain(ks[10], (L, ATT_WIDTH)),
        "w_out": nrm(ks[11], (L, D_MIX, D_MODEL), D_MIX ** -0.5),
        "norm2_gain": gain(ks[12], (L, D_MODEL)),
        "w_group": nrm(ks[13], (L, D_MODEL, N_GROUPS), D_MODEL ** -0.5),
        "b_group": nrm(ks[14], (L, N_GROUPS), 0.01),
        "w_expert": nrm(ks[15], (L, D_MODEL, N_EXPERTS), D_MODEL ** -0.5),
        "b_expert": nrm(ks[16], (L, N_EXPERTS), 0.01),
        "w_gate": nrm(ks[17], (L, N_EXPERTS, D_MODEL, D_EXPERT), D_MODEL ** -0.5),
        "w_up": nrm(ks[18], (L, N_EXPERTS, D_MODEL, D_EXPERT), D_MODEL ** -0.5),
        "w_down": nrm(ks[19], (L, N_EXPERTS, D_EXPERT, D_MODEL), D_EXPERT ** -0.5),
        "final_gain": gain(ks[20], (D_MODEL,)),
    }


def reference(x, norm1_gain, w_in, gla_up_fwd, gla_up_fwd_bias, gla_up_bwd, gla_up_bwd_bias,
              gla_out_gain, q_norm_gain, k_norm_gain, att_out_gain, w_out, norm2_gain,
              w_group, b_group, w_expert, b_expert, w_gate, w_up, w_down, final_gain):
    split_points = np.cumsum(IN_SIZES)[:-1].tolist()
    h = x
    for l in range(DEPTH):
        u = rms_norm(h, norm1_gain[l])
        proj = u @ w_in[l]
        gq, gk, gv, gg, zf, zb, aq, ak, av = jnp.split(proj, split_points, axis=-1)
        o_gla = gla_group(gq, gk, gv, gg, zf, zb, gla_up_fwd[l], gla_up_fwd_bias[l],
                          gla_up_bwd[l], gla_up_bwd_bias[l], gla_out_gain[l])
        o_att = gqa_group(aq, ak, av, q_norm_gain[l], k_norm_gain[l], att_out_gain[l])
        mixed = jnp.concatenate([o_gla, o_att], axis=-1)
        h = h + mixed @ w_out[l]
        h = h + hier_moe(rms_norm(h, norm2_gain[l]), w_group[l], b_group[l], w_expert[l],
                         b_expert[l], w_gate[l], w_up[l], w_down[l])
    return rms_norm(h, final_gain)
```

```python
import numpy as np
from contextlib import ExitStack
import concourse.bass as bass
import concourse.mybir as mybir
from concourse.bass_utils import run_bass_kernel_spmd

F32 = mybir.dt.float32
BF16 = mybir.dt.bfloat16
I32 = mybir.dt.int32
AF = mybir.ActivationFunctionType
ALU = mybir.AluOpType
AX = mybir.AxisListType

SAME_ENGINE_SYNC = True
COMPUTE = ("pe", "act", "dve", "pool")


class V:
    __slots__ = ("ap", "keys")

    def __init__(self, ap, keys):
        self.ap = ap
        self.keys = tuple(keys)

    def __getitem__(self, idx):
        return V(self.ap[idx], self.keys)

    def re(self, s, **kw):
        return V(self.ap.rearrange(s, **kw), self.keys)

    def bc(self, shape):
        return V(self.ap.to_broadcast(list(shape)), self.keys)

    def bitcast(self, dt):
        return V(self.ap.bitcast(dt), self.keys)


class Tile:
    def __init__(self, b, name, handle, nslots):
        self.b = b
        self.name = name
        self.h = handle
        self.nslots = nslots

    def all(self):
        if self.nslots:
            return V(self.h[:], [(self.name, i) for i in range(self.nslots)])
        return V(self.h[:], [(self.name, None)])

    def s(self, i):
        assert self.nslots and 0 <= i < self.nslots
        return V(self.h[:, i], [(self.name, i)])

    def __getitem__(self, idx):
        return self.all()[idx]


class Builder:
    def __init__(self, nc, n_dma_sems=24):
        self.nc = nc
        self.prog = {e: [] for e in ("pe", "act", "dve", "pool", "sp")}
        self.waited = {e: {} for e in self.prog}
        self.lastw = {}
        self.readers = {}
        self.n_dma = n_dma_sems
        self.dma_cnt = {"sp": [0] * n_dma_sems, "pool": [0] * n_dma_sems, "act": [0] * n_dma_sems}
        self.dma_rr = {"sp": 0, "pool": 0, "act": 0}
        self.stack = None
        self.uid = 0
        self.out_tokens = []
        self.latest = {}
        self.bounds_reg = None
        self.marks = {}

    def sbuf(self, name, shape, dtype, nslots=0):
        h = self.stack.enter_context(self.nc.sbuf_tensor(name, list(shape), dtype))
        return Tile(self, name, h, nslots)

    def psum(self, name, shape, dtype, nslots=0):
        h = self.stack.enter_context(self.nc.psum_tensor(name, list(shape), dtype))
        return Tile(self, name, h, nslots)

    def dram(self, name, shape, dtype, kind="Internal"):
        t = self.nc.dram_tensor(name, list(shape), dtype, kind=kind)
        return V(t.ap(), [(name, None)])

    def _deps(self, eng, reads, writes, skip_self):
        toks = []
        for v in reads:
            for k in v.keys:
                toks += list(self.lastw.get(k, {}).items())
        for v in writes:
            for k in v.keys:
                toks += list(self.lastw.get(k, {}).items())
                toks += list(self.readers.get(k, {}).items())
        need = {}
        for src, val in toks:
            if src == ("e", eng) and (skip_self or not SAME_ENGINE_SYNC or eng == "pe"):
                continue
            if self.waited[eng].get(src, -1) >= val:
                continue
            if need.get(src, -1) < val:
                need[src] = val
        for src, val in need.items():
            self.waited[eng][src] = val
        return list(need.items())

    def _commit(self, tok, reads, writes, partial):
        src, val = tok
        if self.latest.get(src, -1) < val:
            self.latest[src] = val
        for v in reads:
            for k in v.keys:
                d = self.readers.setdefault(k, {})
                if d.get(src, -1) < val:
                    d[src] = val
        for v in writes:
            for k in v.keys:
                d = self.lastw.setdefault(k, {})
                if d.get(src, -1) < val:
                    d[src] = val

    def op(self, eng, fn, reads=(), writes=(), skip_self=False, partial=False):
        reads = [r for r in reads if isinstance(r, V)]
        writes = [w for w in writes if isinstance(w, V)]
        waits = self._deps(eng, reads, writes, skip_self)
        idx = len(self.prog[eng])
        tok = (("e", eng), idx)
        self.prog[eng].append(["op", waits, fn, None])
        self._commit(tok, reads, writes, partial)
        return tok

    def dma(self, q, fn, reads=(), writes=(), partial=False):
        reads = [r for r in reads if isinstance(r, V)]
        writes = [w for w in writes if isinstance(w, V)]
        i = self.dma_rr[q]
        self.dma_rr[q] = (i + 1) % self.n_dma
        src = ("d", q, i)
        waits = self._deps(q, reads, writes, False)
        prev = self.dma_cnt[q][i]
        if prev > 0 and self.waited[q].get(src, -1) < prev * 16:
            waits.append((src, prev * 16))
            self.waited[q][src] = prev * 16
        self.dma_cnt[q][i] += 1
        tok = (src, self.dma_cnt[q][i] * 16)
        self.prog[q].append(["dma", waits, fn, src])
        self._commit(tok, reads, writes, partial)
        return tok

    def mark(self, name):
        if name in self.marks:
            return
        self.barrier()
        self.marks[name] = {e: len(v) for e, v in self.prog.items()}

    def barrier(self):
        for eng in self.prog:
            need = {}
            for src, val in self.latest.items():
                if src == ("e", eng) and (eng == "pe" or not SAME_ENGINE_SYNC):
                    continue
                if self.waited[eng].get(src, -1) >= val:
                    continue
                need[src] = val
            for src, val in need.items():
                self.waited[eng][src] = val
            if need:
                self.prog[eng].append(["op", list(need.items()), None, None])

    def wait_all(self, eng, toks):
        need = {}
        for src, val in toks:
            if self.waited[eng].get(src, -1) >= val:
                continue
            if need.get(src, -1) < val:
                need[src] = val
        self.prog[eng].append(["op", list(need.items()), None, None])

    def emit(self, trunc=None, tail=None):
        nc = self.nc
        if trunc is not None:
            self.prog = {e: v[:self.marks[trunc][e]] for e, v in self.prog.items()}
            tail(self)
        signal = {e: set() for e in COMPUTE}
        for e, lst in self.prog.items():
            for item in lst:
                for src, val in item[1]:
                    if src[0] == "e":
                        signal[src[1]].add(val)
        rank = {}
        for e in COMPUTE:
            r = {}
            c = 0
            for idx in sorted(signal[e]):
                c += 1
                r[idx] = c
            rank[e] = r
        from contextlib import ExitStack
        with ExitStack() as st:
            esem = {e: st.enter_context(nc.semaphore("sem_" + e)) for e in COMPUTE}
            dsem = {}
            for q in ("sp", "pool", "act"):
                for i in range(self.n_dma):
                    if self.dma_cnt[q][i] > 0:
                        dsem[("d", q, i)] = st.enter_context(nc.semaphore("dsem_%s_%d" % (q, i)))
            block = st.enter_context(nc.Block())

            def run(ename, eng):
                for idx, (kind, waits, fn, dsrc) in enumerate(self.prog[ename]):
                    for src, val in waits:
                        if src[0] == "e":
                            eng.wait_ge(esem[src[1]], rank[src[1]][val])
                        else:
                            eng.wait_ge(dsem[src], val)
                    if fn is None:
                        continue
                    ins = fn(eng)
                    if kind == "dma":
                        ins.then_inc(dsem[dsrc], 16)
                    elif ename in COMPUTE and idx in rank[ename]:
                        ins.then_inc(esem[ename], 1)

            @block.tensor
            def _(t):
                run("pe", t)

            @block.scalar
            def _(a):
                run("act", a)

            @block.vector
            def _(v):
                run("dve", v)

            @block.gpsimd
            def _(g):
                run("pool", g)

            @block.sync
            def _(s):
                run("sp", s)

    def matmul(self, out, lhsT, rhs, start=True, stop=True):
        return self.op("pe", lambda e: e.matmul(out.ap, lhsT=lhsT.ap, rhs=rhs.ap, start=start, stop=stop),
                       reads=[lhsT, rhs], writes=[out], partial=not start)

    def transpose(self, out, in_, ident):
        return self.op("pe", lambda e: e.transpose(out=out.ap, in_=in_.ap, identity=ident.ap),
                       reads=[in_, ident], writes=[out], partial=True)

    def act(self, out, in_, func, bias=0.0, scale=1.0, accum_out=None, eng="act"):
        ba = bias.ap if isinstance(bias, V) else bias
        sa = scale.ap if isinstance(scale, V) else scale
        kw = {}
        if accum_out is not None:
            kw["accum_out"] = accum_out.ap
        return self.op("act", lambda e: e.activation(out=out.ap, in_=in_.ap, func=func, bias=ba, scale=sa, **kw),
                       reads=[in_, bias, scale], writes=[out] + ([accum_out] if accum_out is not None else []))

    def ts(self, out, in0, s1, s2=None, op0=ALU.mult, op1=None, eng="dve", accum_out=None):
        a1 = s1.ap if isinstance(s1, V) else s1
        a2 = s2.ap if isinstance(s2, V) else s2
        kw = {}
        if op1 is not None:
            kw["op1"] = op1
        if accum_out is not None:
            kw["accum_out"] = accum_out.ap
        return self.op(eng, lambda e: e.tensor_scalar(out=out.ap, in0=in0.ap, scalar1=a1, scalar2=a2, op0=op0, **kw),
                       reads=[in0, s1, s2], writes=[out] + ([accum_out] if accum_out is not None else []))

    def tt(self, out, in0, in1, op, eng="dve"):
        return self.op(eng, lambda e: e.tensor_tensor(out=out.ap, in0=in0.ap, in1=in1.ap, op=op),
                       reads=[in0, in1], writes=[out])

    def stt(self, out, in0, scalar, in1, op0, op1):
        sa = scalar.ap if isinstance(scalar, V) else scalar
        return self.op("dve", lambda e: e.scalar_tensor_tensor(out=out.ap, in0=in0.ap, scalar=sa, in1=in1.ap, op0=op0, op1=op1),
                       reads=[in0, scalar, in1], writes=[out])

    def copy(self, out, in_, eng="dve"):
        if eng == "act":
            return self.op("act", lambda e: e.copy(out=out.ap, in_=in_.ap), reads=[in_], writes=[out])
        return self.op(eng, lambda e: e.tensor_copy(out=out.ap, in_=in_.ap), reads=[in_], writes=[out])

    def reduce(self, out, in_, op, axis=AX.X):
        return self.op("dve", lambda e: e.tensor_reduce(out=out.ap, in_=in_.ap, axis=axis, op=op),
                       reads=[in_], writes=[out])

    def recip(self, out, in_):
        return self.op("dve", lambda e: e.reciprocal(out=out.ap, in_=in_.ap), reads=[in_], writes=[out])

    def max8(self, out, in_):
        return self.op("dve", lambda e: e.max(out=out.ap, in_=in_.ap), reads=[in_], writes=[out])

    def scan(self, out, d0, d1, initial, op0, op1):
        ia = initial.ap if isinstance(initial, V) else initial
        return self.op("dve", lambda e: e.tensor_tensor_scan(out=out.ap, data0=d0.ap, data1=d1.ap, initial=ia, op0=op0, op1=op1),
                       reads=[d0, d1, initial], writes=[out])

    def memset(self, out, val, eng="dve"):
        return self.op(eng, lambda e: e.memset(out.ap, val), writes=[out])

    def iota(self, out, pattern, base=0, channel_multiplier=0, allow=False):
        return self.op("pool", lambda e: e.iota(out.ap, pattern=pattern, base=base, channel_multiplier=channel_multiplier,
                                                allow_small_or_imprecise_dtypes=allow), writes=[out])

    def load(self, out, in_, q="sp", partial=False):
        return self.dma(q, lambda e: e.dma_start(out=out.ap, in_=in_.ap), reads=[in_], writes=[out], partial=partial)

    def bcast_load(self, out, vec):
        return self.dma("sp", lambda e: e.dma_start(out=out.ap, in_=vec.ap.partition_broadcast(128)), reads=[vec], writes=[out])

    def gather(self, out, in_, idx, bounds=None, partial=False):
        def fn(e):
            kw = {}
            if bounds is not None:
                if self.bounds_reg is None:
                    self.bounds_reg = (e.alloc_register("gather_bound"), bounds)
                    e.reg_mov(self.bounds_reg[0], bounds)
                assert self.bounds_reg[1] == bounds
                kw["bounds_check"] = self.bounds_reg[0]
                kw["oob_is_err"] = False
            return e.indirect_dma_start(out=out.ap, out_offset=None, in_=in_.ap,
                                        in_offset=bass.IndirectOffsetOnAxis(ap=idx.ap, axis=0), **kw)
        return self.dma("pool", fn, reads=[in_, idx], writes=[out], partial=partial)

    def scatter(self, out, in_, idx, bounds=None):
        def fn(e):
            kw = {}
            if bounds is not None:
                kw["bounds_check"] = bounds
                kw["oob_is_err"] = False
            return e.indirect_dma_start(out=out.ap, out_offset=bass.IndirectOffsetOnAxis(ap=idx.ap, axis=0),
                                        in_=in_.ap, in_offset=None, **kw)
        return self.dma("pool", fn, reads=[in_, idx], writes=[out], partial=True)


D = 1024
NK = 8
EPS = 1e-6
BIG = 1.0e4


def build(T, debug=False, trunc=None):
    NT = T // 128
    NG = T // 512
    RB = 256
    SUB = RB // 128
    NB = 2 * T // RB + 63
    NBMAX = 2 * T // RB
    nc = bass.Bass("TRN2", target_bir_lowering=False)
    b = Builder(nc)

    def din(name, shape, dt=F32):
        return V(nc.dram_tensor(name, list(shape), dt, kind="ExternalInput").ap(), [(name, None)])

    def dout(name, shape, dt=F32, kind="ExternalOutput"):
        return V(nc.dram_tensor(name, list(shape), dt, kind=kind).ap(), [(name, None)])

    x = din("x", [T, D])
    w_in = din("w_in", [D, 2336])
    g1pk = din("g1pk", [128, 8])
    up_f = din("up_f", [16, 256]); up_b = din("up_b", [16, 256])
    bias_f = din("bias_f", [128, 2]); bias_b = din("bias_b", [128, 2])
    gla_gain = din("gla_gain", [128])
    q_gain = din("q_gain", [64]); k_gain = din("k_gain", [64]); att_gain = din("att_gain", [512])
    w_out = din("w_out", [D, D]); g2 = din("g2", [D])
    w_group = din("w_group", [D, 8]); b_group = din("b_group", [8])
    w_expert = din("w_expert", [D, 64]); b_expert = din("b_expert", [64])
    w_gate = din("w_gate", [64, D, 512]); w_up = din("w_up", [64, D, 512]); w_down = din("w_down", [64, 512, D])
    fgain = din("fgain", [D])
    c_ident = din("c_ident", [128, 128]); c_mask4 = din("c_mask4", [128, 512]); c_lstrict = din("c_lstrict", [128, 128])
    c_cos = din("c_cos", [128, NT, 32]); c_sin = din("c_sin", [128, NT, 32])
    c_pidx = din("c_pidx", [128, 1]); c_thr = din("c_thr", [128, NB]); c_bd = din("c_bd", [128, 256])
    out = dout("out", [T, D])
    dk = "ExternalOutput" if debug else "Internal"
    h1_d = dout("h1_d", [T, D], F32, dk)
    xs_d = dout("xs_d", [NB * RB, D], BF16, dk)
    ys_d = dout("ys_d", [NB * RB, D], F32, dk)
    mp_d = dout("mp_d", [128, NT * 192], F32, "Internal")
    xn2_d = dout("xn2_d", [T, D], BF16, "Internal")
    if debug:
        dbg_mixg = dout("dbg_mixg", [128, 4, T], BF16)
        dbg_mixa = dout("dbg_mixa", [128, 4, T], BF16)
        dbg_route = dout("dbg_route", [128, NT, 4])
        dbg_be = dout("dbg_be", [128, NB])

    with ExitStack() as st0:
        b.stack = st0
        ident_f = b.sbuf("ident_f", [128, 128], F32); ident_b = b.sbuf("ident_b", [128, 128], BF16)
        mask4 = b.sbuf("mask4", [128, 512], F32)
        lstrict_f = b.sbuf("lstrict_f", [128, 128], F32); lstrict_b = b.sbuf("lstrict_b", [128, 128], BF16)
        ones_b = b.sbuf("ones_b", [128, 128], BF16); ones_f = b.sbuf("ones_f", [128, 128], F32)
        zeros_b = b.sbuf("zeros_b", [128, 1024], BF16)
        zeros_f = b.sbuf("zeros_f", [128, 1024], F32)
        b.memset(zeros_f[:], 0.0, eng="pool")
        pidx = b.sbuf("pidx", [128, 1], F32); thr = b.sbuf("thr", [128, NB], F32)
        g1 = b.sbuf("g1", [128, 8], F32)
        b.load(ident_f[:], c_ident); b.load(mask4[:], c_mask4); b.load(lstrict_f[:], c_lstrict)
        bdm = b.sbuf("bdm", [128, 256], F32)
        b.load(bdm[:], c_bd)
        b.load(pidx[:], c_pidx); b.load(thr[:], c_thr); b.load(g1[:], g1pk)
        b.copy(ident_b[:], ident_f[:]); b.copy(lstrict_b[:], lstrict_f[:])
        b.memset(ones_b[:], 1.0); b.memset(ones_f[:], 1.0); b.memset(zeros_b[:], 0.0, eng="pool")
        ones512 = b.sbuf("ones512", [128, 512], F32)
        b.memset(ones512[:], 1.0)
        gates = b.sbuf("gates", [128, NT, 2], F32)
        dest_i = b.sbuf("dest_i", [128, NT, 2], I32)
        idxw_i = b.sbuf("idxw_i", [128, NB], I32)
        run = b.sbuf("run", [128, 64], F32)
        b.memset(run[:], 0.0)
        stS1 = ExitStack(); stS1.__enter__(); b.stack = stS1
        mixg = b.sbuf("mixg", [128, 4, T], BF16)
        STK = stS1

        def xn_group_factory(stk, psum_tr):
            xt = b.sbuf("xt" + stk, [128, 2, D], F32, nslots=2)
            junk = b.sbuf("junk" + stk, [128, D], BF16)
            xnb = b.sbuf("xnb" + stk, [128, 2, D], BF16, nslots=2)
            xnT = b.sbuf("xnT" + stk, [128, 2, NK, 512], BF16, nslots=2)
            st1 = b.sbuf("st1" + stk, [128, 2, 4], F32, nslots=2)

            def load_tile(i):
                b.load(xt.s(i % 2), x[i * 128:(i + 1) * 128, :])

            def group(g):
                gs = g % 2
                for j in range(4):
                    i = g * 4 + j
                    s = i % 2
                    if i == 0:
                        load_tile(0)
                    if i + 1 < NT:
                        load_tile(i + 1)
                    ss = st1.s(s)
                    b.act(junk[:], xt.s(s), AF.Square, accum_out=ss[:, 0:1])
                    b.act(ss[:, 1:2], ss[:, 0:1], AF.Ln, bias=EPS, scale=1.0 / D)
                    b.act(ss[:, 2:3], ss[:, 1:2], AF.Exp, scale=-0.5)
                    b.ts(xnb.s(s), xt.s(s), ss[:, 2:3], None, op0=ALU.mult)
                    ptr_s = psum_tr.s(s)
                    for k in range(NK):
                        b.transpose(ptr_s[:, k, :], xnb.s(s)[:, k * 128:(k + 1) * 128], ident_b[:])
                    b.copy(xnT.s(gs)[:, :, j * 128:(j + 1) * 128], ptr_s, eng="act")
                return xnT.s(gs)
            return group

        def load_w_cols(Wb, stage, cols):
            c0 = 0
            for (a, z) in cols:
                n = z - a
                for k in range(NK):
                    sl = stage.s(k % 2)
                    b.load(sl[:, 0:n], w_in[k * 128:(k + 1) * 128, a:z])
                    b.ts(Wb[:, k, c0:c0 + n], sl[:, 0:n], g1[:, k:k + 1], None, op0=ALU.mult, eng="dve")
                c0 += n
        for dt in range(2):
            with ExitStack() as stg:
                b.stack = stg
                stg.callback(b.barrier)
                sfx = "g%d" % dt
                Wb = b.sbuf("Wb" + sfx, [128, NK, 800], BF16)
                wst = b.sbuf("wst" + sfx, [128, 2, 256], F32, nslots=2)
                load_w_cols(Wb, wst, [(dt * 128, dt * 128 + 128), (256 + dt * 128, 256 + dt * 128 + 128),
                                      (512 + dt * 256, 512 + dt * 256 + 256), (1024 + dt * 256, 1024 + dt * 256 + 256),
                                      (1536, 1552), (1552, 1568)])
                upf = b.sbuf("upf" + sfx, [16, 128], F32); upb = b.sbuf("upb" + sfx, [16, 128], F32)
                b.load(upf[:], up_f[:, dt * 128:(dt + 1) * 128]); b.load(upb[:], up_b[:, dt * 128:(dt + 1) * 128])
                nbias = b.sbuf("nbias" + sfx, [128, 2], F32)
                bst = b.sbuf("bst" + sfx, [128, 2], F32)
                bst2 = b.sbuf("bst2" + sfx, [128, 2], F32)
                b.load(bst[:], bias_f); b.load(bst2[:], bias_b)
                b.ts(nbias[:, 0:1], bst[:, dt:dt + 1], -1.0, None, op0=ALU.mult)
                b.ts(nbias[:, 1:2], bst2[:, dt:dt + 1], -1.0, None, op0=ALU.mult)
                ggain = b.sbuf("ggain" + sfx, [128, 128], F32)
                b.bcast_load(ggain[:], gla_gain)
                qef = b.sbuf("qef" + sfx, [128, T], BF16); kef = b.sbuf("kef" + sfx, [128, T], BF16)
                qeb = b.sbuf("qeb" + sfx, [128, T], BF16); keb = b.sbuf("keb" + sfx, [128, T], BF16)
                decf = b.sbuf("decf" + sfx, [128, NT], F32); decb = b.sbuf("decb" + sfx, [128, NT], F32)
                v_tm = b.sbuf("v_tm" + sfx, [128, NT, 256], BF16)
                G2 = b.sbuf("G2" + sfx, [128, NT, 256], BF16)
                zT = b.sbuf("zT" + sfx, [16, 2, 512], F32)
                qk32 = b.sbuf("qk32" + sfx, [128, 2, 512], F32)
                Pex = b.sbuf("Pex" + sfx, [128, 513], F32)
                Dd = b.sbuf("Dd" + sfx, [128, 512], F32)
                Ee = b.sbuf("Ee" + sfx, [128, 2, 512], F32)
                lap = b.sbuf("lap" + sfx, [128, 512], F32)
                gt = b.sbuf("gt" + sfx, [128, 3, 256], F32)
                with ExitStack() as stx:
                    b.stack = stx
                    stx.callback(b.barrier)
                    ptr = b.psum("ptr" + sfx, [128, 2, NK, 128], BF16, nslots=2)
                    pfm = b.psum("pfm" + sfx, [128, 2, 512], F32, nslots=2)
                    ptm = b.psum("ptm" + sfx, [128, 2, 512], F32, nslots=2)
                    pz = b.psum("pz" + sfx, [16, 2, 512], F32, nslots=2)
                    xgroup = xn_group_factory(sfx, ptr)
                    b.memset(Pex[:, 0:1], 0.0)
                    for g in range(NG):
                        xg = xgroup(g)
                        tok = slice(g * 512, (g + 1) * 512)
                        for qi in range(2):
                            for k in range(NK):
                                b.matmul(pfm.s(qi), Wb[:, k, qi * 128:(qi + 1) * 128], xg[:, k, :], start=(k == 0), stop=(k == NK - 1))
                            b.op("act", (lambda o_, i_, m_: (lambda e: e.mul(out=o_.ap, in_=i_.ap, mul=m_)))(qk32[:, qi, :], pfm.s(qi), (0.125 if qi == 0 else 1.0)), reads=[pfm.s(qi)], writes=[qk32[:, qi, :]])
                        for zi in range(2):
                            for k in range(NK):
                                b.matmul(pz.s(zi), Wb[:, k, 768 + zi * 16:768 + zi * 16 + 16], xg[:, k, :], start=(k == 0), stop=(k == NK - 1))
                            b.copy(zT[:, zi, :], pz.s(zi))
                        for di in range(2):
                            up = upf if di == 0 else upb
                            pl = pfm.s(di)
                            b.matmul(pl, up[:], zT[:, di, :])
                            b.act(lap[:], pl, AF.Exp, bias=nbias[:, di:di + 1], scale=-1.0)
                            b.act(lap[:], lap[:], AF.Ln, bias=1.0)
                            b.scan(Pex[:, 1:513], ones512[:], lap[:], 0.0, ALU.mult, ALU.add)
                            Pc = Pex[:, 1:513].re("p (n c) -> p n c", c=128)
                            Pe = Pex[:, 0:512].re("p (n c) -> p n c", c=128)
                            D3 = Dd[:].re("p (n c) -> p n c", c=128)
                            if di == 0:
                                b.tt(D3, Pc, Pe[:, :, 0:1].bc([128, 4, 128]), ALU.subtract)
                            else:
                                b.tt(D3, Pc[:, :, 127:128].bc([128, 4, 128]), Pe, ALU.subtract)
                            b.act(Ee[:, 0, :], Dd[:], AF.Exp, scale=-1.0 / 16.0)
                            b.act(Ee[:, 1, :], Dd[:], AF.Exp, scale=1.0 / 16.0)
                            qe, ke, dec = (qef, kef, decf) if di == 0 else (qeb, keb, decb)
                            b.tt(qe[:, tok], qk32[:, 0, :], Ee[:, 0, :], ALU.mult)
                            b.tt(ke[:, tok], qk32[:, 1, :], Ee[:, 1, :], ALU.mult)
                            E3 = Ee[:, 0, :].re("p (n c) -> p n c", c=128)
                            col = 127 if di == 0 else 0
                            b.copy(dec[:, g * 4:(g + 1) * 4].re("p (n o) -> p n o", o=1), E3[:, :, col:col + 1])
                        for j in range(4):
                            i = g * 4 + j
                            xl = [xg[:, k, j * 128:(j + 1) * 128] for k in range(NK)]
                            pv = ptm.s(0)
                            for k in range(NK):
                                b.matmul(pv[:, 0:256], xl[k], Wb[:, k, 256:512], start=(k == 0), stop=(k == NK - 1))
                            b.copy(v_tm[:, i, :], pv[:, 0:256], eng="act")
                            pg = ptm.s(1)
                            for k in range(NK):
                                b.matmul(pg[:, 0:256], xl[k], Wb[:, k, 512:768], start=(k == 0), stop=(k == NK - 1))
                            b.act(gt[:, 0, :], pg[:, 0:256], AF.Exp, scale=-1.0)
                            b.ts(gt[:, 0, :], gt[:, 0, :], 1.0, None, op0=ALU.add)
                            b.recip(gt[:, 1, :], gt[:, 0, :])
                            b.tt(gt[:, 2, :], gt[:, 1, :], pg[:, 0:256], ALU.mult)
                            b.tt(G2[:, i, :].re("p (h e) -> p h e", h=2), gt[:, 2, :].re("p (h e) -> p h e", h=2),
                                 ggain[:].re("p (o e) -> p o e", o=1).bc([128, 2, 128]), ALU.mult)
                b.mark('glaprep%d' % dt)
                with ExitStack() as stc:
                    b.stack = stc
                    stc.callback(b.barrier)
                    ketm = b.sbuf("ketm" + sfx, [128, 2, 128], BF16, nslots=2)
                    Sf = b.sbuf("Sf" + sfx, [128, NT, 256], BF16)
                    Sb = b.sbuf("Sb" + sfx, [128, 2, 256], BF16, nslots=2)
                    Tst = b.sbuf("Tst" + sfx, [128, 256], F32)
                    Am = b.sbuf("Am" + sfx, [128, 4, 128], BF16)
                    osb = b.sbuf("osb" + sfx, [128, 256], F32)
                    omx = b.sbuf("omx" + sfx, [128, 256], BF16)
                    jk = b.sbuf("jk" + sfx, [128, 128], BF16)
                    stt_ = b.sbuf("stt" + sfx, [128, 8], F32)
                    pkv_ = b.psum("pkv" + sfx, [128, 512], F32); pkv = pkv_[:, 0:256]
                    pa0 = b.psum("pa0" + sfx, [128, 4, 128], F32)
                    pa1 = b.psum("pa1" + sfx, [128, 4, 128], F32)
                    po_ = b.psum("po" + sfx, [128, 512], F32); po = po_[:, 0:256]
                    ptk = b.psum("ptk" + sfx, [128, 8, 128], BF16)
                    for n in range(NT):
                        ck = slice(n * 128, (n + 1) * 128)
                        if n >= 1:
                            b.stt(Sf[:, n, :], Tst[:], decf[:, n - 1:n], bdm[:], ALU.mult, ALU.mult)
                        if n == NT - 1:
                            break
                        b.transpose(ptk[:, 0, :], kef[:, ck], ident_b[:])
                        b.copy(ketm.s(n % 2), ptk[:, 0, :], eng="act")
                        b.matmul(pkv, ketm.s(n % 2), v_tm[:, n, :])
                        if n == 0:
                            b.copy(Tst[:], pkv)
                        else:
                            b.stt(Tst[:], Tst[:], decf[:, n - 1:n], pkv, ALU.mult, ALU.add)
                    for n in range(NT - 1, -1, -1):
                        ck = slice(n * 128, (n + 1) * 128)
                        sbc = Sb.s(n % 2)
                        if n < NT - 1:
                            b.stt(sbc, Tst[:], decb[:, n + 1:n + 2], bdm[:], ALU.mult, ALU.mult)
                        for hl in range(2):
                            pr = slice(hl * 64, (hl + 1) * 64)
                            pah = pa0 if hl == 0 else pa1
                            b.matmul(pah[:, 0, :], kef[pr, ck], qef[pr, ck])
                            b.matmul(pah[:, 1, :], keb[pr, ck], qeb[pr, ck])
                        for hl in range(2):
                            pah = pa0 if hl == 0 else pa1
                            b.tt(Am[:, 2 * hl:2 * hl + 2, :].re("p a c -> p (a c)"), pah[:, 0:2, :].re("p a c -> p (a c)"), mask4[:, 128:384], ALU.mult)
                        for hl in range(2):
                            pr = slice(hl * 64, (hl + 1) * 64)
                            es = slice(hl * 128, (hl + 1) * 128)
                            mms = [(Am[:, 2 * hl, :], v_tm[:, n, es]), (Am[:, 2 * hl + 1, :], v_tm[:, n, es])]
                            if n > 0:
                                mms.append((qef[:, ck], Sf[:, n, es]))
                            if n < NT - 1:
                                mms.append((qeb[:, ck], sbc[:, es]))
                            for mi, (l_, r_) in enumerate(mms):
                                b.matmul(po[:, es], l_, r_, start=(mi == 0), stop=(mi == len(mms) - 1))
                        if n > 0:
                            b.transpose(ptk[:, 1, :], keb[:, ck], ident_b[:])
                            b.copy(ketm.s(n % 2), ptk[:, 1, :], eng="act")
                            b.matmul(pkv, ketm.s(n % 2), v_tm[:, n, :])
                            if n == NT - 1:
                                b.copy(Tst[:], pkv)
                            else:
                                b.stt(Tst[:], Tst[:], decb[:, n + 1:n + 2], pkv, ALU.mult, ALU.add)
                        for hl in range(2):
                            es = slice(hl * 128, (hl + 1) * 128)
                            b.act(jk[:], po[:, es], AF.Square, accum_out=stt_[:, hl:hl + 1])
                        b.act(stt_[:, 2:4], stt_[:, 0:2], AF.Ln, bias=EPS, scale=1.0 / 128)
                        b.act(stt_[:, 4:6], stt_[:, 2:4], AF.Exp, scale=-0.5)
                        b.tt(osb[:].re("p (h e) -> p h e", h=2), po.re("p (h e) -> p h e", h=2),
                             stt_[:, 4:6].re("p (h o) -> p h o", o=1).bc([128, 2, 128]), ALU.mult)
                        b.tt(omx[:], osb[:], G2[:, n, :], ALU.mult)
                        for hl in range(2):
                            b.transpose(ptk[:, hl, :], omx[:, hl * 128:(hl + 1) * 128], ident_b[:])
                        b.copy(mixg[:, 2 * dt:2 * dt + 2, ck], ptk[:, 0:2, :])
            b.stack = STK
            b.mark('gla%d' % dt)
        with ExitStack() as sta:
            b.stack = sta
            sta.callback(b.barrier)
            mixa = b.sbuf("mixa", [128, 4, T], BF16)
            qT = b.sbuf("qT_att", [128, 4, T], BF16)
            kT = b.sbuf("kT_att", [128, T], BF16)
            Vaug = b.sbuf("Vaug", [128, NT, 2, 65], BF16)
            b.memset(Vaug[:, :, :, 64:65], 1.0)
            qg = b.sbuf("qg_row", [128, 64], F32); kg = b.sbuf("kg_row", [128, 64], F32)
            b.bcast_load(qg[:], q_gain)
            b.bcast_load(kg[:], k_gain)
            b.ts(qg[:], qg[:], 0.125, None, op0=ALU.mult)
            agr = b.sbuf("agr", [128, 512], F32)
            b.bcast_load(agr[:], att_gain)
            cosb = b.sbuf("cosb", [128, NT, 32], F32); sinb = b.sbuf("sinb", [128, NT, 32], F32)
            b.load(cosb[:], c_cos); b.load(sinb[:], c_sin)
            with ExitStack() as stx:
                b.stack = stx
                stx.callback(b.barrier)
                ptr = b.psum("ptr_a", [128, 2, NK, 128], BF16, nslots=2)
                ptm = b.psum("ptm_a", [128, 2, 512], F32, nslots=2)
                ptq = b.psum("ptq_a", [128, 8, 128], BF16)
                Wb = b.sbuf("Wb_a", [128, NK, 768], BF16)
                wst = b.sbuf("wst_a", [128, 2, 512], F32, nslots=2)
                load_w_cols(Wb, wst, [(1568, 2080), (2080, 2336)])
                sq = b.sbuf("sq_a", [128, 512], F32)
                s8 = b.sbuf("s8_a", [128, 24], F32)
                qn = b.sbuf("qn_a", [128, 512], F32)
                t1 = b.sbuf("t1_a", [128, 256], F32); t2 = b.sbuf("t2_a", [128, 256], F32)
                qr = b.sbuf("qr_a", [128, 512], BF16)
                kr = b.sbuf("kr_a", [128, 128], BF16)
                xgroup = xn_group_factory("a", ptr)

                def norm_rope(src_ps, hd, grow, i, dst_even, dst_odd):
                    nh = 1
                    for z_ in hd:
                        nh *= z_
                    w = nh * 64
                    b.act(sq[:, 0:w], src_ps, AF.Square)
                    b.reduce(s8[:, 0:nh], sq[:, 0:w].re("p (h d) -> p h d", d=64), ALU.add)
                    b.act(s8[:, 8:8 + nh], s8[:, 0:nh], AF.Ln, bias=EPS, scale=1.0 / 64)
                    b.act(s8[:, 16:16 + nh], s8[:, 8:8 + nh], AF.Exp, scale=-0.5)
                    b.tt(qn[:, 0:w].re("p (h d) -> p h d", d=64), src_ps.re("p (h d) -> p h d", d=64),
                         s8[:, 16:16 + nh].re("p (h o) -> p h o", o=1).bc([128, nh, 64]), ALU.mult)
                    b.tt(qn[:, 0:w].re("p (h d) -> p h d", d=64), qn[:, 0:w].re("p (h d) -> p h d", d=64),
                         grow[:].re("p (o d) -> p o d", o=1).bc([128, nh, 64]), ALU.mult)
                    if len(hd) == 2:
                        pat = "p (k g i two) -> p k g i two"; kw = dict(k=hd[0], g=hd[1], i=32, two=2)
                        pat3 = "p (k g i) -> p k g i"; kw3 = dict(k=hd[0], g=hd[1], i=32)
                        patc = "p (a c i) -> p a c i"; kwc = dict(a=1, c=1)
                        shp = [128, hd[0], hd[1], 32]
                        q4 = qn[:, 0:w].re(pat, **kw)
                        x0 = q4[:, :, :, :, 0]; x1 = q4[:, :, :, :, 1]
                    else:
                        pat = "p (k i two) -> p k i two"; kw = dict(k=hd[0], i=32, two=2)
                        pat3 = "p (k i) -> p k i"; kw3 = dict(k=hd[0], i=32)
                        patc = "p (a i) -> p a i"; kwc = dict(a=1)
                        shp = [128, hd[0], 32]
                        q4 = qn[:, 0:w].re(pat, **kw)
                        x0 = q4[:, :, :, 0]; x1 = q4[:, :, :, 1]
                    cb = cosb[:, i, :].re(patc, **kwc).bc(shp)
                    sb_ = sinb[:, i, :].re(patc, **kwc).bc(shp)
                    hw = nh * 32
                    a1 = t1[:, 0:hw].re(pat3, **kw3); a2 = t2[:, 0:hw].re(pat3, **kw3)
                    b.tt(a1, x0, cb, ALU.mult); b.tt(a2, x1, sb_, ALU.mult)
                    b.tt(dst_even, a1, a2, ALU.subtract)
                    b.tt(a1, x0, sb_, ALU.mult); b.tt(a2, x1, cb, ALU.mult)
                    b.tt(dst_odd, a1, a2, ALU.add)

                for g in range(NG):
                    xg = xgroup(g)
                    for j in range(4):
                        i = g * 4 + j
                        tk = slice(i * 128, (i + 1) * 128)
                        xl = [xg[:, k, j * 128:(j + 1) * 128] for k in range(NK)]
                        pq = ptm.s(0)
                        for k in range(NK):
                            b.matmul(pq, xl[k], Wb[:, k, 0:512], start=(k == 0), stop=(k == NK - 1))
                        pk = ptm.s(1)
                        for k in range(NK):
                            b.matmul(pk[:, 0:256], xl[k], Wb[:, k, 512:768], start=(k == 0), stop=(k == NK - 1))
                        qr5 = qr[:].re("p (g k i two) -> p k g i two", g=4, k=2, i=32, two=2)
                        norm_rope(pq, (2, 4), qg, i, qr5[:, :, :, :, 0], qr5[:, :, :, :, 1])
                        for g4 in range(4):
                            b.transpose(ptq[:, g4, :], qr[:, g4 * 128:(g4 + 1) * 128], ident_b[:])
                        b.copy(qT[:, :, tk], ptq[:, 0:4, :], eng="act")
                        kr4 = kr[:].re("p (h i two) -> p h i two", i=32, two=2)
                        norm_rope(pk[:, 0:128], (2,), kg, i, kr4[:, :, :, 0], kr4[:, :, :, 1])
                        b.transpose(ptq[:, 4, :], kr[:], ident_b[:])
                        b.copy(kT[:, tk], ptq[:, 4, :], eng="act")
                        b.copy(Vaug[:, i, :, 0:64], pk[:, 128:256].re("p (h d) -> p h d", d=64), eng="act")
            b.mark('attproj')
            for blk in range(NB * SUB):
                b.load(xs_d[blk * 128:(blk + 1) * 128, :], zeros_b[:], q="sp")
            with ExitStack() as stx:
                b.stack = stx
                stx.callback(b.barrier)
                ps = b.psum("ps_att", [128, 4, 512], F32, nslots=4)
                pov = b.psum("po_att", [128, 2, 512], F32, nslots=2)
                ptk = b.psum("ptk_att", [128, 8, 128], BF16)
                Eb = b.sbuf("Eb", [128, 4, 512], BF16, nslots=4)
                otm = b.sbuf("otm", [128, 4, 512], F32)
                rl = b.sbuf("rl", [128, 2, 4], F32, nslots=2)
                st8 = b.sbuf("st8", [128, 4], F32)
                jk = b.sbuf("jk_att", [128, 512], BF16)
                on = b.sbuf("on_att", [128, 512], F32)
                ob = b.sbuf("ob_att", [128, 512], BF16)
                steps = []
                for qc in range(NG):
                    for g4 in range(4):
                        for s_ in range(NT):
                            steps.append((qc, g4, s_))

                def emit_s(idx):
                    qc, g4, s_ = steps[idx]
                    ees = []
                    pss2 = []
                    for kv in range(2):
                        pr = slice(kv * 64, (kv + 1) * 64)
                        pss = ps.s(kv * 2 + idx % 2)
                        b.matmul(pss, kT[pr, s_ * 128:(s_ + 1) * 128], qT[pr, g4, qc * 512:(qc + 1) * 512])
                        pss2.append(pss)
                    for kv in range(2):
                        ee = Eb.s((idx % 2) * 2 + kv)
                        b.act(ee, pss2[kv], AF.Exp)
                        ees.append(ee)
                    return ees

                def emit_pv(idx, ees):
                  qc, g4, s_ = steps[idx]
                  for kv in range(2):
                    ee = ees[kv]
                    h = kv * 4 + g4
                    po_ = pov.s(kv)
                    po3 = po_[:, 0:260].re("p (j e) -> p j e", e=65)
                    for j in range(4):
                        b.matmul(po3[:, j, :], ee[:, j * 128:(j + 1) * 128], Vaug[:, s_, kv, :],
                                 start=(s_ == 0 and j == 0), stop=(s_ == NT - 1 and j == 3))
                    if s_ == NT - 1:
                        rr = rl.s(kv)
                        b.recip(rr.re("p (j o) -> p j o", o=1), po3[:, :, 64:65])
                        b.tt(otm[:].re("p j (h d) -> p j h d", d=64)[:, :, h, :], po3[:, :, 0:64],
                             rr.re("p (j o) -> p j o", o=1).bc([128, 4, 64]), ALU.mult)
                        if g4 == 3 and kv == 1:
                            for j in range(4):
                                i = qc * 4 + j
                                tk = slice(i * 128, (i + 1) * 128)
                                b.act(jk[:], otm[:, j, :], AF.Square, accum_out=st8[:, 0:1])
                                b.act(st8[:, 1:2], st8[:, 0:1], AF.Ln, bias=EPS, scale=1.0 / 512)
                                b.act(st8[:, 2:3], st8[:, 1:2], AF.Exp, scale=-0.5)
                                b.ts(on[:], otm[:, j, :], st8[:, 2:3], None, op0=ALU.mult)
                                b.tt(ob[:], on[:], agr[:], ALU.mult)
                                for c in range(4):
                                    b.transpose(ptk[:, c, :], ob[:, c * 128:(c + 1) * 128], ident_b[:])
                                b.copy(mixa[:, :, tk], ptk[:, 0:4, :])

                pend = None
                for idx in range(len(steps)):
                    ee = emit_s(idx)
                    if pend is not None:
                        emit_pv(*pend)
                    pend = (idx, ee)
                emit_pv(*pend)
            with ExitStack() as std:
                b.stack = std
                std.callback(b.barrier)
                Wo = b.sbuf("Wo", [128, NK, D], BF16)
                wst = b.sbuf("wst_o", [128, 2, 512], F32, nslots=2)
                for k in range(NK):
                    for hf in range(2):
                        b.load(wst.s(hf), w_out[k * 128:(k + 1) * 128, hf * 512:(hf + 1) * 512])
                        b.copy(Wo[:, k, hf * 512:(hf + 1) * 512], wst.s(hf), eng=("dve" if hf == 0 else "act"))
                g2r = b.sbuf("g2r", [128, D], F32)
                b.bcast_load(g2r[:], g2)
                Wr = b.sbuf("Wr", [128, NK, 72], F32)
                with nc.allow_non_contiguous_dma(reason="small router weights"):
                    for k in range(NK):
                        b.load(Wr[:, k, 0:8], w_group[k * 128:(k + 1) * 128, :])
                        b.load(Wr[:, k, 8:72], w_expert[k * 128:(k + 1) * 128, :])
                brow = b.sbuf("brow", [1, 72], F32)
                b.load(brow[:, 0:8], b_group.re("(o n) -> o n", o=1)); b.load(brow[:, 8:72], b_expert.re("(o n) -> o n", o=1))
                xn2b = b.sbuf("xn2b", [128, 2, D], BF16, nslots=2)
                mp = b.sbuf("mp", [128, 2, 192], F32, nslots=2)
                xt = b.sbuf("xt_o", [128, 2, D], F32, nslots=2)
                h1 = b.sbuf("h1_o", [128, 2, D], F32, nslots=2)
                xn2_2 = b.sbuf("xn2_o", [128, 2, D], F32, nslots=2)
                xn2T_2 = b.sbuf("xn2T_o", [128, 2, NK, 128], F32, nslots=2)
                jk_1 = b.sbuf("jk_o", [128, D], BF16)
                sm_2 = b.sbuf("sm_o", [128, 2, 32], F32, nslots=2)
                lg_2 = b.sbuf("lg_o", [128, 2, 72], F32, nslots=2)
                m8_2 = b.sbuf("m8_o", [128, 2, 16], F32, nslots=2)
                og_2 = b.sbuf("og_o", [128, 2, 8], F32, nslots=2)
                tmp8_2 = b.sbuf("tmp8_o", [128, 2, 8], F32, nslots=2)
                ge_2 = b.sbuf("ge_o", [128, 2, 8], F32, nslots=2)
                ml_2 = b.sbuf("ml_o", [128, 2, 64], F32, nslots=2)
                Mb_2 = b.sbuf("Mb_o", [128, 2, 64], BF16, nslots=2)
                py4 = b.psum("py_o", [128, 4, 512], F32, nslots=4)
                ptf = b.psum("ptf_o", [128, 2, 512], F32, nslots=2)
                plg = b.psum("plg_o", [128, 512], F32)
                pps = b.psum("pps_o", [128, 512], F32)
                b.load(xt.s(0), x[0:128, :])
                for i in range(NT):
                    tk = slice(i * 128, (i + 1) * 128)
                    s = i % 2
                    xn2 = xn2_2.s(s); xn2T = xn2T_2.s(s); jk = jk_1[:]; sm = sm_2.s(s); lg = lg_2.s(s); m8 = m8_2.s(s)
                    og = og_2.s(s); tmp8 = tmp8_2.s(s); ge = ge_2.s(s); ml = ml_2.s(s); Mb = Mb_2.s(s)
                    if i + 1 < NT:
                        b.load(xt.s((i + 1) % 2), x[(i + 1) * 128:(i + 2) * 128, :])
                    for hf in range(2):
                        for c in range(NK):
                            b.matmul(py4.s(2 * s + hf), (mixg if c < 4 else mixa)[:, c % 4, tk], Wo[:, c, hf * 512:(hf + 1) * 512], start=(c == 0), stop=(c == NK - 1))
                        b.tt(h1.s(s)[:, hf * 512:(hf + 1) * 512], py4.s(2 * s + hf), xt.s(s)[:, hf * 512:(hf + 1) * 512], ALU.add)
                    b.load(h1_d[tk, :], h1.s(s))
                    b.act(jk, h1.s(s), AF.Square, accum_out=sm[:, 0:1])
                    b.act(sm[:, 1:2], sm[:, 0:1], AF.Ln, bias=EPS, scale=1.0 / D)
                    b.act(sm[:, 2:3], sm[:, 1:2], AF.Exp, scale=-0.5)
                    b.stt(xn2, h1.s(s), sm[:, 2:3], g2r[:], ALU.mult, ALU.mult)
                    b.copy(xn2b.s(s).re("t (c p) -> t c p", c=NK), xn2.re("t (p c) -> t c p", c=NK), eng="act")
                    b.load(xn2_d[tk, :], xn2b.s(s))
                    for c in range(NK):
                        b.transpose(ptf.s(c // 4)[:, (c % 4) * 128:(c % 4 + 1) * 128], xn2[:, c * 128:(c + 1) * 128], ident_f[:])
                    b.copy(xn2T[:, 0:4, :].re("p c t -> p (c t)"), ptf.s(0), eng="act")
                    b.copy(xn2T[:, 4:8, :].re("p c t -> p (c t)"), ptf.s(1))
                    for c in range(NK):
                        b.matmul(plg[:, 0:72], xn2T[:, c, :], Wr[:, c, :], start=(c == 0), stop=False)
                    b.matmul(plg[:, 0:72], ones_f[0:1, :], brow[:], start=False, stop=True)
                    b.copy(lg, plg[:, 0:72])
                    b.max8(m8[:, 0:8], lg[:, 0:8])
                    b.ts(og, lg[:, 0:8], m8[:, 0:1], None, op0=ALU.is_equal)
                    b.ts(sm[:, 3:4], m8[:, 0:1], -1.0, None, op0=ALU.mult)
                    b.act(ge, lg[:, 0:8], AF.Exp, bias=sm[:, 3:4], scale=1.0, accum_out=sm[:, 4:5])
                    b.recip(sm[:, 5:6], sm[:, 4:5])
                    b.ts(tmp8, og, BIG, -BIG, op0=ALU.mult, op1=ALU.add)
                    b.tt(ml.re("p (g j) -> p g j", j=8), lg[:, 8:72].re("p (g j) -> p g j", j=8),
                         tmp8.re("p (g o) -> p g o", o=1).bc([128, 8, 8]), ALU.add)
                    b.max8(m8[:, 8:16], ml)
                    b.ts(mp.s(s)[:, 0:64], ml, m8[:, 8:9], None, op0=ALU.is_equal)
                    b.ts(mp.s(s)[:, 64:128], ml, m8[:, 9:10], None, op0=ALU.is_equal)
                    b.tt(sm[:, 6:7], m8[:, 9:10], m8[:, 8:9], ALU.subtract)
                    b.act(sm[:, 7:8], sm[:, 6:7], AF.Exp)
                    b.ts(sm[:, 8:9], sm[:, 7:8], 1.0, None, op0=ALU.add)
                    b.recip(sm[:, 9:10], sm[:, 8:9])
                    b.tt(sm[:, 10:11], sm[:, 7:8], sm[:, 9:10], ALU.mult)
                    b.tt(gates[:, i, 0:1], sm[:, 9:10], sm[:, 5:6], ALU.mult)
                    b.tt(gates[:, i, 1:2], sm[:, 10:11], sm[:, 5:6], ALU.mult)
                    b.tt(Mb, mp.s(s)[:, 0:64], mp.s(s)[:, 64:128], ALU.add)
                    b.matmul(pps[:, 0:64], lstrict_b[:], Mb)
                    b.matmul(pps[:, 64:128], ones_b[:], Mb)
                    b.tt(mp.s(s)[:, 128:192], pps[:, 0:64], run[:], ALU.add)
                    b.tt(run[:], pps[:, 64:128], run[:], ALU.add)
                    b.load(mp_d[:, i * 192:(i + 1) * 192], mp.s(s))
        b.mark('router')
        stS1.__exit__(None, None, None)
        b.barrier()
        b.stack = st0
        with ExitStack() as stl:
            b.stack = stl
            stl.callback(b.barrier)
            mpa = b.sbuf("mpa", [128, NT, 192], F32)
            b.load(mpa[:].re("p n c -> p (n c)"), mp_d)
            cmpA = b.sbuf("cmpA", [128, 64, NBMAX], BF16)
            nblk = b.sbuf("nblk", [128, 64], F32)
            pend = b.sbuf("pend", [128, 64], F32)
            pstart = b.sbuf("pstart", [128, 64], F32)
            ones64 = b.sbuf("ones64", [128, 64], F32)
            b.memset(ones64[:], 1.0)
            b.tt(cmpA[:], run[:].re("p (e o) -> p e o", o=1).bc([128, 64, NBMAX]),
                 thr[:, 0:NBMAX].re("p (o n) -> p o n", o=1).bc([128, 64, NBMAX]), ALU.is_gt)
            b.reduce(nblk[:], cmpA[:], ALU.add)
            b.ts(nblk[:], nblk[:], float(RB), None, op0=ALU.mult)
            b.scan(pend[:], ones64[:], nblk[:], 0.0, ALU.mult, ALU.add)
            b.tt(pstart[:], pend[:], nblk[:], ALU.subtract)
            cmpB = b.sbuf("cmpB", [128, NB, 64], BF16)
            bef = b.sbuf("bef", [128, NB], F32)
            b.tt(cmpB[:], pend[:].re("p (o e) -> p o e", o=1).bc([128, NB, 64]),
                 thr[:].re("p (n o) -> p n o", o=1).bc([128, NB, 64]), ALU.is_le)
            b.reduce(bef[:], cmpB[:], ALU.add)
            if debug:
                b.load(dbg_be, bef[:])
            b.ts(bef[:], bef[:], 128.0, pidx[:, 0:1], op0=ALU.mult, op1=ALU.add)
            b.copy(idxw_i[:], bef[:])
            destf = b.sbuf("destf", [128, NT, 2], F32)
            tq = b.sbuf("tq", [128, 64], F32); tq2 = b.sbuf("tq2", [128, 64], F32)
            for i in range(NT):
                b.tt(tq[:], mpa[:, i, 128:192], pstart[:], ALU.add)
                for k2 in range(2):
                    b.tt(tq2[:], tq[:], mpa[:, i, k2 * 64:(k2 + 1) * 64], ALU.mult)
                    b.reduce(destf[:, i, k2:k2 + 1], tq2[:], ALU.add)
            b.copy(dest_i[:], destf[:])
            if debug:
                dr = b.sbuf("dr", [128, NT, 4], F32)
                b.copy(dr[:, :, 0:2], destf[:]); b.copy(dr[:, :, 2:4], gates[:])
                b.load(dbg_route, dr[:])
            b.mark('layout')
            xr = b.sbuf("xr", [128, 2, D], BF16, nslots=2)
            b.load(xr.s(0), xn2_d[0:128, :])
            for i in range(NT):
                if i + 1 < NT:
                    b.load(xr.s((i + 1) % 2), xn2_d[(i + 1) * 128:(i + 2) * 128, :])
                for k2 in range(2):
                    b.scatter(xs_d, xr.s(i % 2), dest_i[:, i, k2:k2 + 1])
        b.stack = st0

        b.mark('scatter')
        with ExitStack() as stm:
            b.stack = stm
            stm.callback(b.barrier)
            wstg = b.sbuf("wstg", [128, 4, 4096], F32, nslots=4)
            wgb = b.sbuf("wgb", [128, 2, NK, 512], BF16, nslots=2)
            wub = b.sbuf("wub", [128, 2, NK, 512], BF16, nslots=2)
            wdb = b.sbuf("wdb", [128, 2, 4, D], BF16, nslots=2)
            xsb = b.sbuf("xsb", [128, 2, D], BF16, nslots=2)
            xsT = b.sbuf("xsT", [128, 2, NK, 128], BF16, nslots=2)
            hh = b.sbuf("hh", [128, 2, 512], BF16, nslots=2)
            hT = b.sbuf("hT", [128, 4, 128], BF16)
            et = b.sbuf("et", [128, 2, 3, 512], F32, nslots=2)
            ysb = b.sbuf("ysb", [128, 2, D], F32, nslots=2)
            ptx = b.psum("ptx", [128, NK, 128], BF16)
            pth = b.psum("pth", [128, NK, 128], BF16)
            pg2 = b.psum("pg_m", [128, 2, 512], F32, nslots=2)
            pu2 = b.psum("pu_m", [128, 2, 512], F32, nslots=2)
            pyy = b.psum("pyy", [128, 2, 512], F32, nslots=2)
            wg_v = w_gate.re("e (p c) f -> (e p) (c f)", c=NK)
            wu_v = w_up.re("e (p c) f -> (e p) (c f)", c=NK)
            wd_v = w_down.re("e (p c) d -> (e p) (c d)", c=4)
            sc = [0]

            order = []
            lo, hi = 0, NB - 1
            while lo <= hi:
                order.append(lo); lo += 1
                if lo <= hi:
                    order.append(hi); hi -= 1
            seq = [(slot, sub) for slot in order for sub in range(SUB)]
            NSEQ = len(seq)

            conv_q = {"gu": [], "d": []}

            def fetch_w(pos):
                slot = order[pos]
                ws = pos % 2
                for (wv, dstb, kind) in ((wg_v, wgb, "g"), (wu_v, wub, "u"), (wd_v, wdb, "d")):
                    sl = wstg.s(sc[0] % 4); sc[0] += 1
                    b.gather(sl, wv, idxw_i[:, slot:slot + 1], bounds=64 * 128 - 1)
                    dv = dstb.s(ws).re("p a f -> p (a f)")
                    for pc in range(4):
                        cs = slice(pc * 1024, (pc + 1) * 1024)
                        if kind == "g":
                            eng = "act"
                        elif kind == "u":
                            eng = "dve"
                        else:
                            eng = "act" if pc < 2 else "dve"
                        conv_q["d" if kind == "d" else "gu"].append((dv[:, cs], sl[:, cs], eng))

            def flush_conv(which):
                lst = conv_q[which]
                for (dv_, sl_, eng_) in lst:
                    b.copy(dv_, sl_, eng=eng_)
                conv_q[which] = []

            def blk_of(q):
                slot, sub = seq[q]
                return slot * SUB + sub

            def fetch_x(q):
                blk = blk_of(q)
                b.load(xsb.s(q % 2), xs_d[blk * 128:(blk + 1) * 128, :])

            def stage1(q):
                ws = (q // SUB) % 2
                p2 = q % 2
                if q + 1 < NSEQ:
                    fetch_x(q + 1)
                pg = pg2.s(p2); pu = pu2.s(p2); et_ = et.s(p2); xT = xsT.s(p2)
                for c in range(NK):
                    b.transpose(ptx[:, c, :], xsb.s(p2)[:, c * 128:(c + 1) * 128], ident_b[:])
                b.copy(xT, ptx[:], eng="act")
                for c in range(NK):
                    b.matmul(pg, xT[:, c, :], wgb.s(ws)[:, c, :], start=(c == 0), stop=(c == NK - 1))
                for c in range(NK):
                    b.matmul(pu, xT[:, c, :], wub.s(ws)[:, c, :], start=(c == 0), stop=(c == NK - 1))
                b.act(et_[:, 2, :], pg, AF.Silu)
                b.tt(hh.s(p2).re("s (c p) -> s c p", c=4), et_[:, 2, :].re("s (p c) -> s c p", c=4), pu.re("s (p c) -> s c p", c=4), ALU.mult)

            def stage2(q):
                blk = blk_of(q)
                ws = (q // SUB) % 2
                p2 = q % 2
                for c in range(4):
                    b.transpose(pth[:, c, :], hh.s(p2)[:, c * 128:(c + 1) * 128], ident_b[:])
                b.copy(hT[:], pth[:, 0:4, :], eng="act")
                for hf in range(2):
                    for c in range(4):
                        b.matmul(pyy.s(hf), hT[:, c, :], wdb.s(ws)[:, c, hf * 512:(hf + 1) * 512], start=(c == 0), stop=(c == 3))
                b.copy(ysb.s(p2)[:, 0:512], pyy.s(0), eng="act")
                b.copy(ysb.s(p2)[:, 512:1024], pyy.s(1))
                b.load(ys_d[blk * 128:(blk + 1) * 128, :], ysb.s(p2))

            fetch_w(0)
            flush_conv("gu"); flush_conv("d")
            fetch_x(0)
            stage1(0)
            for q in range(NSEQ):
                first = (q % SUB == 0)
                if first and q // SUB + 1 < NB:
                    fetch_w(q // SUB + 1)
                if q + 1 < NSEQ:
                    if (q + 1) % SUB == 0:
                        flush_conv("gu")
                    stage1(q + 1)
                if first:
                    flush_conv("gu")
                else:
                    flush_conv("d")
                stage2(q)
            flush_conv("gu"); flush_conv("d")
        b.stack = st0

        b.mark('moe')
        with ExitStack() as stf:
            b.stack = stf
            fgr = b.sbuf("fgr", [128, D], F32)
            b.bcast_load(fgr[:], fgain)
            y1 = b.sbuf("y1", [128, 2, D], F32, nslots=2); y2 = b.sbuf("y2", [128, 2, D], F32, nslots=2)
            hh1 = b.sbuf("hh1", [128, 2, D], F32, nslots=2)
            acc = b.sbuf("acc", [128, D], F32); acc2 = b.sbuf("acc2", [128, D], F32)
            ot = b.sbuf("ot", [128, 2, D], F32, nslots=2)
            jk = b.sbuf("jk_f", [128, D], BF16)
            sf = b.sbuf("sf", [128, 4], F32)
            outs = []

            def fetch_f(i):
                s = i % 2
                b.gather(y1.s(s), ys_d, dest_i[:, i, 0:1])
                b.gather(y2.s(s), ys_d, dest_i[:, i, 1:2])
                b.load(hh1.s(s), h1_d[i * 128:(i + 1) * 128, :])

            fetch_f(0)
            for i in range(NT):
                s = i % 2
                if i + 1 < NT:
                    fetch_f(i + 1)
                b.stt(acc[:], y1.s(s), gates[:, i, 0:1], hh1.s(s), ALU.mult, ALU.add)
                b.stt(acc2[:], y2.s(s), gates[:, i, 1:2], acc[:], ALU.mult, ALU.add)
                b.act(jk[:], acc2[:], AF.Square, accum_out=sf[:, 0:1])
                b.act(sf[:, 1:2], sf[:, 0:1], AF.Ln, bias=EPS, scale=1.0 / D)
                b.act(sf[:, 2:3], sf[:, 1:2], AF.Exp, scale=-0.5)
                b.stt(ot.s(s), acc2[:], sf[:, 2:3], fgr[:], ALU.mult, ALU.mult)
                outs.append(b.load(out[i * 128:(i + 1) * 128, :], ot.s(s)))
            b.wait_all("sp", outs)

        def tail(bb):
            bb.waited = {e: {} for e in bb.prog}
            bb.latest = {}
            bb.lastw = {}
            bb.readers = {}
            for e, lst in bb.prog.items():
                for idx, it in enumerate(lst):
                    if it[0] == "dma":
                        bb.latest[it[3]] = max(bb.latest.get(it[3], 0), 0)
            toks = []
            cnt = {}
            for e, lst in bb.prog.items():
                for idx, it in enumerate(lst):
                    if it[0] == "dma":
                        cnt[it[3]] = cnt.get(it[3], 0) + 16
                        bb.latest[it[3]] = cnt[it[3]]
                    elif it[2] is not None and e in COMPUTE:
                        bb.latest[("e", e)] = idx
            for q in bb.dma_cnt:
                for i in range(bb.n_dma):
                    bb.dma_cnt[q][i] = cnt.get(("d", q, i), 0) // 16
            bb.barrier()
            for i in range(NT):
                toks.append(bb.load(out[i * 128:(i + 1) * 128, :], zeros_f[:]))
            bb.wait_all("sp", toks)
        b.emit(trunc=trunc, tail=tail)
    return nc


GRID_W = 64
ROPE_THETA = 10000.0
_NC_CACHE = {}


def _consts(T):
    NT = T // 128
    RB = 256
    NB = 2 * T // RB + 63
    s = np.arange(128)[:, None]
    c = np.arange(128)[None, :]
    maskU = (s <= c).astype(np.float32)
    maskL = (s >= c).astype(np.float32)
    t = np.arange(T)
    row = (t // GRID_W).astype(np.float32)
    col = (t % GRID_W).astype(np.float32)
    axis_dim = 32
    inv_freq = (ROPE_THETA ** (-np.arange(0, axis_dim, 2, dtype=np.float32) / axis_dim)).astype(np.float32)
    ang = np.concatenate([row[:, None] * inv_freq, col[:, None] * inv_freq], axis=-1).astype(np.float32)
    cos = np.cos(ang).astype(np.float32).reshape(NT, 128, 32).transpose(1, 0, 2)
    sin = np.sin(ang).astype(np.float32).reshape(NT, 128, 32).transpose(1, 0, 2)
    return dict(
        c_ident=np.eye(128, dtype=np.float32),
        c_mask4=np.ascontiguousarray(np.concatenate([maskU, maskU, maskL, maskL], axis=1)),
        c_lstrict=(s < c).astype(np.float32),
        c_cos=np.ascontiguousarray(cos), c_sin=np.ascontiguousarray(sin),
        c_pidx=np.arange(128, dtype=np.float32).reshape(128, 1),
        c_thr=np.ascontiguousarray(np.broadcast_to((float(RB) * np.arange(NB, dtype=np.float32))[None, :], (128, NB))),
        c_bd=np.ascontiguousarray(((np.arange(128)[:, None] // 64) == (np.arange(256)[None, :] // 128)).astype(np.float32)),
    )


def _shared_inputs(T, norm1_gain, w_in, gla_up_fwd, gla_up_fwd_bias, gla_up_bwd, gla_up_bwd_bias, gla_out_gain,
                   q_norm_gain, k_norm_gain, att_out_gain, w_out, norm2_gain, w_group, b_group, w_expert, b_expert,
                   w_gate, w_up, w_down, final_gain):
    f = lambda a: np.ascontiguousarray(np.asarray(a, dtype=np.float32))
    d = dict(
        w_in=f(w_in[0]), g1pk=f(np.asarray(norm1_gain[0]).reshape(8, 128).T),
        up_f=f(gla_up_fwd[0]), up_b=f(gla_up_bwd[0]),
        bias_f=f(np.asarray(gla_up_fwd_bias[0]).reshape(2, 128).T), bias_b=f(np.asarray(gla_up_bwd_bias[0]).reshape(2, 128).T),
        gla_gain=f(gla_out_gain[0]), q_gain=f(q_norm_gain[0]), k_gain=f(k_norm_gain[0]), att_gain=f(att_out_gain[0]),
        w_out=f(w_out[0]), g2=f(norm2_gain[0]), w_group=f(w_group[0]), b_group=f(b_group[0]),
        w_expert=f(w_expert[0]), b_expert=f(b_expert[0]),
        w_gate=f(w_gate[0]), w_up=f(w_up[0]), w_down=f(w_down[0]), fgain=f(final_gain),
    )
    d.update(_consts(T))
    return d


def kernel(x, **params):
    x = np.asarray(x, dtype=np.float32)
    B, T, _ = x.shape
    if T not in _NC_CACHE:
        _NC_CACHE[T] = build(T)
    nc = _NC_CACHE[T]
    shared = _shared_inputs(T, **params)
    in_maps = []
    for bi in range(B):
        m = dict(shared)
        m["x"] = np.ascontiguousarray(x[bi])
        in_maps.append(m)
    res = run_bass_kernel_spmd(nc, in_maps, core_ids=list(range(B)))
    return np.stack([np.asarray(r["out"], dtype=np.float32) for r in res.results], axis=0)
```

```python
import numpy as np
from contextlib import ExitStack
import concourse.bass as bass
import concourse.mybir as mybir
from concourse.bass_utils import run_bass_kernel_spmd

F32 = mybir.dt.float32
BF16 = mybir.dt.bfloat16
I32 = mybir.dt.int32
AF = mybir.ActivationFunctionType
ALU = mybir.AluOpType
AX = mybir.AxisListType

SAME_ENGINE_SYNC = True
COMPUTE = ("pe", "act", "dve", "pool")


class V:
    __slots__ = ("ap", "keys")

    def __init__(self, ap, keys):
        self.ap = ap
        self.keys = tuple(keys)

    def __getitem__(self, idx):
        return V(self.ap[idx], self.keys)

    def re(self, s, **kw):
        return V(self.ap.rearrange(s, **kw), self.keys)

    def bc(self, shape):
        return V(self.ap.to_broadcast(list(shape)), self.keys)

    def bitcast(self, dt):
        return V(self.ap.bitcast(dt), self.keys)


class Tile:
    def __init__(self, b, name, handle, nslots):
        self.b = b
        self.name = name
        self.h = handle
        self.nslots = nslots

    def all(self):
        if self.nslots:
            return V(self.h[:], [(self.name, i) for i in range(self.nslots)])
        return V(self.h[:], [(self.name, None)])

    def s(self, i):
        assert self.nslots and 0 <= i < self.nslots
        return V(self.h[:, i], [(self.name, i)])

    def __getitem__(self, idx):
        return self.all()[idx]


class Builder:
    def __init__(self, nc, n_dma_sems=24):
        self.nc = nc
        self.prog = {e: [] for e in ("pe", "act", "dve", "pool", "sp")}
        self.waited = {e: {} for e in self.prog}
        self.lastw = {}
        self.readers = {}
        self.n_dma = n_dma_sems
        self.dma_cnt = {"sp": [0] * n_dma_sems, "pool": [0] * n_dma_sems, "act": [0] * n_dma_sems}
        self.dma_rr = {"sp": 0, "pool": 0, "act": 0}
        self.stack = None
        self.uid = 0
        self.out_tokens = []
        self.latest = {}
        self.bounds_reg = None
        self.marks = {}

    def sbuf(self, name, shape, dtype, nslots=0):
        h = self.stack.enter_context(self.nc.sbuf_tensor(name, list(shape), dtype))
        return Tile(self, name, h, nslots)

    def psum(self, name, shape, dtype, nslots=0):
        h = self.stack.enter_context(self.nc.psum_tensor(name, list(shape), dtype))
        return Tile(self, name, h, nslots)

    def dram(self, name, shape, dtype, kind="Internal"):
        t = self.nc.dram_tensor(name, list(shape), dtype, kind=kind)
        return V(t.ap(), [(name, None)])

    def _deps(self, eng, reads, writes, skip_self):
        toks = []
        for v in reads:
            for k in v.keys:
                toks += list(self.lastw.get(k, {}).items())
        for v in writes:
            for k in v.keys:
                toks += list(self.lastw.get(k, {}).items())
                toks += list(self.readers.get(k, {}).items())
        need = {}
        for src, val in toks:
            if src == ("e", eng) and (skip_self or not SAME_ENGINE_SYNC or eng == "pe"):
                continue
            if self.waited[eng].get(src, -1) >= val:
                continue
            if need.get(src, -1) < val:
                need[src] = val
        for src, val in need.items():
            self.waited[eng][src] = val
        return list(need.items())

    def _commit(self, tok, reads, writes, partial):
        src, val = tok
        if self.latest.get(src, -1) < val:
            self.latest[src] = val
        for v in reads:
            for k in v.keys:
                d = self.readers.setdefault(k, {})
                if d.get(src, -1) < val:
                    d[src] = val
        for v in writes:
            for k in v.keys:
                d = self.lastw.setdefault(k, {})
                if d.get(src, -1) < val:
                    d[src] = val

    def op(self, eng, fn, reads=(), writes=(), skip_self=False, partial=False):
        reads = [r for r in reads if isinstance(r, V)]
        writes = [w for w in writes if isinstance(w, V)]
        waits = self._deps(eng, reads, writes, skip_self)
        idx = len(self.prog[eng])
        tok = (("e", eng), idx)
        self.prog[eng].append(["op", waits, fn, None])
        self._commit(tok, reads, writes, partial)
        return tok

    def dma(self, q, fn, reads=(), writes=(), partial=False):
        reads = [r for r in reads if isinstance(r, V)]
        writes = [w for w in writes if isinstance(w, V)]
        i = self.dma_rr[q]
        self.dma_rr[q] = (i + 1) % self.n_dma
        src = ("d", q, i)
        waits = self._deps(q, reads, writes, False)
        prev = self.dma_cnt[q][i]
        if prev > 0 and self.waited[q].get(src, -1) < prev * 16:
            waits.append((src, prev * 16))
            self.waited[q][src] = prev * 16
        self.dma_cnt[q][i] += 1
        tok = (src, self.dma_cnt[q][i] * 16)
        self.prog[q].append(["dma", waits, fn, src])
        self._commit(tok, reads, writes, partial)
        return tok

    def mark(self, name):
        if name in self.marks:
            return
        self.barrier()
        self.marks[name] = {e: len(v) for e, v in self.prog.items()}

    def barrier(self):
        for eng in self.prog:
            need = {}
            for src, val in self.latest.items():
                if src == ("e", eng) and (eng == "pe" or not SAME_ENGINE_SYNC):
                    continue
                if self.waited[eng].get(src, -1) >= val:
                    continue
                need[src] = val
            for src, val in need.items():
                self.waited[eng][src] = val
            if need:
                self.prog[eng].append(["op", list(need.items()), None, None])

    def wait_all(self, eng, toks):
        need = {}
        for src, val in toks:
            if self.waited[eng].get(src, -1) >= val:
                continue
            if need.get(src, -1) < val:
                need[src] = val
        self.prog[eng].append(["op", list(need.items()), None, None])

    def emit(self, trunc=None, tail=None):
        nc = self.nc
        if trunc is not None:
            self.prog = {e: v[:self.marks[trunc][e]] for e, v in self.prog.items()}
            tail(self)
        signal = {e: set() for e in COMPUTE}
        for e, lst in self.prog.items():
            for item in lst:
                for src, val in item[1]:
                    if src[0] == "e":
                        signal[src[1]].add(val)
        rank = {}
        for e in COMPUTE:
            r = {}
            c = 0
            for idx in sorted(signal[e]):
                c += 1
                r[idx] = c
            rank[e] = r
        from contextlib import ExitStack
        with ExitStack() as st:
            esem = {e: st.enter_context(nc.semaphore("sem_" + e)) for e in COMPUTE}
            dsem = {}
            for q in ("sp", "pool", "act"):
                for i in range(self.n_dma):
                    if self.dma_cnt[q][i] > 0:
                        dsem[("d", q, i)] = st.enter_context(nc.semaphore("dsem_%s_%d" % (q, i)))
            block = st.enter_context(nc.Block())

            def run(ename, eng):
                for idx, (kind, waits, fn, dsrc) in enumerate(self.prog[ename]):
                    for src, val in waits:
                        if src[0] == "e":
                            eng.wait_ge(esem[src[1]], rank[src[1]][val])
                        else:
                            eng.wait_ge(dsem[src], val)
                    if fn is None:
                        continue
                    ins = fn(eng)
                    if kind == "dma":
                        ins.then_inc(dsem[dsrc], 16)
                    elif ename in COMPUTE and idx in rank[ename]:
                        ins.then_inc(esem[ename], 1)

            @block.tensor
            def _(t):
                run("pe", t)

            @block.scalar
            def _(a):
                run("act", a)

            @block.vector
            def _(v):
                run("dve", v)

            @block.gpsimd
            def _(g):
                run("pool", g)

            @block.sync
            def _(s):
                run("sp", s)

    def matmul(self, out, lhsT, rhs, start=True, stop=True):
        return self.op("pe", lambda e: e.matmul(out.ap, lhsT=lhsT.ap, rhs=rhs.ap, start=start, stop=stop),
                       reads=[lhsT, rhs], writes=[out], partial=not start)

    def transpose(self, out, in_, ident):
        return self.op("pe", lambda e: e.transpose(out=out.ap, in_=in_.ap, identity=ident.ap),
                       reads=[in_, ident], writes=[out], partial=True)

    def act(self, out, in_, func, bias=0.0, scale=1.0, accum_out=None, eng="act"):
        ba = bias.ap if isinstance(bias, V) else bias
        sa = scale.ap if isinstance(scale, V) else scale
        kw = {}
        if accum_out is not None:
            kw["accum_out"] = accum_out.ap
        return self.op("act", lambda e: e.activation(out=out.ap, in_=in_.ap, func=func, bias=ba, scale=sa, **kw),
                       reads=[in_, bias, scale], writes=[out] + ([accum_out] if accum_out is not None else []))

    def ts(self, out, in0, s1, s2=None, op0=ALU.mult, op1=None, eng="dve", accum_out=None):
        a1 = s1.ap if isinstance(s1, V) else s1
        a2 = s2.ap if isinstance(s2, V) else s2
        kw = {}
        if op1 is not None:
            kw["op1"] = op1
        if accum_out is not None:
            kw["accum_out"] = accum_out.ap
        return self.op(eng, lambda e: e.tensor_scalar(out=out.ap, in0=in0.ap, scalar1=a1, scalar2=a2, op0=op0, **kw),
                       reads=[in0, s1, s2], writes=[out] + ([accum_out] if accum_out is not None else []))

    def tt(self, out, in0, in1, op, eng="dve"):
        return self.op(eng, lambda e: e.tensor_tensor(out=out.ap, in0=in0.ap, in1=in1.ap, op=op),
                       reads=[in0, in1], writes=[out])

    def stt(self, out, in0, scalar, in1, op0, op1):
        sa = scalar.ap if isinstance(scalar, V) else scalar
        return self.op("dve", lambda e: e.scalar_tensor_tensor(out=out.ap, in0=in0.ap, scalar=sa, in1=in1.ap, op0=op0, op1=op1),
                       reads=[in0, scalar, in1], writes=[out])

    def copy(self, out, in_, eng="dve"):
        if eng == "act":
            return self.op("act", lambda e: e.copy(out=out.ap, in_=in_.ap), reads=[in_], writes=[out])
        return self.op(eng, lambda e: e.tensor_copy(out=out.ap, in_=in_.ap), reads=[in_], writes=[out])

    def reduce(self, out, in_, op, axis=AX.X):
        return self.op("dve", lambda e: e.tensor_reduce(out=out.ap, in_=in_.ap, axis=axis, op=op),
                       reads=[in_], writes=[out])

    def recip(self, out, in_):
        return self.op("dve", lambda e: e.reciprocal(out=out.ap, in_=in_.ap), reads=[in_], writes=[out])

    def max8(self, out, in_):
        return self.op("dve", lambda e: e.max(out=out.ap, in_=in_.ap), reads=[in_], writes=[out])

    def scan(self, out, d0, d1, initial, op0, op1):
        ia = initial.ap if isinstance(initial, V) else initial
        return self.op("dve", lambda e: e.tensor_tensor_scan(out=out.ap, data0=d0.ap, data1=d1.ap, initial=ia, op0=op0, op1=op1),
                       reads=[d0, d1, initial], writes=[out])

    def memset(self, out, val, eng="dve"):
        return self.op(eng, lambda e: e.memset(out.ap, val), writes=[out])

    def iota(self, out, pattern, base=0, channel_multiplier=0, allow=False):
        return self.op("pool", lambda e: e.iota(out.ap, pattern=pattern, base=base, channel_multiplier=channel_multiplier,
                                                allow_small_or_imprecise_dtypes=allow), writes=[out])

    def load(self, out, in_, q="sp", partial=False):
        return self.dma(q, lambda e: e.dma_start(out=out.ap, in_=in_.ap), reads=[in_], writes=[out], partial=partial)

    def bcast_load(self, out, vec):
        return self.dma("sp", lambda e: e.dma_start(out=out.ap, in_=vec.ap.partition_broadcast(128)), reads=[vec], writes=[out])

    def gather(self, out, in_, idx, bounds=None, partial=False):
        def fn(e):
            kw = {}
            if bounds is not None:
                if self.bounds_reg is None:
                    self.bounds_reg = (e.alloc_register("gather_bound"), bounds)
                    e.reg_mov(self.bounds_reg[0], bounds)
                assert self.bounds_reg[1] == bounds
                kw["bounds_check"] = self.bounds_reg[0]
                kw["oob_is_err"] = False
            return e.indirect_dma_start(out=out.ap, out_offset=None, in_=in_.ap,
                                        in_offset=bass.IndirectOffsetOnAxis(ap=idx.ap, axis=0), **kw)
        return self.dma("pool", fn, reads=[in_, idx], writes=[out], partial=partial)

    def scatter(self, out, in_, idx, bounds=None):
        def fn(e):
            kw = {}
            if bounds is not None:
                kw["bounds_check"] = bounds
                kw["oob_is_err"] = False
            return e.indirect_dma_start(out=out.ap, out_offset=bass.IndirectOffsetOnAxis(ap=idx.ap, axis=0),
                                        in_=in_.ap, in_offset=None, **kw)
        return self.dma("pool", fn, reads=[in_, idx], writes=[out], partial=True)


D = 1024
NK = 8
EPS = 1e-6
BIG = 1.0e4


def build(T, debug=False, trunc=None):
    NT = T // 128
    NG = T // 512
    RB = 256
    SUB = RB // 128
    NB = 2 * T // RB + 63
    NBMAX = 2 * T // RB
    nc = bass.Bass("TRN2", target_bir_lowering=False)
    b = Builder(nc)

    def din(name, shape, dt=F32):
        return V(nc.dram_tensor(name, list(shape), dt, kind="ExternalInput").ap(), [(name, None)])

    def dout(name, shape, dt=F32, kind="ExternalOutput"):
        return V(nc.dram_tensor(name, list(shape), dt, kind=kind).ap(), [(name, None)])

    x = din("x", [T, D])
    w_in = din("w_in", [D, 2336])
    g1pk = din("g1pk", [128, 8])
    up_f = din("up_f", [16, 256]); up_b = din("up_b", [16, 256])
    bias_f = din("bias_f", [128, 2]); bias_b = din("bias_b", [128, 2])
    gla_gain = din("gla_gain", [128])
    q_gain = din("q_gain", [64]); k_gain = din("k_gain", [64]); att_gain = din("att_gain", [512])
    w_out = din("w_out", [D, D]); g2 = din("g2", [D])
    w_group = din("w_group", [D, 8]); b_group = din("b_group", [8])
    w_expert = din("w_expert", [D, 64]); b_expert = din("b_expert", [64])
    w_gate = din("w_gate", [64, D, 512]); w_up = din("w_up", [64, D, 512]); w_down = din("w_down", [64, 512, D])
    fgain = din("fgain", [D])
    c_ident = din("c_ident", [128, 128]); c_mask4 = din("c_mask4", [128, 512]); c_lstrict = din("c_lstrict", [128, 128])
    c_cos = din("c_cos", [128, NT, 32]); c_sin = din("c_sin", [128, NT, 32])
    c_pidx = din("c_pidx", [128, 1]); c_thr = din("c_thr", [128, NB]); c_bd = din("c_bd", [128, 256])
    out = dout("out", [T, D])
    dk = "ExternalOutput" if debug else "Internal"
    h1_d = dout("h1_d", [T, D], F32, dk)
    xs_d = dout("xs_d", [NB * RB, D], BF16, dk)
    ys_d = dout("ys_d", [NB * RB, D], F32, dk)
    mp_d = dout("mp_d", [128, NT * 192], F32, "Internal")
    xn2_d = dout("xn2_d", [T, D], BF16, "Internal")
    if debug:
        dbg_mixg = dout("dbg_mixg", [128, 4, T], BF16)
        dbg_mixa = dout("dbg_mixa", [128, 4, T], BF16)
        dbg_route = dout("dbg_route", [128, NT, 4])
        dbg_be = dout("dbg_be", [128, NB])

    with ExitStack() as st0:
        b.stack = st0
        ident_f = b.sbuf("ident_f", [128, 128], F32); ident_b = b.sbuf("ident_b", [128, 128], BF16)
        mask4 = b.sbuf("mask4", [128, 512], F32)
        lstrict_f = b.sbuf("lstrict_f", [128, 128], F32); lstrict_b = b.sbuf("lstrict_b", [128, 128], BF16)
        ones_b = b.sbuf("ones_b", [128, 128], BF16); ones_f = b.sbuf("ones_f", [128, 128], F32)
        zeros_b = b.sbuf("zeros_b", [128, 1024], BF16)
        zeros_f = b.sbuf("zeros_f", [128, 1024], F32)
        b.memset(zeros_f[:], 0.0, eng="pool")
        pidx = b.sbuf("pidx", [128, 1], F32); thr = b.sbuf("thr", [128, NB], F32)
        g1 = b.sbuf("g1", [128, 8], F32)
        b.load(ident_f[:], c_ident); b.load(mask4[:], c_mask4); b.load(lstrict_f[:], c_lstrict)
        bdm = b.sbuf("bdm", [128, 256], F32)
        b.load(bdm[:], c_bd)
        b.load(pidx[:], c_pidx); b.load(thr[:], c_thr); b.load(g1[:], g1pk)
        b.copy(ident_b[:], ident_f[:]); b.copy(lstrict_b[:], lstrict_f[:])
        b.memset(ones_b[:], 1.0); b.memset(ones_f[:], 1.0); b.memset(zeros_b[:], 0.0, eng="pool")
        ones512 = b.sbuf("ones512", [128, 512], F32)
        b.memset(ones512[:], 1.0)
        gates = b.sbuf("gates", [128, NT, 2], F32)
        dest_i = b.sbuf("dest_i", [128, NT, 2], I32)
        idxw_i = b.sbuf("idxw_i", [128, NB], I32)
        run = b.sbuf("run", [128, 64], F32)
        b.memset(run[:], 0.0)
        stS1 = ExitStack(); stS1.__enter__(); b.stack = stS1
        mixg = b.sbuf("mixg", [128, 4, T], BF16)
        STK = stS1

        def xn_group_factory(stk, psum_tr):
            xt = b.sbuf("xt" + stk, [128, 2, D], F32, nslots=2)
            junk = b.sbuf("junk" + stk, [128, D], BF16)
            xnb = b.sbuf("xnb" + stk, [128, 2, D], BF16, nslots=2)
            xnT = b.sbuf("xnT" + stk, [128, 2, NK, 512], BF16, nslots=2)
            st1 = b.sbuf("st1" + stk, [128, 2, 4], F32, nslots=2)

            def load_tile(i):
                b.load(xt.s(i % 2), x[i * 128:(i + 1) * 128, :])

            def group(g):
                gs = g % 2
                for j in range(4):
                    i = g * 4 + j
                    s = i % 2
                    if i == 0:
                        load_tile(0)
                    if i + 1 < NT:
                        load_tile(i + 1)
                    ss = st1.s(s)
                    b.act(junk[:], xt.s(s), AF.Square, accum_out=ss[:, 0:1])
                    b.act(ss[:, 1:2], ss[:, 0:1], AF.Ln, bias=EPS, scale=1.0 / D)
                    b.act(ss[:, 2:3], ss[:, 1:2], AF.Exp, scale=-0.5)
                    b.ts(xnb.s(s), xt.s(s), ss[:, 2:3], None, op0=ALU.mult)
                    ptr_s = psum_tr.s(s)
                    for k in range(NK):
                        b.transpose(ptr_s[:, k, :], xnb.s(s)[:, k * 128:(k + 1) * 128], ident_b[:])
                    b.copy(xnT.s(gs)[:, :, j * 128:(j + 1) * 128], ptr_s, eng="act")
                return xnT.s(gs)
            return group

        def load_w_cols(Wb, stage, cols):
            c0 = 0
            for (a, z) in cols:
                n = z - a
                for k in range(NK):
                    sl = stage.s(k % 2)
                    b.load(sl[:, 0:n], w_in[k * 128:(k + 1) * 128, a:z])
                    b.ts(Wb[:, k, c0:c0 + n], sl[:, 0:n], g1[:, k:k + 1], None, op0=ALU.mult, eng="dve")
                c0 += n
        for dt in range(2):
            with ExitStack() as stg:
                b.stack = stg
                stg.callback(b.barrier)
                sfx = "g%d" % dt
                Wb = b.sbuf("Wb" + sfx, [128, NK, 800], BF16)
                wst = b.sbuf("wst" + sfx, [128, 2, 256], F32, nslots=2)
                load_w_cols(Wb, wst, [(dt * 128, dt * 128 + 128), (256 + dt * 128, 256 + dt * 128 + 128),
                                      (512 + dt * 256, 512 + dt * 256 + 256), (1024 + dt * 256, 1024 + dt * 256 + 256),
                                      (1536, 1552), (1552, 1568)])
                upf = b.sbuf("upf" + sfx, [16, 128], F32); upb = b.sbuf("upb" + sfx, [16, 128], F32)
                b.load(upf[:], up_f[:, dt * 128:(dt + 1) * 128]); b.load(upb[:], up_b[:, dt * 128:(dt + 1) * 128])
                nbias = b.sbuf("nbias" + sfx, [128, 2], F32)
                bst = b.sbuf("bst" + sfx, [128, 2], F32)
                bst2 = b.sbuf("bst2" + sfx, [128, 2], F32)
                b.load(bst[:], bias_f); b.load(bst2[:], bias_b)
                b.ts(nbias[:, 0:1], bst[:, dt:dt + 1], -1.0, None, op0=ALU.mult)
                b.ts(nbias[:, 1:2], bst2[:, dt:dt + 1], -1.0, None, op0=ALU.mult)
                ggain = b.sbuf("ggain" + sfx, [128, 128], F32)
                b.bcast_load(ggain[:], gla_gain)
                qef = b.sbuf("qef" + sfx, [128, T], BF16); kef = b.sbuf("kef" + sfx, [128, T], BF16)
                qeb = b.sbuf("qeb" + sfx, [128, T], BF16); keb = b.sbuf("keb" + sfx, [128, T], BF16)
                decf = b.sbuf("decf" + sfx, [128, NT], F32); decb = b.sbuf("decb" + sfx, [128, NT], F32)
                v_tm = b.sbuf("v_tm" + sfx, [128, NT, 256], BF16)
                G2 = b.sbuf("G2" + sfx, [128, NT, 256], BF16)
                zT = b.sbuf("zT" + sfx, [16, 2, 512], F32)
                qk32 = b.sbuf("qk32" + sfx, [128, 2, 512], F32)
                Pex = b.sbuf("Pex" + sfx, [128, 513], F32)
                Dd = b.sbuf("Dd" + sfx, [128, 512], F32)
                Ee = b.sbuf("Ee" + sfx, [128, 2, 512], F32)
                lap = b.sbuf("lap" + sfx, [128, 512], F32)
                gt = b.sbuf("gt" + sfx, [128, 3, 256], F32)
                with ExitStack() as stx:
                    b.stack = stx
                    stx.callback(b.barrier)
                    ptr = b.psum("ptr" + sfx, [128, 2, NK, 128], BF16, nslots=2)
                    pfm = b.psum("pfm" + sfx, [128, 2, 512], F32, nslots=2)
                    ptm = b.psum("ptm" + sfx, [128, 2, 512], F32, nslots=2)
                    pz = b.psum("pz" + sfx, [16, 2, 512], F32, nslots=2)
                    xgroup = xn_group_factory(sfx, ptr)
                    b.memset(Pex[:, 0:1], 0.0)
                    for g in range(NG):
                        xg = xgroup(g)
                        tok = slice(g * 512, (g + 1) * 512)
                        for qi in range(2):
                            for k in range(NK):
                                b.matmul(pfm.s(qi), Wb[:, k, qi * 128:(qi + 1) * 128], xg[:, k, :], start=(k == 0), stop=(k == NK - 1))
                            b.op("act", (lambda o_, i_, m_: (lambda e: e.mul(out=o_.ap, in_=i_.ap, mul=m_)))(qk32[:, qi, :], pfm.s(qi), (0.125 if qi == 0 else 1.0)), reads=[pfm.s(qi)], writes=[qk32[:, qi, :]])
                        for zi in range(2):
                            for k in range(NK):
                                b.matmul(pz.s(zi), Wb[:, k, 768 + zi * 16:768 + zi * 16 + 16], xg[:, k, :], start=(k == 0), stop=(k == NK - 1))
                            b.copy(zT[:, zi, :], pz.s(zi))
                        for di in range(2):
                            up = upf if di == 0 else upb
                            pl = pfm.s(di)
                            b.matmul(pl, up[:], zT[:, di, :])
                            b.act(lap[:], pl, AF.Exp, bias=nbias[:, di:di + 1], scale=-1.0)
                            b.act(lap[:], lap[:], AF.Ln, bias=1.0)
                            b.scan(Pex[:, 1:513], ones512[:], lap[:], 0.0, ALU.mult, ALU.add)
                            Pc = Pex[:, 1:513].re("p (n c) -> p n c", c=128)
                            Pe = Pex[:, 0:512].re("p (n c) -> p n c", c=128)
                            D3 = Dd[:].re("p (n c) -> p n c", c=128)
                            if di == 0:
                                b.tt(D3, Pc, Pe[:, :, 0:1].bc([128, 4, 128]), ALU.subtract)
                            else:
                                b.tt(D3, Pc[:, :, 127:128].bc([128, 4, 128]), Pe, ALU.subtract)
                            b.act(Ee[:, 0, :], Dd[:], AF.Exp, scale=-1.0 / 16.0)
                            b.act(Ee[:, 1, :], Dd[:], AF.Exp, scale=1.0 / 16.0)
                            qe, ke, dec = (qef, kef, decf) if di == 0 else (qeb, keb, decb)
                            b.tt(qe[:, tok], qk32[:, 0, :], Ee[:, 0, :], ALU.mult)
                            b.tt(ke[:, tok], qk32[:, 1, :], Ee[:, 1, :], ALU.mult)
                            E3 = Ee[:, 0, :].re("p (n c) -> p n c", c=128)
                            col = 127 if di == 0 else 0
                            b.copy(dec[:, g * 4:(g + 1) * 4].re("p (n o) -> p n o", o=1), E3[:, :, col:col + 1])
                        for j in range(4):
                            i = g * 4 + j
                            xl = [xg[:, k, j * 128:(j + 1) * 128] for k in range(NK)]
                            pv = ptm.s(0)
                            for k in range(NK):
                                b.matmul(pv[:, 0:256], xl[k], Wb[:, k, 256:512], start=(k == 0), stop=(k == NK - 1))
                            b.copy(v_tm[:, i, :], pv[:, 0:256], eng="act")
                            pg = ptm.s(1)
                            for k in range(NK):
                                b.matmul(pg[:, 0:256], xl[k], Wb[:, k, 512:768], start=(k == 0), stop=(k == NK - 1))
                            b.act(gt[:, 0, :], pg[:, 0:256], AF.Exp, scale=-1.0)
                            b.ts(gt[:, 0, :], gt[:, 0, :], 1.0, None, op0=ALU.add)
                            b.recip(gt[:, 1, :], gt[:, 0, :])
                            b.tt(gt[:, 2, :], gt[:, 1, :], pg[:, 0:256], ALU.mult)
                            b.tt(G2[:, i, :].re("p (h e) -> p h e", h=2), gt[:, 2, :].re("p (h e) -> p h e", h=2),
                                 ggain[:].re("p (o e) -> p o e", o=1).bc([128, 2, 128]), ALU.mult)
                b.mark('glaprep%d' % dt)
                with ExitStack() as stc:
                    b.stack = stc
                    stc.callback(b.barrier)
                    ketm = b.sbuf("ketm" + sfx, [128, 2, 128], BF16, nslots=2)
                    Sf = b.sbuf("Sf" + sfx, [128, NT, 256], BF16)
                    Sb = b.sbuf("Sb" + sfx, [128, 2, 256], BF16, nslots=2)
                    Tst = b.sbuf("Tst" + sfx, [128, 256], F32)
                    Am = b.sbuf("Am" + sfx, [128, 4, 128], BF16)
                    osb = b.sbuf("osb" + sfx, [128, 256], F32)
                    omx = b.sbuf("omx" + sfx, [128, 256], BF16)
                    jk = b.sbuf("jk" + sfx, [128, 128], BF16)
                    stt_ = b.sbuf("stt" + sfx, [128, 8], F32)
                    pkv_ = b.psum("pkv" + sfx, [128, 512], F32); pkv = pkv_[:, 0:256]
                    pa0 = b.psum("pa0" + sfx, [128, 4, 128], F32)
                    pa1 = b.psum("pa1" + sfx, [128, 4, 128], F32)
                    po_ = b.psum("po" + sfx, [128, 512], F32); po = po_[:, 0:256]
                    ptk = b.psum("ptk" + sfx, [128, 8, 128], BF16)
                    for n in range(NT):
                        ck = slice(n * 128, (n + 1) * 128)
                        if n >= 1:
                            b.stt(Sf[:, n, :], Tst[:], decf[:, n - 1:n], bdm[:], ALU.mult, ALU.mult)
                        if n == NT - 1:
                            break
                        b.transpose(ptk[:, 0, :], kef[:, ck], ident_b[:])
                        b.copy(ketm.s(n % 2), ptk[:, 0, :], eng="act")
                        b.matmul(pkv, ketm.s(n % 2), v_tm[:, n, :])
                        if n == 0:
                            b.copy(Tst[:], pkv)
                        else:
                            b.stt(Tst[:], Tst[:], decf[:, n - 1:n], pkv, ALU.mult, ALU.add)
                    for n in range(NT - 1, -1, -1):
                        ck = slice(n * 128, (n + 1) * 128)
                        sbc = Sb.s(n % 2)
                        if n < NT - 1:
                            b.stt(sbc, Tst[:], decb[:, n + 1:n + 2], bdm[:], ALU.mult, ALU.mult)
                        for hl in range(2):
                            pr = slice(hl * 64, (hl + 1) * 64)
                            pah = pa0 if hl == 0 else pa1
                            b.matmul(pah[:, 0, :], kef[pr, ck], qef[pr, ck])
                            b.matmul(pah[:, 1, :], keb[pr, ck], qeb[pr, ck])
                        for hl in range(2):
                            pah = pa0 if hl == 0 else pa1
                            b.tt(Am[:, 2 * hl:2 * hl + 2, :].re("p a c -> p (a c)"), pah[:, 0:2, :].re("p a c -> p (a c)"), mask4[:, 128:384], ALU.mult)
                        for hl in range(2):
                            pr = slice(hl * 64, (hl + 1) * 64)
                            es = slice(hl * 128, (hl + 1) * 128)
                            mms = [(Am[:, 2 * hl, :], v_tm[:, n, es]), (Am[:, 2 * hl + 1, :], v_tm[:, n, es])]
                            if n > 0:
                                mms.append((qef[:, ck], Sf[:, n, es]))
                            if n < NT - 1:
                                mms.append((qeb[:, ck], sbc[:, es]))
                            for mi, (l_, r_) in enumerate(mms):
                                b.matmul(po[:, es], l_, r_, start=(mi == 0), stop=(mi == len(mms) - 1))
                        if n > 0:
                            b.transpose(ptk[:, 1, :], keb[:, ck], ident_b[:])
                            b.copy(ketm.s(n % 2), ptk[:, 1, :], eng="act")
                            b.matmul(pkv, ketm.s(n % 2), v_tm[:, n, :])
                            if n == NT - 1:
                                b.copy(Tst[:], pkv)
                            else:
                                b.stt(Tst[:], Tst[:], decb[:, n + 1:n + 2], pkv, ALU.mult, ALU.add)
                        for hl in range(2):
                            es = slice(hl * 128, (hl + 1) * 128)
                            b.act(jk[:], po[:, es], AF.Square, accum_out=stt_[:, hl:hl + 1])
                        b.act(stt_[:, 2:4], stt_[:, 0:2], AF.Ln, bias=EPS, scale=1.0 / 128)
                        b.act(stt_[:, 4:6], stt_[:, 2:4], AF.Exp, scale=-0.5)
                        b.tt(osb[:].re("p (h e) -> p h e", h=2), po.re("p (h e) -> p h e", h=2),
                             stt_[:, 4:6].re("p (h o) -> p h o", o=1).bc([128, 2, 128]), ALU.mult)
                        b.tt(omx[:], osb[:], G2[:, n, :], ALU.mult)
                        for hl in range(2):
                            b.transpose(ptk[:, hl, :], omx[:, hl * 128:(hl + 1) * 128], ident_b[:])
                        b.copy(mixg[:, 2 * dt:2 * dt + 2, ck], ptk[:, 0:2, :])
            b.stack = STK
            b.mark('gla%d' % dt)
        with ExitStack() as sta:
            b.stack = sta
            sta.callback(b.barrier)
            mixa = b.sbuf("mixa", [128, 4, T], BF16)
            qT = b.sbuf("qT_att", [128, 4, T], BF16)
            kT = b.sbuf("kT_att", [128, T], BF16)
            Vaug = b.sbuf("Vaug", [128, NT, 2, 65], BF16)
            b.memset(Vaug[:, :, :, 64:65], 1.0)
            qg = b.sbuf("qg_row", [128, 64], F32); kg = b.sbuf("kg_row", [128, 64], F32)
            b.bcast_load(qg[:], q_gain)
            b.bcast_load(kg[:], k_gain)
            b.ts(qg[:], qg[:], 0.125, None, op0=ALU.mult)
            agr = b.sbuf("agr", [128, 512], F32)
            b.bcast_load(agr[:], att_gain)
            cosb = b.sbuf("cosb", [128, NT, 32], F32); sinb = b.sbuf("sinb", [128, NT, 32], F32)
            b.load(cosb[:], c_cos); b.load(sinb[:], c_sin)
            with ExitStack() as stx:
                b.stack = stx
                stx.callback(b.barrier)
                ptr = b.psum("ptr_a", [128, 2, NK, 128], BF16, nslots=2)
                ptm = b.psum("ptm_a", [128, 2, 512], F32, nslots=2)
                ptq = b.psum("ptq_a", [128, 8, 128], BF16)
                Wb = b.sbuf("Wb_a", [128, NK, 768], BF16)
                wst = b.sbuf("wst_a", [128, 2, 512], F32, nslots=2)
                load_w_cols(Wb, wst, [(1568, 2080), (2080, 2336)])
                sq = b.sbuf("sq_a", [128, 512], F32)
                s8 = b.sbuf("s8_a", [128, 24], F32)
                qn = b.sbuf("qn_a", [128, 512], F32)
                t1 = b.sbuf("t1_a", [128, 256], F32); t2 = b.sbuf("t2_a", [128, 256], F32)
                qr = b.sbuf("qr_a", [128, 512], BF16)
                kr = b.sbuf("kr_a", [128, 128], BF16)
                xgroup = xn_group_factory("a", ptr)

                def norm_rope(src_ps, hd, grow, i, dst_even, dst_odd):
                    nh = 1
                    for z_ in hd:
                        nh *= z_
                    w = nh * 64
                    b.act(sq[:, 0:w], src_ps, AF.Square)
                    b.reduce(s8[:, 0:nh], sq[:, 0:w].re("p (h d) -> p h d", d=64), ALU.add)
                    b.act(s8[:, 8:8 + nh], s8[:, 0:nh], AF.Ln, bias=EPS, scale=1.0 / 64)
                    b.act(s8[:, 16:16 + nh], s8[:, 8:8 + nh], AF.Exp, scale=-0.5)
                    b.tt(qn[:, 0:w].re("p (h d) -> p h d", d=64), src_ps.re("p (h d) -> p h d", d=64),
                         s8[:, 16:16 + nh].re("p (h o) -> p h o", o=1).bc([128, nh, 64]), ALU.mult)
                    b.tt(qn[:, 0:w].re("p (h d) -> p h d", d=64), qn[:, 0:w].re("p (h d) -> p h d", d=64),
                         grow[:].re("p (o d) -> p o d", o=1).bc([128, nh, 64]), ALU.mult)
                    if len(hd) == 2:
                        pat = "p (k g i two) -> p k g i two"; kw = dict(k=hd[0], g=hd[1], i=32, two=2)
                        pat3 = "p (k g i) -> p k g i"; kw3 = dict(k=hd[0], g=hd[1], i=32)
                        patc = "p (a c i) -> p a c i"; kwc = dict(a=1, c=1)
                        shp = [128, hd[0], hd[1], 32]
                        q4 = qn[:, 0:w].re(pat, **kw)
                        x0 = q4[:, :, :, :, 0]; x1 = q4[:, :, :, :, 1]
                    else:
                        pat = "p (k i two) -> p k i two"; kw = dict(k=hd[0], i=32, two=2)
                        pat3 = "p (k i) -> p k i"; kw3 = dict(k=hd[0], i=32)
                        patc = "p (a i) -> p a i"; kwc = dict(a=1)
                        shp = [128, hd[0], 32]
                        q4 = qn[:, 0:w].re(pat, **kw)
                        x0 = q4[:, :, :, 0]; x1 = q4[:, :, :, 1]
                    cb = cosb[:, i, :].re(patc, **kwc).bc(shp)
                    sb_ = sinb[:, i, :].re(patc, **kwc).bc(shp)
                    hw = nh * 32
                    a1 = t1[:, 0:hw].re(pat3, **kw3); a2 = t2[:, 0:hw].re(pat3, **kw3)
                    b.tt(a1, x0, cb, ALU.mult); b.tt(a2, x1, sb_, ALU.mult)
                    b.tt(dst_even, a1, a2, ALU.subtract)
                    b.tt(a1, x0, sb_, ALU.mult); b.tt(a2, x1, cb, ALU.mult)
                    b.tt(dst_odd, a1, a2, ALU.add)

                for g in range(NG):
                    xg = xgroup(g)
                    for j in range(4):
                        i = g * 4 + j
                        tk = slice(i * 128, (i + 1) * 128)
                        xl = [xg[:, k, j * 128:(j + 1) * 128] for k in range(NK)]
                        pq = ptm.s(0)
                        for k in range(NK):
                            b.matmul(pq, xl[k], Wb[:, k, 0:512], start=(k == 0), stop=(k == NK - 1))
                        pk = ptm.s(1)
                        for k in range(NK):
                            b.matmul(pk[:, 0:256], xl[k], Wb[:, k, 512:768], start=(k == 0), stop=(k == NK - 1))
                        qr5 = qr[:].re("p (g k i two) -> p k g i two", g=4, k=2, i=32, two=2)
                        norm_rope(pq, (2, 4), qg, i, qr5[:, :, :, :, 0], qr5[:, :, :, :, 1])
                        for g4 in range(4):
                            b.transpose(ptq[:, g4, :], qr[:, g4 * 128:(g4 + 1) * 128], ident_b[:])
                        b.copy(qT[:, :, tk], ptq[:, 0:4, :], eng="act")
                        kr4 = kr[:].re("p (h i two) -> p h i two", i=32, two=2)
                        norm_rope(pk[:, 0:128], (2,), kg, i, kr4[:, :, :, 0], kr4[:, :, :, 1])
                        b.transpose(ptq[:, 4, :], kr[:], ident_b[:])
                        b.copy(kT[:, tk], ptq[:, 4, :], eng="act")
                        b.copy(Vaug[:, i, :, 0:64], pk[:, 128:256].re("p (h d) -> p h d", d=64), eng="act")
            b.mark('attproj')
            for blk in range(NB * SUB):
                b.load(xs_d[blk * 128:(blk + 1) * 128, :], zeros_b[:], q="sp")
            with ExitStack() as stx:
                b.stack = stx
                stx.callback(b.barrier)
                ps = b.psum("ps_att", [128, 4, 512], F32, nslots=4)
                pov = b.psum("po_att", [128, 2, 512], F32, nslots=2)
                ptk = b.psum("ptk_att", [128, 8, 128], BF16)
                Eb = b.sbuf("Eb", [128, 4, 512], BF16, nslots=4)
                otm = b.sbuf("otm", [128, 4, 512], F32)
                rl = b.sbuf("rl", [128, 2, 4], F32, nslots=2)
                st8 = b.sbuf("st8", [128, 4], F32)
                jk = b.sbuf("jk_att", [128, 512], BF16)
                on = b.sbuf("on_att", [128, 512], F32)
                ob = b.sbuf("ob_att", [128, 512], BF16)
                steps = []
                for qc in range(NG):
                    for g4 in range(4):
                        for s_ in range(NT):
                            steps.append((qc, g4, s_))

                def emit_s(idx):
                    qc, g4, s_ = steps[idx]
                    ees = []
                    pss2 = []
                    for kv in range(2):
                        pr = slice(kv * 64, (kv + 1) * 64)
                        pss = ps.s(kv * 2 + idx % 2)
                        b.matmul(pss, kT[pr, s_ * 128:(s_ + 1) * 128], qT[pr, g4, qc * 512:(qc + 1) * 512])
                        pss2.append(pss)
                    for kv in range(2):
                        ee = Eb.s((idx % 2) * 2 + kv)
                        b.act(ee, pss2[kv], AF.Exp)
                        ees.append(ee)
                    return ees

                def emit_pv(idx, ees):
                  qc, g4, s_ = steps[idx]
                  for kv in range(2):
                    ee = ees[kv]
                    h = kv * 4 + g4
                    po_ = pov.s(kv)
                    po3 = po_[:, 0:260].re("p (j e) -> p j e", e=65)
                    for j in range(4):
                        b.matmul(po3[:, j, :], ee[:, j * 128:(j + 1) * 128], Vaug[:, s_, kv, :],
                                 start=(s_ == 0 and j == 0), stop=(s_ == NT - 1 and j == 3))
                    if s_ == NT - 1:
                        rr = rl.s(kv)
                        b.recip(rr.re("p (j o) -> p j o", o=1), po3[:, :, 64:65])
                        b.tt(otm[:].re("p j (h d) -> p j h d", d=64)[:, :, h, :], po3[:, :, 0:64],
                             rr.re("p (j o) -> p j o", o=1).bc([128, 4, 64]), ALU.mult)
                        if g4 == 3 and kv == 1:
                            for j in range(4):
                                i = qc * 4 + j
                                tk = slice(i * 128, (i + 1) * 128)
                                b.act(jk[:], otm[:, j, :], AF.Square, accum_out=st8[:, 0:1])
                                b.act(st8[:, 1:2], st8[:, 0:1], AF.Ln, bias=EPS, scale=1.0 / 512)
                                b.act(st8[:, 2:3], st8[:, 1:2], AF.Exp, scale=-0.5)
                                b.ts(on[:], otm[:, j, :], st8[:, 2:3], None, op0=ALU.mult)
                                b.tt(ob[:], on[:], agr[:], ALU.mult)
                                for c in range(4):
                                    b.transpose(ptk[:, c, :], ob[:, c * 128:(c + 1) * 128], ident_b[:])
                                b.copy(mixa[:, :, tk], ptk[:, 0:4, :])

                pend = None
                for idx in range(len(steps)):
                    ee = emit_s(idx)
                    if pend is not None:
                        emit_pv(*pend)
                    pend = (idx, ee)
                emit_pv(*pend)
            with ExitStack() as std:
                b.stack = std
                std.callback(b.barrier)
                Wo = b.sbuf("Wo", [128, NK, D], BF16)
                wst = b.sbuf("wst_o", [128, 2, 512], F32, nslots=2)
                for k in range(NK):
                    for hf in range(2):
                        b.load(wst.s(hf), w_out[k * 128:(k + 1) * 128, hf * 512:(hf + 1) * 512])
                        b.copy(Wo[:, k, hf * 512:(hf + 1) * 512], wst.s(hf), eng=("dve" if hf == 0 else "act"))
                g2r = b.sbuf("g2r", [128, D], F32)
                b.bcast_load(g2r[:], g2)
                Wr = b.sbuf("Wr", [128, NK, 72], F32)
                with nc.allow_non_contiguous_dma(reason="small router weights"):
                    for k in range(NK):
                        b.load(Wr[:, k, 0:8], w_group[k * 128:(k + 1) * 128, :])
                        b.load(Wr[:, k, 8:72], w_expert[k * 128:(k + 1) * 128, :])
                brow = b.sbuf("brow", [1, 72], F32)
                b.load(brow[:, 0:8], b_group.re("(o n) -> o n", o=1)); b.load(brow[:, 8:72], b_expert.re("(o n) -> o n", o=1))
                xn2b = b.sbuf("xn2b", [128, 2, D], BF16, nslots=2)
                mp = b.sbuf("mp", [128, 2, 192], F32, nslots=2)
                xt = b.sbuf("xt_o", [128, 2, D], F32, nslots=2)
                h1 = b.sbuf("h1_o", [128, 2, D], F32, nslots=2)
                xn2_2 = b.sbuf("xn2_o", [128, 2, D], F32, nslots=2)
                xn2T_2 = b.sbuf("xn2T_o", [128, 2, NK, 128], F32, nslots=2)
                jk_1 = b.sbuf("jk_o", [128, D], BF16)
                sm_2 = b.sbuf("sm_o", [128, 2, 32], F32, nslots=2)
                lg_2 = b.sbuf("lg_o", [128, 2, 72], F32, nslots=2)
                m8_2 = b.sbuf("m8_o", [128, 2, 16], F32, nslots=2)
                og_2 = b.sbuf("og_o", [128, 2, 8], F32, nslots=2)
                tmp8_2 = b.sbuf("tmp8_o", [128, 2, 8], F32, nslots=2)
                ge_2 = b.sbuf("ge_o", [128, 2, 8], F32, nslots=2)
                ml_2 = b.sbuf("ml_o", [128, 2, 64], F32, nslots=2)
                Mb_2 = b.sbuf("Mb_o", [128, 2, 64], BF16, nslots=2)
                py4 = b.psum("py_o", [128, 4, 512], F32, nslots=4)
                ptf = b.psum("ptf_o", [128, 2, 512], F32, nslots=2)
                plg = b.psum("plg_o", [128, 512], F32)
                pps = b.psum("pps_o", [128, 512], F32)
                b.load(xt.s(0), x[0:128, :])
                for i in range(NT):
                    tk = slice(i * 128, (i + 1) * 128)
                    s = i % 2
                    xn2 = xn2_2.s(s); xn2T = xn2T_2.s(s); jk = jk_1[:]; sm = sm_2.s(s); lg = lg_2.s(s); m8 = m8_2.s(s)
                    og = og_2.s(s); tmp8 = tmp8_2.s(s); ge = ge_2.s(s); ml = ml_2.s(s); Mb = Mb_2.s(s)
                    if i + 1 < NT:
                        b.load(xt.s((i + 1) % 2), x[(i + 1) * 128:(i + 2) * 128, :])
                    for hf in range(2):
                        for c in range(NK):
                            b.matmul(py4.s(2 * s + hf), (mixg if c < 4 else mixa)[:, c % 4, tk], Wo[:, c, hf * 512:(hf + 1) * 512], start=(c == 0), stop=(c == NK - 1))
                        b.tt(h1.s(s)[:, hf * 512:(hf + 1) * 512], py4.s(2 * s + hf), xt.s(s)[:, hf * 512:(hf + 1) * 512], ALU.add)
                    b.load(h1_d[tk, :], h1.s(s))
                    b.act(jk, h1.s(s), AF.Square, accum_out=sm[:, 0:1])
                    b.act(sm[:, 1:2], sm[:, 0:1], AF.Ln, bias=EPS, scale=1.0 / D)
                    b.act(sm[:, 2:3], sm[:, 1:2], AF.Exp, scale=-0.5)
                    b.stt(xn2, h1.s(s), sm[:, 2:3], g2r[:], ALU.mult, ALU.mult)
                    b.copy(xn2b.s(s).re("t (c p) -> t c p", c=NK), xn2.re("t (p c) -> t c p", c=NK), eng="act")
                    b.load(xn2_d[tk, :], xn2b.s(s))
                    for c in range(NK):
                        b.transpose(ptf.s(c // 4)[:, (c % 4) * 128:(c % 4 + 1) * 128], xn2[:, c * 128:(c + 1) * 128], ident_f[:])
                    b.copy(xn2T[:, 0:4, :].re("p c t -> p (c t)"), ptf.s(0), eng="act")
                    b.copy(xn2T[:, 4:8, :].re("p c t -> p (c t)"), ptf.s(1))
                    for c in range(NK):
                        b.matmul(plg[:, 0:72], xn2T[:, c, :], Wr[:, c, :], start=(c == 0), stop=False)
                    b.matmul(plg[:, 0:72], ones_f[0:1, :], brow[:], start=False, stop=True)
                    b.copy(lg, plg[:, 0:72])
                    b.max8(m8[:, 0:8], lg[:, 0:8])
                    b.ts(og, lg[:, 0:8], m8[:, 0:1], None, op0=ALU.is_equal)
                    b.ts(sm[:, 3:4], m8[:, 0:1], -1.0, None, op0=ALU.mult)
                    b.act(ge, lg[:, 0:8], AF.Exp, bias=sm[:, 3:4], scale=1.0, accum_out=sm[:, 4:5])
                    b.recip(sm[:, 5:6], sm[:, 4:5])
                    b.ts(tmp8, og, BIG, -BIG, op0=ALU.mult, op1=ALU.add)
                    b.tt(ml.re("p (g j) -> p g j", j=8), lg[:, 8:72].re("p (g j) -> p g j", j=8),
                         tmp8.re("p (g o) -> p g o", o=1).bc([128, 8, 8]), ALU.add)
                    b.max8(m8[:, 8:16], ml)
                    b.ts(mp.s(s)[:, 0:64], ml, m8[:, 8:9], None, op0=ALU.is_equal)
                    b.ts(mp.s(s)[:, 64:128], ml, m8[:, 9:10], None, op0=ALU.is_equal)
                    b.tt(sm[:, 6:7], m8[:, 9:10], m8[:, 8:9], ALU.subtract)
                    b.act(sm[:, 7:8], sm[:, 6:7], AF.Exp)
                    b.ts(sm[:, 8:9], sm[:, 7:8], 1.0, None, op0=ALU.add)
                    b.recip(sm[:, 9:10], sm[:, 8:9])
                    b.tt(sm[:, 10:11], sm[:, 7:8], sm[:, 9:10], ALU.mult)
                    b.tt(gates[:, i, 0:1], sm[:, 9:10], sm[:, 5:6], ALU.mult)
                    b.tt(gates[:, i, 1:2], sm[:, 10:11], sm[:, 5:6], ALU.mult)
                    b.tt(Mb, mp.s(s)[:, 0:64], mp.s(s)[:, 64:128], ALU.add)
                    b.matmul(pps[:, 0:64], lstrict_b[:], Mb)
                    b.matmul(pps[:, 64:128], ones_b[:], Mb)
                    b.tt(mp.s(s)[:, 128:192], pps[:, 0:64], run[:], ALU.add)
                    b.tt(run[:], pps[:, 64:128], run[:], ALU.add)
                    b.load(mp_d[:, i * 192:(i + 1) * 192], mp.s(s))
        b.mark('router')
        stS1.__exit__(None, None, None)
        b.barrier()
        b.stack = st0
        with ExitStack() as stl:
            b.stack = stl
            stl.callback(b.barrier)
            mpa = b.sbuf("mpa", [128, NT, 192], F32)
            b.load(mpa[:].re("p n c -> p (n c)"), mp_d)
            cmpA = b.sbuf("cmpA", [128, 64, NBMAX], BF16)
            nblk = b.sbuf("nblk", [128, 64], F32)
            pend = b.sbuf("pend", [128, 64], F32)
            pstart = b.sbuf("pstart", [128, 64], F32)
            ones64 = b.sbuf("ones64", [128, 64], F32)
            b.memset(ones64[:], 1.0)
            b.tt(cmpA[:], run[:].re("p (e o) -> p e o", o=1).bc([128, 64, NBMAX]),
                 thr[:, 0:NBMAX].re("p (o n) -> p o n", o=1).bc([128, 64, NBMAX]), ALU.is_gt)
            b.reduce(nblk[:], cmpA[:], ALU.add)
            b.ts(nblk[:], nblk[:], float(RB), None, op0=ALU.mult)
            b.scan(pend[:], ones64[:], nblk[:], 0.0, ALU.mult, ALU.add)
            b.tt(pstart[:], pend[:], nblk[:], ALU.subtract)
            cmpB = b.sbuf("cmpB", [128, NB, 64], BF16)
            bef = b.sbuf("bef", [128, NB], F32)
            b.tt(cmpB[:], pend[:].re("p (o e) -> p o e", o=1).bc([128, NB, 64]),
                 thr[:].re("p (n o) -> p n o", o=1).bc([128, NB, 64]), ALU.is_le)
            b.reduce(bef[:], cmpB[:], ALU.add)
            if debug:
                b.load(dbg_be, bef[:])
            b.ts(bef[:], bef[:], 128.0, pidx[:, 0:1], op0=ALU.mult, op1=ALU.add)
            b.copy(idxw_i[:], bef[:])
            destf = b.sbuf("destf", [128, NT, 2], F32)
            tqa = b.sbuf("tqa", [128, NT, 64], F32); tqb = b.sbuf("tqb", [128, NT, 64], F32)
            b.tt(tqa[:], mpa[:, :, 128:192], pstart[:].re("p (o e) -> p o e", o=1).bc([128, NT, 64]), ALU.add)
            for k2 in range(2):
                b.tt(tqb[:], tqa[:], mpa[:, :, k2 * 64:(k2 + 1) * 64], ALU.mult)
                b.reduce(destf[:, :, k2:k2 + 1].re("p n o -> p (n o)"), tqb[:], ALU.add)
            b.copy(dest_i[:], destf[:])
            if debug:
                dr = b.sbuf("dr", [128, NT, 4], F32)
                b.copy(dr[:, :, 0:2], destf[:]); b.copy(dr[:, :, 2:4], gates[:])
                b.load(dbg_route, dr[:])
            b.mark('layout')
            xr = b.sbuf("xr", [128, 2, D], BF16, nslots=2)
            b.load(xr.s(0), xn2_d[0:128, :])
            for i in range(NT):
                if i + 1 < NT:
                    b.load(xr.s((i + 1) % 2), xn2_d[(i + 1) * 128:(i + 2) * 128, :])
                for k2 in range(2):
                    b.scatter(xs_d, xr.s(i % 2), dest_i[:, i, k2:k2 + 1])
        b.stack = st0

        b.mark('scatter')
        with ExitStack() as stm:
            b.stack = stm
            stm.callback(b.barrier)
            wstg = b.sbuf("wstg", [128, 4, 4096], F32, nslots=4)
            wgb = b.sbuf("wgb", [128, 2, NK, 512], BF16, nslots=2)
            wub = b.sbuf("wub", [128, 2, NK, 512], BF16, nslots=2)
            wdb = b.sbuf("wdb", [128, 2, 4, D], BF16, nslots=2)
            xsb = b.sbuf("xsb", [128, 2, D], BF16, nslots=2)
            xsT = b.sbuf("xsT", [128, 2, NK, 128], BF16, nslots=2)
            hh = b.sbuf("hh", [128, 2, 512], BF16, nslots=2)
            hT = b.sbuf("hT", [128, 4, 128], BF16)
            et = b.sbuf("et", [128, 2, 3, 512], F32, nslots=2)
            ysb = b.sbuf("ysb", [128, 2, D], F32, nslots=2)
            ptx = b.psum("ptx", [128, NK, 128], BF16)
            pth = b.psum("pth", [128, NK, 128], BF16)
            pg2 = b.psum("pg_m", [128, 2, 512], F32, nslots=2)
            pu2 = b.psum("pu_m", [128, 2, 512], F32, nslots=2)
            pyy = b.psum("pyy", [128, 2, 512], F32, nslots=2)
            wg_v = w_gate.re("e (p c) f -> (e p) (c f)", c=NK)
            wu_v = w_up.re("e (p c) f -> (e p) (c f)", c=NK)
            wd_v = w_down.re("e (p c) d -> (e p) (c d)", c=4)
            sc = [0]

            order = []
            lo, hi = 0, NB - 1
            while lo <= hi:
                order.append(lo); lo += 1
                if lo <= hi:
                    order.append(hi); hi -= 1
            seq = [(slot, sub) for slot in order for sub in range(SUB)]
            NSEQ = len(seq)

            def fetch_w(pos):
                slot = order[pos]
                ws = pos % 2
                for (wv, dstb, eng) in ((wg_v, wgb, "act"), (wu_v, wub, "dve"), (wd_v, wdb, "mix")):
                    sl = wstg.s(sc[0] % 4); sc[0] += 1
                    b.gather(sl, wv, idxw_i[:, slot:slot + 1], bounds=64 * 128 - 1)
                    dv = dstb.s(ws).re("p a f -> p (a f)")
                    if eng == "mix":
                        b.copy(dv[:, 0:2048], sl[:, 0:2048], eng="act")
                        b.copy(dv[:, 2048:4096], sl[:, 2048:4096], eng="dve")
                    else:
                        b.copy(dv, sl, eng=eng)

            def blk_of(q):
                slot, sub = seq[q]
                return slot * SUB + sub

            def fetch_x(q):
                blk = blk_of(q)
                b.load(xsb.s(q % 2), xs_d[blk * 128:(blk + 1) * 128, :])

            def stage1(q):
                ws = (q // SUB) % 2
                p2 = q % 2
                if q + 1 < NSEQ:
                    fetch_x(q + 1)
                pg = pg2.s(p2); pu = pu2.s(p2); et_ = et.s(p2); xT = xsT.s(p2)
                for c in range(NK):
                    b.transpose(ptx[:, c, :], xsb.s(p2)[:, c * 128:(c + 1) * 128], ident_b[:])
                b.copy(xT, ptx[:], eng="act")
                for c in range(NK):
                    b.matmul(pg, xT[:, c, :], wgb.s(ws)[:, c, :], start=(c == 0), stop=(c == NK - 1))
                for c in range(NK):
                    b.matmul(pu, xT[:, c, :], wub.s(ws)[:, c, :], start=(c == 0), stop=(c == NK - 1))
                b.act(et_[:, 2, :], pg, AF.Silu)
                b.tt(hh.s(p2).re("s (c p) -> s c p", c=4), et_[:, 2, :].re("s (p c) -> s c p", c=4), pu.re("s (p c) -> s c p", c=4), ALU.mult)

            def stage2(q):
                blk = blk_of(q)
                ws = (q // SUB) % 2
                p2 = q % 2
                for c in range(4):
                    b.transpose(pth[:, c, :], hh.s(p2)[:, c * 128:(c + 1) * 128], ident_b[:])
                b.copy(hT[:], pth[:, 0:4, :], eng="act")
                for hf in range(2):
                    for c in range(4):
                        b.matmul(pyy.s(hf), hT[:, c, :], wdb.s(ws)[:, c, hf * 512:(hf + 1) * 512], start=(c == 0), stop=(c == 3))
                b.copy(ysb.s(p2)[:, 0:512], pyy.s(0), eng="act")
                b.copy(ysb.s(p2)[:, 512:1024], pyy.s(1))
                b.load(ys_d[blk * 128:(blk + 1) * 128, :], ysb.s(p2))

            fetch_w(0)
            fetch_x(0)
            stage1(0)
            for q in range(NSEQ):
                if q % SUB == 0 and q // SUB + 1 < NB:
                    fetch_w(q // SUB + 1)
                if q + 1 < NSEQ:
                    stage1(q + 1)
                stage2(q)
        b.stack = st0

        b.mark('moe')
        with ExitStack() as stf:
            b.stack = stf
            fgr = b.sbuf("fgr", [128, D], F32)
            b.bcast_load(fgr[:], fgain)
            y1 = b.sbuf("y1", [128, 2, D], F32, nslots=2); y2 = b.sbuf("y2", [128, 2, D], F32, nslots=2)
            hh1 = b.sbuf("hh1", [128, 2, D], F32, nslots=2)
            acc = b.sbuf("acc", [128, D], F32); acc2 = b.sbuf("acc2", [128, 2, D], F32, nslots=2)
            ot = b.sbuf("ot", [128, 2, D], F32, nslots=2)
            jk = b.sbuf("jk_f", [128, D], BF16)
            sf = b.sbuf("sf", [128, 2, 4], F32, nslots=2)
            outs = []

            def fetch_f(i):
                s = i % 2
                b.gather(y1.s(s), ys_d, dest_i[:, i, 0:1])
                b.gather(y2.s(s), ys_d, dest_i[:, i, 1:2])
                b.load(hh1.s(s), h1_d[i * 128:(i + 1) * 128, :])

            def combine(i):
                s = i % 2
                if i + 1 < NT:
                    fetch_f(i + 1)
                b.stt(acc[:], y1.s(s), gates[:, i, 0:1], hh1.s(s), ALU.mult, ALU.add)
                b.stt(acc2.s(s), y2.s(s), gates[:, i, 1:2], acc[:], ALU.mult, ALU.add)
                sfs = sf.s(s)
                b.act(jk[:], acc2.s(s), AF.Square, accum_out=sfs[:, 0:1])
                b.act(sfs[:, 1:2], sfs[:, 0:1], AF.Ln, bias=EPS, scale=1.0 / D)
                b.act(sfs[:, 2:3], sfs[:, 1:2], AF.Exp, scale=-0.5)

            def finish(i):
                s = i % 2
                b.stt(ot.s(s), acc2.s(s), sf.s(s)[:, 2:3], fgr[:], ALU.mult, ALU.mult)
                outs.append(b.load(out[i * 128:(i + 1) * 128, :], ot.s(s)))

            fetch_f(0)
            combine(0)
            for i in range(NT):
                if i + 1 < NT:
                    combine(i + 1)
                finish(i)
            b.wait_all("sp", outs)

        def tail(bb):
            bb.waited = {e: {} for e in bb.prog}
            bb.latest = {}
            bb.lastw = {}
            bb.readers = {}
            for e, lst in bb.prog.items():
                for idx, it in enumerate(lst):
                    if it[0] == "dma":
                        bb.latest[it[3]] = max(bb.latest.get(it[3], 0), 0)
            toks = []
            cnt = {}
            for e, lst in bb.prog.items():
                for idx, it in enumerate(lst):
                    if it[0] == "dma":
                        cnt[it[3]] = cnt.get(it[3], 0) + 16
                        bb.latest[it[3]] = cnt[it[3]]
                    elif it[2] is not None and e in COMPUTE:
                        bb.latest[("e", e)] = idx
            for q in bb.dma_cnt:
                for i in range(bb.n_dma):
                    bb.dma_cnt[q][i] = cnt.get(("d", q, i), 0) // 16
            bb.barrier()
            for i in range(NT):
                toks.append(bb.load(out[i * 128:(i + 1) * 128, :], zeros_f[:]))
            bb.wait_all("sp", toks)
        b.emit(trunc=trunc, tail=tail)
    return nc


GRID_W = 64
ROPE_THETA = 10000.0
_NC_CACHE = {}


def _consts(T):
    NT = T // 128
    RB = 256
    NB = 2 * T // RB + 63
    s = np.arange(128)[:, None]
    c = np.arange(128)[None, :]
    maskU = (s <= c).astype(np.float32)
    maskL = (s >= c).astype(np.float32)
    t = np.arange(T)
    row = (t // GRID_W).astype(np.float32)
    col = (t % GRID_W).astype(np.float32)
    axis_dim = 32
    inv_freq = (ROPE_THETA ** (-np.arange(0, axis_dim, 2, dtype=np.float32) / axis_dim)).astype(np.float32)
    ang = np.concatenate([row[:, None] * inv_freq, col[:, None] * inv_freq], axis=-1).astype(np.float32)
    cos = np.cos(ang).astype(np.float32).reshape(NT, 128, 32).transpose(1, 0, 2)
    sin = np.sin(ang).astype(np.float32).reshape(NT, 128, 32).transpose(1, 0, 2)
    return dict(
        c_ident=np.eye(128, dtype=np.float32),
        c_mask4=np.ascontiguousarray(np.concatenate([maskU, maskU, maskL, maskL], axis=1)),
        c_lstrict=(s < c).astype(np.float32),
        c_cos=np.ascontiguousarray(cos), c_sin=np.ascontiguousarray(sin),
        c_pidx=np.arange(128, dtype=np.float32).reshape(128, 1),
        c_thr=np.ascontiguousarray(np.broadcast_to((float(RB) * np.arange(NB, dtype=np.float32))[None, :], (128, NB))),
        c_bd=np.ascontiguousarray(((np.arange(128)[:, None] // 64) == (np.arange(256)[None, :] // 128)).astype(np.float32)),
    )


def _shared_inputs(T, norm1_gain, w_in, gla_up_fwd, gla_up_fwd_bias, gla_up_bwd, gla_up_bwd_bias, gla_out_gain,
                   q_norm_gain, k_norm_gain, att_out_gain, w_out, norm2_gain, w_group, b_group, w_expert, b_expert,
                   w_gate, w_up, w_down, final_gain):
    f = lambda a: np.ascontiguousarray(np.asarray(a, dtype=np.float32))
    d = dict(
        w_in=f(w_in[0]), g1pk=f(np.asarray(norm1_gain[0]).reshape(8, 128).T),
        up_f=f(gla_up_fwd[0]), up_b=f(gla_up_bwd[0]),
        bias_f=f(np.asarray(gla_up_fwd_bias[0]).reshape(2, 128).T), bias_b=f(np.asarray(gla_up_bwd_bias[0]).reshape(2, 128).T),
        gla_gain=f(gla_out_gain[0]), q_gain=f(q_norm_gain[0]), k_gain=f(k_norm_gain[0]), att_gain=f(att_out_gain[0]),
        w_out=f(w_out[0]), g2=f(norm2_gain[0]), w_group=f(w_group[0]), b_group=f(b_group[0]),
        w_expert=f(w_expert[0]), b_expert=f(b_expert[0]),
        w_gate=f(w_gate[0]), w_up=f(w_up[0]), w_down=f(w_down[0]), fgain=f(final_gain),
    )
    d.update(_consts(T))
    return d


def kernel(x, **params):
    x = np.asarray(x, dtype=np.float32)
    B, T, _ = x.shape
    if T not in _NC_CACHE:
        _NC_CACHE[T] = build(T)
    nc = _NC_CACHE[T]
    shared = _shared_inputs(T, **params)
    in_maps = []
    for bi in range(B):
        m = dict(shared)
        m["x"] = np.ascontiguousarray(x[bi])
        in_maps.append(m)
    res = run_bass_kernel_spmd(nc, in_maps, core_ids=list(range(B)))
    return np.stack([np.asarray(r["out"], dtype=np.float32) for r in res.results], axis=0)
```

```python
import numpy as np
from contextlib import ExitStack
import concourse.bass as bass
import concourse.mybir as mybir
from concourse.bass_utils import run_bass_kernel_spmd

F32 = mybir.dt.float32
BF16 = mybir.dt.bfloat16
I32 = mybir.dt.int32
AF = mybir.ActivationFunctionType
ALU = mybir.AluOpType
AX = mybir.AxisListType

SAME_ENGINE_SYNC = True
COMPUTE = ("pe", "act", "dve", "pool")


class V:
    __slots__ = ("ap", "keys")

    def __init__(self, ap, keys):
        self.ap = ap
        self.keys = tuple(keys)

    def __getitem__(self, idx):
        return V(self.ap[idx], self.keys)

    def re(self, s, **kw):
        return V(self.ap.rearrange(s, **kw), self.keys)

    def bc(self, shape):
        return V(self.ap.to_broadcast(list(shape)), self.keys)

    def bitcast(self, dt):
        return V(self.ap.bitcast(dt), self.keys)


class Tile:
    def __init__(self, b, name, handle, nslots):
        self.b = b
        self.name = name
        self.h = handle
        self.nslots = nslots

    def all(self):
        if self.nslots:
            return V(self.h[:], [(self.name, i) for i in range(self.nslots)])
        return V(self.h[:], [(self.name, None)])

    def s(self, i):
        assert self.nslots and 0 <= i < self.nslots
        return V(self.h[:, i], [(self.name, i)])

    def __getitem__(self, idx):
        return self.all()[idx]


class Builder:
    def __init__(self, nc, n_dma_sems=24):
        self.nc = nc
        self.prog = {e: [] for e in ("pe", "act", "dve", "pool", "sp")}
        self.waited = {e: {} for e in self.prog}
        self.lastw = {}
        self.readers = {}
        self.n_dma = n_dma_sems
        self.dma_cnt = {"sp": [0] * n_dma_sems, "pool": [0] * n_dma_sems, "act": [0] * n_dma_sems}
        self.dma_rr = {"sp": 0, "pool": 0, "act": 0}
        self.stack = None
        self.uid = 0
        self.out_tokens = []
        self.latest = {}
        self.bounds_reg = None
        self.marks = {}

    def sbuf(self, name, shape, dtype, nslots=0):
        h = self.stack.enter_context(self.nc.sbuf_tensor(name, list(shape), dtype))
        return Tile(self, name, h, nslots)

    def psum(self, name, shape, dtype, nslots=0):
        h = self.stack.enter_context(self.nc.psum_tensor(name, list(shape), dtype))
        return Tile(self, name, h, nslots)

    def dram(self, name, shape, dtype, kind="Internal"):
        t = self.nc.dram_tensor(name, list(shape), dtype, kind=kind)
        return V(t.ap(), [(name, None)])

    def _deps(self, eng, reads, writes, skip_self):
        toks = []
        for v in reads:
            for k in v.keys:
                toks += list(self.lastw.get(k, {}).items())
        for v in writes:
            for k in v.keys:
                toks += list(self.lastw.get(k, {}).items())
                toks += list(self.readers.get(k, {}).items())
        need = {}
        for src, val in toks:
            if src == ("e", eng) and (skip_self or not SAME_ENGINE_SYNC or eng == "pe"):
                continue
            if self.waited[eng].get(src, -1) >= val:
                continue
            if need.get(src, -1) < val:
                need[src] = val
        for src, val in need.items():
            self.waited[eng][src] = val
        return list(need.items())

    def _commit(self, tok, reads, writes, partial):
        src, val = tok
        if self.latest.get(src, -1) < val:
            self.latest[src] = val
        for v in reads:
            for k in v.keys:
                d = self.readers.setdefault(k, {})
                if d.get(src, -1) < val:
                    d[src] = val
        for v in writes:
            for k in v.keys:
                d = self.lastw.setdefault(k, {})
                if d.get(src, -1) < val:
                    d[src] = val

    def op(self, eng, fn, reads=(), writes=(), skip_self=False, partial=False):
        reads = [r for r in reads if isinstance(r, V)]
        writes = [w for w in writes if isinstance(w, V)]
        waits = self._deps(eng, reads, writes, skip_self)
        idx = len(self.prog[eng])
        tok = (("e", eng), idx)
        self.prog[eng].append(["op", waits, fn, None])
        self._commit(tok, reads, writes, partial)
        return tok

    def dma(self, q, fn, reads=(), writes=(), partial=False):
        reads = [r for r in reads if isinstance(r, V)]
        writes = [w for w in writes if isinstance(w, V)]
        i = self.dma_rr[q]
        self.dma_rr[q] = (i + 1) % self.n_dma
        src = ("d", q, i)
        waits = self._deps(q, reads, writes, False)
        prev = self.dma_cnt[q][i]
        if prev > 0 and self.waited[q].get(src, -1) < prev * 16:
            waits.append((src, prev * 16))
            self.waited[q][src] = prev * 16
        self.dma_cnt[q][i] += 1
        tok = (src, self.dma_cnt[q][i] * 16)
        self.prog[q].append(["dma", waits, fn, src])
        self._commit(tok, reads, writes, partial)
        return tok

    def mark(self, name):
        if name in self.marks:
            return
        self.barrier()
        self.marks[name] = {e: len(v) for e, v in self.prog.items()}

    def barrier(self):
        for eng in self.prog:
            need = {}
            for src, val in self.latest.items():
                if src == ("e", eng) and (eng == "pe" or not SAME_ENGINE_SYNC):
                    continue
                if self.waited[eng].get(src, -1) >= val:
                    continue
                need[src] = val
            for src, val in need.items():
                self.waited[eng][src] = val
            if need:
                self.prog[eng].append(["op", list(need.items()), None, None])

    def wait_all(self, eng, toks):
        need = {}
        for src, val in toks:
            if self.waited[eng].get(src, -1) >= val:
                continue
            if need.get(src, -1) < val:
                need[src] = val
        self.prog[eng].append(["op", list(need.items()), None, None])

    def emit(self, trunc=None, tail=None):
        nc = self.nc
        if trunc is not None:
            self.prog = {e: v[:self.marks[trunc][e]] for e, v in self.prog.items()}
            tail(self)
        signal = {e: set() for e in COMPUTE}
        for e, lst in self.prog.items():
            for item in lst:
                for src, val in item[1]:
                    if src[0] == "e":
                        signal[src[1]].add(val)
        rank = {}
        for e in COMPUTE:
            r = {}
            c = 0
            for idx in sorted(signal[e]):
                c += 1
                r[idx] = c
            rank[e] = r
        from contextlib import ExitStack
        with ExitStack() as st:
            esem = {e: st.enter_context(nc.semaphore("sem_" + e)) for e in COMPUTE}
            dsem = {}
            for q in ("sp", "pool", "act"):
                for i in range(self.n_dma):
                    if self.dma_cnt[q][i] > 0:
                        dsem[("d", q, i)] = st.enter_context(nc.semaphore("dsem_%s_%d" % (q, i)))
            block = st.enter_context(nc.Block())

            def run(ename, eng):
                for idx, (kind, waits, fn, dsrc) in enumerate(self.prog[ename]):
                    for src, val in waits:
                        if src[0] == "e":
                            eng.wait_ge(esem[src[1]], rank[src[1]][val])
                        else:
                            eng.wait_ge(dsem[src], val)
                    if fn is None:
                        continue
                    ins = fn(eng)
                    if kind == "dma":
                        ins.then_inc(dsem[dsrc], 16)
                    elif ename in COMPUTE and idx in rank[ename]:
                        ins.then_inc(esem[ename], 1)

            @block.tensor
            def _(t):
                run("pe", t)

            @block.scalar
            def _(a):
                run("act", a)

            @block.vector
            def _(v):
                run("dve", v)

            @block.gpsimd
            def _(g):
                run("pool", g)

            @block.sync
            def _(s):
                run("sp", s)

    def matmul(self, out, lhsT, rhs, start=True, stop=True):
        return self.op("pe", lambda e: e.matmul(out.ap, lhsT=lhsT.ap, rhs=rhs.ap, start=start, stop=stop),
                       reads=[lhsT, rhs], writes=[out], partial=not start)

    def transpose(self, out, in_, ident):
        return self.op("pe", lambda e: e.transpose(out=out.ap, in_=in_.ap, identity=ident.ap),
                       reads=[in_, ident], writes=[out], partial=True)

    def act(self, out, in_, func, bias=0.0, scale=1.0, accum_out=None, eng="act"):
        ba = bias.ap if isinstance(bias, V) else bias
        sa = scale.ap if isinstance(scale, V) else scale
        kw = {}
        if accum_out is not None:
            kw["accum_out"] = accum_out.ap
        return self.op("act", lambda e: e.activation(out=out.ap, in_=in_.ap, func=func, bias=ba, scale=sa, **kw),
                       reads=[in_, bias, scale], writes=[out] + ([accum_out] if accum_out is not None else []))

    def ts(self, out, in0, s1, s2=None, op0=ALU.mult, op1=None, eng="dve", accum_out=None):
        a1 = s1.ap if isinstance(s1, V) else s1
        a2 = s2.ap if isinstance(s2, V) else s2
        kw = {}
        if op1 is not None:
            kw["op1"] = op1
        if accum_out is not None:
            kw["accum_out"] = accum_out.ap
        return self.op(eng, lambda e: e.tensor_scalar(out=out.ap, in0=in0.ap, scalar1=a1, scalar2=a2, op0=op0, **kw),
                       reads=[in0, s1, s2], writes=[out] + ([accum_out] if accum_out is not None else []))

    def tt(self, out, in0, in1, op, eng="dve"):
        return self.op(eng, lambda e: e.tensor_tensor(out=out.ap, in0=in0.ap, in1=in1.ap, op=op),
                       reads=[in0, in1], writes=[out])

    def stt(self, out, in0, scalar, in1, op0, op1):
        sa = scalar.ap if isinstance(scalar, V) else scalar
        return self.op("dve", lambda e: e.scalar_tensor_tensor(out=out.ap, in0=in0.ap, scalar=sa, in1=in1.ap, op0=op0, op1=op1),
                       reads=[in0, scalar, in1], writes=[out])

    def copy(self, out, in_, eng="dve"):
        if eng == "act":
            return self.op("act", lambda e: e.copy(out=out.ap, in_=in_.ap), reads=[in_], writes=[out])
        return self.op(eng, lambda e: e.tensor_copy(out=out.ap, in_=in_.ap), reads=[in_], writes=[out])

    def reduce(self, out, in_, op, axis=AX.X):
        return self.op("dve", lambda e: e.tensor_reduce(out=out.ap, in_=in_.ap, axis=axis, op=op),
                       reads=[in_], writes=[out])

    def recip(self, out, in_):
        return self.op("dve", lambda e: e.reciprocal(out=out.ap, in_=in_.ap), reads=[in_], writes=[out])

    def max8(self, out, in_):
        return self.op("dve", lambda e: e.max(out=out.ap, in_=in_.ap), reads=[in_], writes=[out])

    def scan(self, out, d0, d1, initial, op0, op1):
        ia = initial.ap if isinstance(initial, V) else initial
        return self.op("dve", lambda e: e.tensor_tensor_scan(out=out.ap, data0=d0.ap, data1=d1.ap, initial=ia, op0=op0, op1=op1),
                       reads=[d0, d1, initial], writes=[out])

    def memset(self, out, val, eng="dve"):
        return self.op(eng, lambda e: e.memset(out.ap, val), writes=[out])

    def iota(self, out, pattern, base=0, channel_multiplier=0, allow=False):
        return self.op("pool", lambda e: e.iota(out.ap, pattern=pattern, base=base, channel_multiplier=channel_multiplier,
                                                allow_small_or_imprecise_dtypes=allow), writes=[out])

    def load(self, out, in_, q="sp", partial=False):
        return self.dma(q, lambda e: e.dma_start(out=out.ap, in_=in_.ap), reads=[in_], writes=[out], partial=partial)

    def bcast_load(self, out, vec):
        return self.dma("sp", lambda e: e.dma_start(out=out.ap, in_=vec.ap.partition_broadcast(128)), reads=[vec], writes=[out])

    def gather(self, out, in_, idx, bounds=None, partial=False):
        def fn(e):
            kw = {}
            if bounds is not None:
                if self.bounds_reg is None:
                    self.bounds_reg = (e.alloc_register("gather_bound"), bounds)
                    e.reg_mov(self.bounds_reg[0], bounds)
                assert self.bounds_reg[1] == bounds
                kw["bounds_check"] = self.bounds_reg[0]
                kw["oob_is_err"] = False
            return e.indirect_dma_start(out=out.ap, out_offset=None, in_=in_.ap,
                                        in_offset=bass.IndirectOffsetOnAxis(ap=idx.ap, axis=0), **kw)
        return self.dma("pool", fn, reads=[in_, idx], writes=[out], partial=partial)

    def scatter(self, out, in_, idx, bounds=None):
        def fn(e):
            kw = {}
            if bounds is not None:
                kw["bounds_check"] = bounds
                kw["oob_is_err"] = False
            return e.indirect_dma_start(out=out.ap, out_offset=bass.IndirectOffsetOnAxis(ap=idx.ap, axis=0),
                                        in_=in_.ap, in_offset=None, **kw)
        return self.dma("pool", fn, reads=[in_, idx], writes=[out], partial=True)


D = 1024
NK = 8
EPS = 1e-6
BIG = 1.0e4


def build(T, debug=False, trunc=None):
    NT = T // 128
    NG = T // 512
    RB = 256
    SUB = RB // 128
    NB = 2 * T // RB + 63
    NBMAX = 2 * T // RB
    nc = bass.Bass("TRN2", target_bir_lowering=False)
    b = Builder(nc)

    def din(name, shape, dt=F32):
        return V(nc.dram_tensor(name, list(shape), dt, kind="ExternalInput").ap(), [(name, None)])

    def dout(name, shape, dt=F32, kind="ExternalOutput"):
        return V(nc.dram_tensor(name, list(shape), dt, kind=kind).ap(), [(name, None)])

    x = din("x", [T, D])
    w_in = din("w_in", [D, 2336])
    g1pk = din("g1pk", [128, 8])
    up_f = din("up_f", [16, 256]); up_b = din("up_b", [16, 256])
    bias_f = din("bias_f", [128, 2]); bias_b = din("bias_b", [128, 2])
    gla_gain = din("gla_gain", [128])
    q_gain = din("q_gain", [64]); k_gain = din("k_gain", [64]); att_gain = din("att_gain", [512])
    w_out = din("w_out", [D, D]); g2 = din("g2", [D])
    w_group = din("w_group", [D, 8]); b_group = din("b_group", [8])
    w_expert = din("w_expert", [D, 64]); b_expert = din("b_expert", [64])
    w_gate = din("w_gate", [64, D, 512]); w_up = din("w_up", [64, D, 512]); w_down = din("w_down", [64, 512, D])
    fgain = din("fgain", [D])
    c_ident = din("c_ident", [128, 128]); c_mask4 = din("c_mask4", [128, 512]); c_lstrict = din("c_lstrict", [128, 128])
    c_cos = din("c_cos", [128, NT, 32]); c_sin = din("c_sin", [128, NT, 32])
    c_pidx = din("c_pidx", [128, 1]); c_thr = din("c_thr", [128, NB]); c_bd = din("c_bd", [128, 256])
    out = dout("out", [T, D])
    dk = "ExternalOutput" if debug else "Internal"
    h1_d = dout("h1_d", [T, D], F32, dk)
    xs_d = dout("xs_d", [NB * RB, D], BF16, dk)
    ys_d = dout("ys_d", [NB * RB, D], F32, dk)
    mp_d = dout("mp_d", [128, NT * 192], F32, "Internal")
    xn2_d = dout("xn2_d", [T, D], BF16, "Internal")
    if debug:
        dbg_mixg = dout("dbg_mixg", [128, 4, T], BF16)
        dbg_mixa = dout("dbg_mixa", [128, 4, T], BF16)
        dbg_route = dout("dbg_route", [128, NT, 4])
        dbg_be = dout("dbg_be", [128, NB])

    with ExitStack() as st0:
        b.stack = st0
        ident_f = b.sbuf("ident_f", [128, 128], F32); ident_b = b.sbuf("ident_b", [128, 128], BF16)
        mask4 = b.sbuf("mask4", [128, 512], F32)
        lstrict_f = b.sbuf("lstrict_f", [128, 128], F32); lstrict_b = b.sbuf("lstrict_b", [128, 128], BF16)
        ones_b = b.sbuf("ones_b", [128, 128], BF16); ones_f = b.sbuf("ones_f", [128, 128], F32)
        zeros_b = b.sbuf("zeros_b", [128, 1024], BF16)
        zeros_f = b.sbuf("zeros_f", [128, 1024], F32)
        b.memset(zeros_f[:], 0.0, eng="pool")
        pidx = b.sbuf("pidx", [128, 1], F32); thr = b.sbuf("thr", [128, NB], F32)
        g1 = b.sbuf("g1", [128, 8], F32)
        b.load(ident_f[:], c_ident); b.load(mask4[:], c_mask4); b.load(lstrict_f[:], c_lstrict)
        bdm = b.sbuf("bdm", [128, 256], F32)
        b.load(bdm[:], c_bd)
        b.load(pidx[:], c_pidx); b.load(thr[:], c_thr); b.load(g1[:], g1pk)
        b.copy(ident_b[:], ident_f[:]); b.copy(lstrict_b[:], lstrict_f[:])
        b.memset(ones_b[:], 1.0); b.memset(ones_f[:], 1.0); b.memset(zeros_b[:], 0.0, eng="pool")
        ones512 = b.sbuf("ones512", [128, 512], F32)
        b.memset(ones512[:], 1.0)
        gates = b.sbuf("gates", [128, NT, 2], F32)
        dest_i = b.sbuf("dest_i", [128, NT, 2], I32)
        idxw_i = b.sbuf("idxw_i", [128, NB], I32)
        run = b.sbuf("run", [128, 64], F32)
        b.memset(run[:], 0.0)
        stS1 = ExitStack(); stS1.__enter__(); b.stack = stS1
        mixg = b.sbuf("mixg", [128, 4, T], BF16)
        STK = stS1

        def xn_group_factory(stk, psum_tr):
            xt = b.sbuf("xt" + stk, [128, 2, D], F32, nslots=2)
            junk = b.sbuf("junk" + stk, [128, D], BF16)
            xnb = b.sbuf("xnb" + stk, [128, 2, D], BF16, nslots=2)
            xnT = b.sbuf("xnT" + stk, [128, 2, NK, 512], BF16, nslots=2)
            st1 = b.sbuf("st1" + stk, [128, 2, 4], F32, nslots=2)

            def load_tile(i):
                b.load(xt.s(i % 2), x[i * 128:(i + 1) * 128, :])

            def group(g):
                gs = g % 2
                for j in range(4):
                    i = g * 4 + j
                    s = i % 2
                    if i == 0:
                        load_tile(0)
                    if i + 1 < NT:
                        load_tile(i + 1)
                    ss = st1.s(s)
                    b.act(junk[:], xt.s(s), AF.Square, accum_out=ss[:, 0:1])
                    b.act(ss[:, 1:2], ss[:, 0:1], AF.Ln, bias=EPS, scale=1.0 / D)
                    b.act(ss[:, 2:3], ss[:, 1:2], AF.Exp, scale=-0.5)
                    b.ts(xnb.s(s), xt.s(s), ss[:, 2:3], None, op0=ALU.mult)
                    ptr_s = psum_tr.s(s)
                    for k in range(NK):
                        b.transpose(ptr_s[:, k, :], xnb.s(s)[:, k * 128:(k + 1) * 128], ident_b[:])
                    b.copy(xnT.s(gs)[:, :, j * 128:(j + 1) * 128], ptr_s, eng="act")
                return xnT.s(gs)
            return group

        def load_w_cols(Wb, stage, cols):
            c0 = 0
            for (a, z) in cols:
                n = z - a
                for k in range(NK):
                    sl = stage.s(k % 2)
                    b.load(sl[:, 0:n], w_in[k * 128:(k + 1) * 128, a:z])
                    b.ts(Wb[:, k, c0:c0 + n], sl[:, 0:n], g1[:, k:k + 1], None, op0=ALU.mult, eng="dve")
                c0 += n
        for dt in range(2):
            with ExitStack() as stg:
                b.stack = stg
                stg.callback(b.barrier)
                sfx = "g%d" % dt
                Wb = b.sbuf("Wb" + sfx, [128, NK, 800], BF16)
                wst = b.sbuf("wst" + sfx, [128, 2, 256], F32, nslots=2)
                load_w_cols(Wb, wst, [(dt * 128, dt * 128 + 128), (256 + dt * 128, 256 + dt * 128 + 128),
                                      (512 + dt * 256, 512 + dt * 256 + 256), (1024 + dt * 256, 1024 + dt * 256 + 256),
                                      (1536, 1552), (1552, 1568)])
                upf = b.sbuf("upf" + sfx, [16, 128], F32); upb = b.sbuf("upb" + sfx, [16, 128], F32)
                b.load(upf[:], up_f[:, dt * 128:(dt + 1) * 128]); b.load(upb[:], up_b[:, dt * 128:(dt + 1) * 128])
                nbias = b.sbuf("nbias" + sfx, [128, 2], F32)
                bst = b.sbuf("bst" + sfx, [128, 2], F32)
                bst2 = b.sbuf("bst2" + sfx, [128, 2], F32)
                b.load(bst[:], bias_f); b.load(bst2[:], bias_b)
                b.ts(nbias[:, 0:1], bst[:, dt:dt + 1], -1.0, None, op0=ALU.mult)
                b.ts(nbias[:, 1:2], bst2[:, dt:dt + 1], -1.0, None, op0=ALU.mult)
                ggain = b.sbuf("ggain" + sfx, [128, 128], F32)
                b.bcast_load(ggain[:], gla_gain)
                qef = b.sbuf("qef" + sfx, [128, T], BF16); kef = b.sbuf("kef" + sfx, [128, T], BF16)
                qeb = b.sbuf("qeb" + sfx, [128, T], BF16); keb = b.sbuf("keb" + sfx, [128, T], BF16)
                decf = b.sbuf("decf" + sfx, [128, NT], F32); decb = b.sbuf("decb" + sfx, [128, NT], F32)
                v_tm = b.sbuf("v_tm" + sfx, [128, NT, 256], BF16)
                G2 = b.sbuf("G2" + sfx, [128, NT, 256], BF16)
                zT = b.sbuf("zT" + sfx, [16, 2, 512], F32)
                qk32 = b.sbuf("qk32" + sfx, [128, 2, 512], F32)
                Pex = b.sbuf("Pex" + sfx, [128, 513], F32)
                Dd = b.sbuf("Dd" + sfx, [128, 512], F32)
                Ee = b.sbuf("Ee" + sfx, [128, 2, 512], F32)
                lap = b.sbuf("lap" + sfx, [128, 512], F32)
                gt = b.sbuf("gt" + sfx, [128, 3, 256], F32)
                with ExitStack() as stx:
                    b.stack = stx
                    stx.callback(b.barrier)
                    ptr = b.psum("ptr" + sfx, [128, 2, NK, 128], BF16, nslots=2)
                    pfm = b.psum("pfm" + sfx, [128, 2, 512], F32, nslots=2)
                    ptm = b.psum("ptm" + sfx, [128, 2, 512], F32, nslots=2)
                    pz = b.psum("pz" + sfx, [16, 2, 512], F32, nslots=2)
                    xgroup = xn_group_factory(sfx, ptr)
                    b.memset(Pex[:, 0:1], 0.0)
                    for g in range(NG):
                        xg = xgroup(g)
                        tok = slice(g * 512, (g + 1) * 512)
                        for qi in range(2):
                            for k in range(NK):
                                b.matmul(pfm.s(qi), Wb[:, k, qi * 128:(qi + 1) * 128], xg[:, k, :], start=(k == 0), stop=(k == NK - 1))
                            b.op("act", (lambda o_, i_, m_: (lambda e: e.mul(out=o_.ap, in_=i_.ap, mul=m_)))(qk32[:, qi, :], pfm.s(qi), (0.125 if qi == 0 else 1.0)), reads=[pfm.s(qi)], writes=[qk32[:, qi, :]])
                        for zi in range(2):
                            for k in range(NK):
                                b.matmul(pz.s(zi), Wb[:, k, 768 + zi * 16:768 + zi * 16 + 16], xg[:, k, :], start=(k == 0), stop=(k == NK - 1))
                            b.copy(zT[:, zi, :], pz.s(zi))
                        for di in range(2):
                            up = upf if di == 0 else upb
                            pl = pfm.s(di)
                            b.matmul(pl, up[:], zT[:, di, :])
                            b.act(lap[:], pl, AF.Exp, bias=nbias[:, di:di + 1], scale=-1.0)
                            b.act(lap[:], lap[:], AF.Ln, bias=1.0)
                            b.scan(Pex[:, 1:513], ones512[:], lap[:], 0.0, ALU.mult, ALU.add)
                            Pc = Pex[:, 1:513].re("p (n c) -> p n c", c=128)
                            Pe = Pex[:, 0:512].re("p (n c) -> p n c", c=128)
                            D3 = Dd[:].re("p (n c) -> p n c", c=128)
                            if di == 0:
                                b.tt(D3, Pc, Pe[:, :, 0:1].bc([128, 4, 128]), ALU.subtract)
                            else:
                                b.tt(D3, Pc[:, :, 127:128].bc([128, 4, 128]), Pe, ALU.subtract)
                            b.act(Ee[:, 0, :], Dd[:], AF.Exp, scale=-1.0 / 16.0)
                            b.act(Ee[:, 1, :], Dd[:], AF.Exp, scale=1.0 / 16.0)
                            qe, ke, dec = (qef, kef, decf) if di == 0 else (qeb, keb, decb)
                            b.tt(qe[:, tok], qk32[:, 0, :], Ee[:, 0, :], ALU.mult)
                            b.tt(ke[:, tok], qk32[:, 1, :], Ee[:, 1, :], ALU.mult)
                            E3 = Ee[:, 0, :].re("p (n c) -> p n c", c=128)
                            col = 127 if di == 0 else 0
                            b.copy(dec[:, g * 4:(g + 1) * 4].re("p (n o) -> p n o", o=1), E3[:, :, col:col + 1])
                        for j in range(4):
                            i = g * 4 + j
                            xl = [xg[:, k, j * 128:(j + 1) * 128] for k in range(NK)]
                            pv = ptm.s(0)
                            for k in range(NK):
                                b.matmul(pv[:, 0:256], xl[k], Wb[:, k, 256:512], start=(k == 0), stop=(k == NK - 1))
                            b.copy(v_tm[:, i, :], pv[:, 0:256], eng="act")
                            pg = ptm.s(1)
                            for k in range(NK):
                                b.matmul(pg[:, 0:256], xl[k], Wb[:, k, 512:768], start=(k == 0), stop=(k == NK - 1))
                            b.act(gt[:, 0, :], pg[:, 0:256], AF.Exp, scale=-1.0)
                            b.ts(gt[:, 0, :], gt[:, 0, :], 1.0, None, op0=ALU.add)
                            b.recip(gt[:, 1, :], gt[:, 0, :])
                            b.tt(gt[:, 2, :], gt[:, 1, :], pg[:, 0:256], ALU.mult)
                            b.tt(G2[:, i, :].re("p (h e) -> p h e", h=2), gt[:, 2, :].re("p (h e) -> p h e", h=2),
                                 ggain[:].re("p (o e) -> p o e", o=1).bc([128, 2, 128]), ALU.mult)
                b.mark('glaprep%d' % dt)
                with ExitStack() as stc:
                    b.stack = stc
                    stc.callback(b.barrier)
                    ketm = b.sbuf("ketm" + sfx, [128, 2, 128], BF16, nslots=2)
                    Sf = b.sbuf("Sf" + sfx, [128, NT, 256], BF16)
                    Sb = b.sbuf("Sb" + sfx, [128, 2, 256], BF16, nslots=2)
                    Tst = b.sbuf("Tst" + sfx, [128, 256], F32)
                    Am = b.sbuf("Am" + sfx, [128, 4, 128], BF16)
                    osb = b.sbuf("osb" + sfx, [128, 256], F32)
                    omx = b.sbuf("omx" + sfx, [128, 256], BF16)
                    jk = b.sbuf("jk" + sfx, [128, 128], BF16)
                    stt_ = b.sbuf("stt" + sfx, [128, 8], F32)
                    pkv_ = b.psum("pkv" + sfx, [128, 512], F32); pkv = pkv_[:, 0:256]
                    pa0 = b.psum("pa0" + sfx, [128, 4, 128], F32)
                    pa1 = b.psum("pa1" + sfx, [128, 4, 128], F32)
                    po_ = b.psum("po" + sfx, [128, 512], F32); po = po_[:, 0:256]
                    ptk = b.psum("ptk" + sfx, [128, 8, 128], BF16)
                    for n in range(NT):
                        ck = slice(n * 128, (n + 1) * 128)
                        if n >= 1:
                            b.stt(Sf[:, n, :], Tst[:], decf[:, n - 1:n], bdm[:], ALU.mult, ALU.mult)
                        if n == NT - 1:
                            break
                        b.transpose(ptk[:, 0, :], kef[:, ck], ident_b[:])
                        b.copy(ketm.s(n % 2), ptk[:, 0, :], eng="act")
                        b.matmul(pkv, ketm.s(n % 2), v_tm[:, n, :])
                        if n == 0:
                            b.copy(Tst[:], pkv)
                        else:
                            b.stt(Tst[:], Tst[:], decf[:, n - 1:n], pkv, ALU.mult, ALU.add)
                    for n in range(NT - 1, -1, -1):
                        ck = slice(n * 128, (n + 1) * 128)
                        sbc = Sb.s(n % 2)
                        if n < NT - 1:
                            b.stt(sbc, Tst[:], decb[:, n + 1:n + 2], bdm[:], ALU.mult, ALU.mult)
                        for hl in range(2):
                            pr = slice(hl * 64, (hl + 1) * 64)
                            pah = pa0 if hl == 0 else pa1
                            b.matmul(pah[:, 0, :], kef[pr, ck], qef[pr, ck])
                            b.matmul(pah[:, 1, :], keb[pr, ck], qeb[pr, ck])
                        for hl in range(2):
                            pah = pa0 if hl == 0 else pa1
                            b.tt(Am[:, 2 * hl:2 * hl + 2, :].re("p a c -> p (a c)"), pah[:, 0:2, :].re("p a c -> p (a c)"), mask4[:, 128:384], ALU.mult)
                        for hl in range(2):
                            pr = slice(hl * 64, (hl + 1) * 64)
                            es = slice(hl * 128, (hl + 1) * 128)
                            mms = [(Am[:, 2 * hl, :], v_tm[:, n, es]), (Am[:, 2 * hl + 1, :], v_tm[:, n, es])]
                            if n > 0:
                                mms.append((qef[:, ck], Sf[:, n, es]))
                            if n < NT - 1:
                                mms.append((qeb[:, ck], sbc[:, es]))
                            for mi, (l_, r_) in enumerate(mms):
                                b.matmul(po[:, es], l_, r_, start=(mi == 0), stop=(mi == len(mms) - 1))
                        if n > 0:
                            b.transpose(ptk[:, 1, :], keb[:, ck], ident_b[:])
                            b.copy(ketm.s(n % 2), ptk[:, 1, :], eng="act")
                            b.matmul(pkv, ketm.s(n % 2), v_tm[:, n, :])
                            if n == NT - 1:
                                b.copy(Tst[:], pkv)
                            else:
                                b.stt(Tst[:], Tst[:], decb[:, n + 1:n + 2], pkv, ALU.mult, ALU.add)
                        for hl in range(2):
                            es = slice(hl * 128, (hl + 1) * 128)
                            b.act(jk[:], po[:, es], AF.Square, accum_out=stt_[:, hl:hl + 1])
                        b.act(stt_[:, 2:4], stt_[:, 0:2], AF.Ln, bias=EPS, scale=1.0 / 128)
                        b.act(stt_[:, 4:6], stt_[:, 2:4], AF.Exp, scale=-0.5)
                        b.tt(osb[:].re("p (h e) -> p h e", h=2), po.re("p (h e) -> p h e", h=2),
                             stt_[:, 4:6].re("p (h o) -> p h o", o=1).bc([128, 2, 128]), ALU.mult)
                        b.tt(omx[:], osb[:], G2[:, n, :], ALU.mult)
                        for hl in range(2):
                            b.transpose(ptk[:, hl, :], omx[:, hl * 128:(hl + 1) * 128], ident_b[:])
                        b.copy(mixg[:, 2 * dt:2 * dt + 2, ck], ptk[:, 0:2, :])
            b.stack = STK
            b.mark('gla%d' % dt)
        with ExitStack() as sta:
            b.stack = sta
            sta.callback(b.barrier)
            mixa = b.sbuf("mixa", [128, 4, T], BF16)
            qT = b.sbuf("qT_att", [128, 4, T], BF16)
            kT = b.sbuf("kT_att", [128, T], BF16)
            Vaug = b.sbuf("Vaug", [128, NT, 2, 65], BF16)
            b.memset(Vaug[:, :, :, 64:65], 1.0)
            qg = b.sbuf("qg_row", [128, 64], F32); kg = b.sbuf("kg_row", [128, 64], F32)
            b.bcast_load(qg[:], q_gain)
            b.bcast_load(kg[:], k_gain)
            b.ts(qg[:], qg[:], 0.125, None, op0=ALU.mult)
            agr = b.sbuf("agr", [128, 512], F32)
            b.bcast_load(agr[:], att_gain)
            cosb = b.sbuf("cosb", [128, NT, 32], F32); sinb = b.sbuf("sinb", [128, NT, 32], F32)
            b.load(cosb[:], c_cos); b.load(sinb[:], c_sin)
            with ExitStack() as stx:
                b.stack = stx
                stx.callback(b.barrier)
                ptr = b.psum("ptr_a", [128, 2, NK, 128], BF16, nslots=2)
                ptm = b.psum("ptm_a", [128, 2, 512], F32, nslots=2)
                ptq = b.psum("ptq_a", [128, 8, 128], BF16)
                Wb = b.sbuf("Wb_a", [128, NK, 768], BF16)
                wst = b.sbuf("wst_a", [128, 2, 512], F32, nslots=2)
                load_w_cols(Wb, wst, [(1568, 2080), (2080, 2336)])
                sq = b.sbuf("sq_a", [128, 512], F32)
                s8 = b.sbuf("s8_a", [128, 24], F32)
                qn = b.sbuf("qn_a", [128, 512], F32)
                t1 = b.sbuf("t1_a", [128, 256], F32); t2 = b.sbuf("t2_a", [128, 256], F32)
                qr = b.sbuf("qr_a", [128, 512], BF16)
                kr = b.sbuf("kr_a", [128, 128], BF16)
                xgroup = xn_group_factory("a", ptr)

                def norm_rope(src_ps, hd, grow, i, dst_even, dst_odd):
                    nh = 1
                    for z_ in hd:
                        nh *= z_
                    w = nh * 64
                    b.act(sq[:, 0:w], src_ps, AF.Square)
                    b.reduce(s8[:, 0:nh], sq[:, 0:w].re("p (h d) -> p h d", d=64), ALU.add)
                    b.act(s8[:, 8:8 + nh], s8[:, 0:nh], AF.Ln, bias=EPS, scale=1.0 / 64)
                    b.act(s8[:, 16:16 + nh], s8[:, 8:8 + nh], AF.Exp, scale=-0.5)
                    b.tt(qn[:, 0:w].re("p (h d) -> p h d", d=64), src_ps.re("p (h d) -> p h d", d=64),
                         s8[:, 16:16 + nh].re("p (h o) -> p h o", o=1).bc([128, nh, 64]), ALU.mult)
                    b.tt(qn[:, 0:w].re("p (h d) -> p h d", d=64), qn[:, 0:w].re("p (h d) -> p h d", d=64),
                         grow[:].re("p (o d) -> p o d", o=1).bc([128, nh, 64]), ALU.mult)
                    if len(hd) == 2:
                        pat = "p (k g i two) -> p k g i two"; kw = dict(k=hd[0], g=hd[1], i=32, two=2)
                        pat3 = "p (k g i) -> p k g i"; kw3 = dict(k=hd[0], g=hd[1], i=32)
                        patc = "p (a c i) -> p a c i"; kwc = dict(a=1, c=1)
                        shp = [128, hd[0], hd[1], 32]
                        q4 = qn[:, 0:w].re(pat, **kw)
                        x0 = q4[:, :, :, :, 0]; x1 = q4[:, :, :, :, 1]
                    else:
                        pat = "p (k i two) -> p k i two"; kw = dict(k=hd[0], i=32, two=2)
                        pat3 = "p (k i) -> p k i"; kw3 = dict(k=hd[0], i=32)
                        patc = "p (a i) -> p a i"; kwc = dict(a=1)
                        shp = [128, hd[0], 32]
                        q4 = qn[:, 0:w].re(pat, **kw)
                        x0 = q4[:, :, :, 0]; x1 = q4[:, :, :, 1]
                    cb = cosb[:, i, :].re(patc, **kwc).bc(shp)
                    sb_ = sinb[:, i, :].re(patc, **kwc).bc(shp)
                    hw = nh * 32
                    a1 = t1[:, 0:hw].re(pat3, **kw3); a2 = t2[:, 0:hw].re(pat3, **kw3)
                    b.tt(a1, x0, cb, ALU.mult); b.tt(a2, x1, sb_, ALU.mult)
                    b.tt(dst_even, a1, a2, ALU.subtract)
                    b.tt(a1, x0, sb_, ALU.mult); b.tt(a2, x1, cb, ALU.mult)
                    b.tt(dst_odd, a1, a2, ALU.add)

                for g in range(NG):
                    xg = xgroup(g)
                    for j in range(4):
                        i = g * 4 + j
                        tk = slice(i * 128, (i + 1) * 128)
                        xl = [xg[:, k, j * 128:(j + 1) * 128] for k in range(NK)]
                        pq = ptm.s(0)
                        for k in range(NK):
                            b.matmul(pq, xl[k], Wb[:, k, 0:512], start=(k == 0), stop=(k == NK - 1))
                        pk = ptm.s(1)
                        for k in range(NK):
                            b.matmul(pk[:, 0:256], xl[k], Wb[:, k, 512:768], start=(k == 0), stop=(k == NK - 1))
                        qr5 = qr[:].re("p (g k i two) -> p k g i two", g=4, k=2, i=32, two=2)
                        norm_rope(pq, (2, 4), qg, i, qr5[:, :, :, :, 0], qr5[:, :, :, :, 1])
                        for g4 in range(4):
                            b.transpose(ptq[:, g4, :], qr[:, g4 * 128:(g4 + 1) * 128], ident_b[:])
                        b.copy(qT[:, :, tk], ptq[:, 0:4, :], eng="act")
                        kr4 = kr[:].re("p (h i two) -> p h i two", i=32, two=2)
                        norm_rope(pk[:, 0:128], (2,), kg, i, kr4[:, :, :, 0], kr4[:, :, :, 1])
                        b.transpose(ptq[:, 4, :], kr[:], ident_b[:])
                        b.copy(kT[:, tk], ptq[:, 4, :], eng="act")
                        b.copy(Vaug[:, i, :, 0:64], pk[:, 128:256].re("p (h d) -> p h d", d=64), eng="act")
            b.mark('attproj')
            for blk in range(NB * SUB):
                b.load(xs_d[blk * 128:(blk + 1) * 128, :], zeros_b[:], q="sp")
            with ExitStack() as stx:
                b.stack = stx
                stx.callback(b.barrier)
                ps = b.psum("ps_att", [128, 4, 512], F32, nslots=4)
                pov = b.psum("po_att", [128, 2, 512], F32, nslots=2)
                ptk = b.psum("ptk_att", [128, 8, 128], BF16)
                Eb = b.sbuf("Eb", [128, 4, 512], BF16, nslots=4)
                otm = b.sbuf("otm", [128, 4, 512], F32)
                rl = b.sbuf("rl", [128, 2, 4], F32, nslots=2)
                st8 = b.sbuf("st8", [128, 4], F32)
                jk = b.sbuf("jk_att", [128, 512], BF16)
                on = b.sbuf("on_att", [128, 512], F32)
                ob = b.sbuf("ob_att", [128, 512], BF16)
                steps = []
                for qc in range(NG):
                    for g4 in range(4):
                        for s_ in range(NT):
                            steps.append((qc, g4, s_))

                def emit_s(idx):
                    qc, g4, s_ = steps[idx]
                    ees = []
                    pss2 = []
                    for kv in range(2):
                        pr = slice(kv * 64, (kv + 1) * 64)
                        pss = ps.s(kv * 2 + idx % 2)
                        b.matmul(pss, kT[pr, s_ * 128:(s_ + 1) * 128], qT[pr, g4, qc * 512:(qc + 1) * 512])
                        pss2.append(pss)
                    for kv in range(2):
                        ee = Eb.s((idx % 2) * 2 + kv)
                        b.act(ee, pss2[kv], AF.Exp)
                        ees.append(ee)
                    return ees

                def emit_pv(idx, ees):
                  qc, g4, s_ = steps[idx]
                  for kv in range(2):
                    ee = ees[kv]
                    h = kv * 4 + g4
                    po_ = pov.s(kv)
                    po3 = po_[:, 0:260].re("p (j e) -> p j e", e=65)
                    for j in range(4):
                        b.matmul(po3[:, j, :], ee[:, j * 128:(j + 1) * 128], Vaug[:, s_, kv, :],
                                 start=(s_ == 0 and j == 0), stop=(s_ == NT - 1 and j == 3))
                    if s_ == NT - 1:
                        rr = rl.s(kv)
                        b.recip(rr.re("p (j o) -> p j o", o=1), po3[:, :, 64:65])
                        b.tt(otm[:].re("p j (h d) -> p j h d", d=64)[:, :, h, :], po3[:, :, 0:64],
                             rr.re("p (j o) -> p j o", o=1).bc([128, 4, 64]), ALU.mult)
                        if g4 == 3 and kv == 1:
                            for j in range(4):
                                i = qc * 4 + j
                                tk = slice(i * 128, (i + 1) * 128)
                                b.act(jk[:], otm[:, j, :], AF.Square, accum_out=st8[:, 0:1])
                                b.act(st8[:, 1:2], st8[:, 0:1], AF.Ln, bias=EPS, scale=1.0 / 512)
                                b.act(st8[:, 2:3], st8[:, 1:2], AF.Exp, scale=-0.5)
                                b.ts(on[:], otm[:, j, :], st8[:, 2:3], None, op0=ALU.mult)
                                b.tt(ob[:], on[:], agr[:], ALU.mult)
                                for c in range(4):
                                    b.transpose(ptk[:, c, :], ob[:, c * 128:(c + 1) * 128], ident_b[:])
                                b.copy(mixa[:, :, tk], ptk[:, 0:4, :])

                pend = None
                for idx in range(len(steps)):
                    ee = emit_s(idx)
                    if pend is not None:
                        emit_pv(*pend)
                    pend = (idx, ee)
                emit_pv(*pend)
            with ExitStack() as std:
                b.stack = std
                std.callback(b.barrier)
                Wo = b.sbuf("Wo", [128, NK, D], BF16)
                wst = b.sbuf("wst_o", [128, 2, 512], F32, nslots=2)
                for k in range(NK):
                    for hf in range(2):
                        b.load(wst.s(hf), w_out[k * 128:(k + 1) * 128, hf * 512:(hf + 1) * 512])
                        b.copy(Wo[:, k, hf * 512:(hf + 1) * 512], wst.s(hf), eng=("dve" if hf == 0 else "act"))
                g2r = b.sbuf("g2r", [128, D], F32)
                b.bcast_load(g2r[:], g2)
                Wr = b.sbuf("Wr", [128, NK, 72], F32)
                with nc.allow_non_contiguous_dma(reason="small router weights"):
                    for k in range(NK):
                        b.load(Wr[:, k, 0:8], w_group[k * 128:(k + 1) * 128, :])
                        b.load(Wr[:, k, 8:72], w_expert[k * 128:(k + 1) * 128, :])
                brow = b.sbuf("brow", [1, 72], F32)
                b.load(brow[:, 0:8], b_group.re("(o n) -> o n", o=1)); b.load(brow[:, 8:72], b_expert.re("(o n) -> o n", o=1))
                xn2b = b.sbuf("xn2b", [128, 2, D], BF16, nslots=2)
                mp = b.sbuf("mp", [128, 2, 192], F32, nslots=2)
                xt = b.sbuf("xt_o", [128, 2, D], F32, nslots=2)
                h1 = b.sbuf("h1_o", [128, 2, D], F32, nslots=2)
                xn2_2 = b.sbuf("xn2_o", [128, 2, D], F32, nslots=2)
                xn2T_2 = b.sbuf("xn2T_o", [128, 2, NK, 128], F32, nslots=2)
                jk_1 = b.sbuf("jk_o", [128, D], BF16)
                sm_2 = b.sbuf("sm_o", [128, 2, 32], F32, nslots=2)
                lg_2 = b.sbuf("lg_o", [128, 2, 72], F32, nslots=2)
                m8_2 = b.sbuf("m8_o", [128, 2, 16], F32, nslots=2)
                og_2 = b.sbuf("og_o", [128, 2, 8], F32, nslots=2)
                tmp8_2 = b.sbuf("tmp8_o", [128, 2, 8], F32, nslots=2)
                ge_2 = b.sbuf("ge_o", [128, 2, 8], F32, nslots=2)
                ml_2 = b.sbuf("ml_o", [128, 2, 64], F32, nslots=2)
                Mb_2 = b.sbuf("Mb_o", [128, 2, 64], BF16, nslots=2)
                py4 = b.psum("py_o", [128, 4, 512], F32, nslots=4)
                ptf = b.psum("ptf_o", [128, 2, 512], F32, nslots=2)
                plg = b.psum("plg_o", [128, 512], F32)
                pps = b.psum("pps_o", [128, 512], F32)
                b.load(xt.s(0), x[0:128, :])
                for i in range(NT):
                    tk = slice(i * 128, (i + 1) * 128)
                    s = i % 2
                    xn2 = xn2_2.s(s); xn2T = xn2T_2.s(s); jk = jk_1[:]; sm = sm_2.s(s); lg = lg_2.s(s); m8 = m8_2.s(s)
                    og = og_2.s(s); tmp8 = tmp8_2.s(s); ge = ge_2.s(s); ml = ml_2.s(s); Mb = Mb_2.s(s)
                    if i + 1 < NT:
                        b.load(xt.s((i + 1) % 2), x[(i + 1) * 128:(i + 2) * 128, :])
                    for hf in range(2):
                        for c in range(NK):
                            b.matmul(py4.s(2 * s + hf), (mixg if c < 4 else mixa)[:, c % 4, tk], Wo[:, c, hf * 512:(hf + 1) * 512], start=(c == 0), stop=(c == NK - 1))
                        b.tt(h1.s(s)[:, hf * 512:(hf + 1) * 512], py4.s(2 * s + hf), xt.s(s)[:, hf * 512:(hf + 1) * 512], ALU.add)
                    b.load(h1_d[tk, :], h1.s(s))
                    b.act(jk, h1.s(s), AF.Square, accum_out=sm[:, 0:1])
                    b.act(sm[:, 1:2], sm[:, 0:1], AF.Ln, bias=EPS, scale=1.0 / D)
                    b.act(sm[:, 2:3], sm[:, 1:2], AF.Exp, scale=-0.5)
                    b.stt(xn2, h1.s(s), sm[:, 2:3], g2r[:], ALU.mult, ALU.mult)
                    b.copy(xn2b.s(s).re("t (c p) -> t c p", c=NK), xn2.re("t (p c) -> t c p", c=NK), eng="act")
                    b.load(xn2_d[tk, :], xn2b.s(s))
                    for c in range(NK):
                        b.transpose(ptf.s(c // 4)[:, (c % 4) * 128:(c % 4 + 1) * 128], xn2[:, c * 128:(c + 1) * 128], ident_f[:])
                    b.copy(xn2T[:, 0:4, :].re("p c t -> p (c t)"), ptf.s(0), eng="act")
                    b.copy(xn2T[:, 4:8, :].re("p c t -> p (c t)"), ptf.s(1))
                    for c in range(NK):
                        b.matmul(plg[:, 0:72], xn2T[:, c, :], Wr[:, c, :], start=(c == 0), stop=False)
                    b.matmul(plg[:, 0:72], ones_f[0:1, :], brow[:], start=False, stop=True)
                    b.copy(lg, plg[:, 0:72])
                    b.max8(m8[:, 0:8], lg[:, 0:8])
                    b.ts(og, lg[:, 0:8], m8[:, 0:1], None, op0=ALU.is_equal)
                    b.ts(sm[:, 3:4], m8[:, 0:1], -1.0, None, op0=ALU.mult)
                    b.act(ge, lg[:, 0:8], AF.Exp, bias=sm[:, 3:4], scale=1.0, accum_out=sm[:, 4:5])
                    b.recip(sm[:, 5:6], sm[:, 4:5])
                    b.ts(tmp8, og, BIG, -BIG, op0=ALU.mult, op1=ALU.add)
                    b.tt(ml.re("p (g j) -> p g j", j=8), lg[:, 8:72].re("p (g j) -> p g j", j=8),
                         tmp8.re("p (g o) -> p g o", o=1).bc([128, 8, 8]), ALU.add)
                    b.max8(m8[:, 8:16], ml)
                    b.ts(mp.s(s)[:, 0:64], ml, m8[:, 8:9], None, op0=ALU.is_equal)
                    b.ts(mp.s(s)[:, 64:128], ml, m8[:, 9:10], None, op0=ALU.is_equal)
                    b.tt(sm[:, 6:7], m8[:, 9:10], m8[:, 8:9], ALU.subtract)
                    b.act(sm[:, 7:8], sm[:, 6:7], AF.Exp)
                    b.ts(sm[:, 8:9], sm[:, 7:8], 1.0, None, op0=ALU.add)
                    b.recip(sm[:, 9:10], sm[:, 8:9])
                    b.tt(sm[:, 10:11], sm[:, 7:8], sm[:, 9:10], ALU.mult)
                    b.tt(gates[:, i, 0:1], sm[:, 9:10], sm[:, 5:6], ALU.mult)
                    b.tt(gates[:, i, 1:2], sm[:, 10:11], sm[:, 5:6], ALU.mult)
                    b.tt(Mb, mp.s(s)[:, 0:64], mp.s(s)[:, 64:128], ALU.add)
                    b.matmul(pps[:, 0:64], lstrict_b[:], Mb)
                    b.matmul(pps[:, 64:128], ones_b[:], Mb)
                    b.tt(mp.s(s)[:, 128:192], pps[:, 0:64], run[:], ALU.add)
                    b.tt(run[:], pps[:, 64:128], run[:], ALU.add)
                    b.load(mp_d[:, i * 192:(i + 1) * 192], mp.s(s))
        b.mark('router')
        stS1.__exit__(None, None, None)
        b.barrier()
        b.stack = st0
        with ExitStack() as stl:
            b.stack = stl
            stl.callback(b.barrier)
            mpa = b.sbuf("mpa", [128, NT, 192], F32)
            b.load(mpa[:].re("p n c -> p (n c)"), mp_d)
            cmpA = b.sbuf("cmpA", [128, 64, NBMAX], BF16)
            nblk = b.sbuf("nblk", [128, 64], F32)
            pend = b.sbuf("pend", [128, 64], F32)
            pstart = b.sbuf("pstart", [128, 64], F32)
            ones64 = b.sbuf("ones64", [128, 64], F32)
            b.memset(ones64[:], 1.0)
            b.tt(cmpA[:], run[:].re("p (e o) -> p e o", o=1).bc([128, 64, NBMAX]),
                 thr[:, 0:NBMAX].re("p (o n) -> p o n", o=1).bc([128, 64, NBMAX]), ALU.is_gt)
            b.reduce(nblk[:], cmpA[:], ALU.add)
            b.ts(nblk[:], nblk[:], float(RB), None, op0=ALU.mult)
            b.scan(pend[:], ones64[:], nblk[:], 0.0, ALU.mult, ALU.add)
            b.tt(pstart[:], pend[:], nblk[:], ALU.subtract)
            cmpB = b.sbuf("cmpB", [128, NB, 64], BF16)
            bef = b.sbuf("bef", [128, NB], F32)
            b.tt(cmpB[:], pend[:].re("p (o e) -> p o e", o=1).bc([128, NB, 64]),
                 thr[:].re("p (n o) -> p n o", o=1).bc([128, NB, 64]), ALU.is_le)
            b.reduce(bef[:], cmpB[:], ALU.add)
            if debug:
                b.load(dbg_be, bef[:])
            b.ts(bef[:], bef[:], 128.0, pidx[:, 0:1], op0=ALU.mult, op1=ALU.add)
            b.copy(idxw_i[:], bef[:])
            destf = b.sbuf("destf", [128, NT, 2], F32)
            tqa = b.sbuf("tqa", [128, NT, 64], F32); tqb = b.sbuf("tqb", [128, NT, 64], F32)
            b.tt(tqa[:], mpa[:, :, 128:192], pstart[:].re("p (o e) -> p o e", o=1).bc([128, NT, 64]), ALU.add)
            for k2 in range(2):
                b.tt(tqb[:], tqa[:], mpa[:, :, k2 * 64:(k2 + 1) * 64], ALU.mult)
                b.reduce(destf[:, :, k2:k2 + 1].re("p n o -> p (n o)"), tqb[:], ALU.add)
            b.copy(dest_i[:], destf[:])
            if debug:
                dr = b.sbuf("dr", [128, NT, 4], F32)
                b.copy(dr[:, :, 0:2], destf[:]); b.copy(dr[:, :, 2:4], gates[:])
                b.load(dbg_route, dr[:])
            b.mark('layout')
            xr = b.sbuf("xr", [128, 2, D], BF16, nslots=2)
            b.load(xr.s(0), xn2_d[0:128, :])
            for i in range(NT):
                if i + 1 < NT:
                    b.load(xr.s((i + 1) % 2), xn2_d[(i + 1) * 128:(i + 2) * 128, :])
                for k2 in range(2):
                    b.scatter(xs_d, xr.s(i % 2), dest_i[:, i, k2:k2 + 1])
        b.stack = st0

        b.mark('scatter')
        with ExitStack() as stm:
            b.stack = stm
            stm.callback(b.barrier)
            wstg = b.sbuf("wstg", [128, 4, 4096], F32, nslots=4)
            wgb = b.sbuf("wgb", [128, 2, NK, 512], BF16, nslots=2)
            wub = b.sbuf("wub", [128, 2, NK, 512], BF16, nslots=2)
            wdb = b.sbuf("wdb", [128, 2, 4, D], BF16, nslots=2)
            xsb = b.sbuf("xsb", [128, 2, D], BF16, nslots=2)
            xsT = b.sbuf("xsT", [128, 2, NK, 128], BF16, nslots=2)
            hh = b.sbuf("hh", [128, 2, 512], BF16, nslots=2)
            hT = b.sbuf("hT", [128, 4, 128], BF16)
            et = b.sbuf("et", [128, 2, 3, 512], F32, nslots=2)
            ysb = b.sbuf("ysb", [128, 2, D], F32, nslots=2)
            ptx = b.psum("ptx", [128, NK, 128], BF16)
            pth = b.psum("pth", [128, NK, 128], BF16)
            pg2 = b.psum("pg_m", [128, 2, 512], F32, nslots=2)
            pu2 = b.psum("pu_m", [128, 2, 512], F32, nslots=2)
            pyy = b.psum("pyy", [128, 2, 512], F32, nslots=2)
            wg_v = w_gate.re("e (p c) f -> (e p) (c f)", c=NK)
            wu_v = w_up.re("e (p c) f -> (e p) (c f)", c=NK)
            wd_v = w_down.re("e (p c) d -> (e p) (c d)", c=4)
            sc = [0]

            order = []
            lo, hi = 0, NB - 1
            while lo <= hi:
                order.append(lo); lo += 1
                if lo <= hi:
                    order.append(hi); hi -= 1
            seq = [(slot, sub) for slot in order for sub in range(SUB)]
            NSEQ = len(seq)

            def fetch_w(pos):
                slot = order[pos]
                ws = pos % 2
                for (wv, dstb, eng) in ((wg_v, wgb, "act"), (wu_v, wub, "dve"), (wd_v, wdb, "mix")):
                    sl = wstg.s(sc[0] % 4); sc[0] += 1
                    b.gather(sl, wv, idxw_i[:, slot:slot + 1], bounds=64 * 128 - 1)
                    dv = dstb.s(ws).re("p a f -> p (a f)")
                    if eng == "mix":
                        b.copy(dv[:, 0:2048], sl[:, 0:2048], eng="dve")
                        b.copy(dv[:, 2048:4096], sl[:, 2048:4096], eng="dve")
                    else:
                        b.copy(dv, sl, eng=eng)

            def blk_of(q):
                slot, sub = seq[q]
                return slot * SUB + sub

            def fetch_x(q):
                blk = blk_of(q)
                b.load(xsb.s(q % 2), xs_d[blk * 128:(blk + 1) * 128, :])

            def stage1(q):
                ws = (q // SUB) % 2
                p2 = q % 2
                if q + 1 < NSEQ:
                    fetch_x(q + 1)
                pg = pg2.s(p2); pu = pu2.s(p2); et_ = et.s(p2); xT = xsT.s(p2)
                for c in range(NK):
                    b.transpose(ptx[:, c, :], xsb.s(p2)[:, c * 128:(c + 1) * 128], ident_b[:])
                b.copy(xT, ptx[:], eng="act")
                for c in range(NK):
                    b.matmul(pg, xT[:, c, :], wgb.s(ws)[:, c, :], start=(c == 0), stop=(c == NK - 1))
                for c in range(NK):
                    b.matmul(pu, xT[:, c, :], wub.s(ws)[:, c, :], start=(c == 0), stop=(c == NK - 1))
                b.act(et_[:, 2, :], pg, AF.Silu)
                b.tt(hh.s(p2).re("s (c p) -> s c p", c=4), et_[:, 2, :].re("s (p c) -> s c p", c=4), pu.re("s (p c) -> s c p", c=4), ALU.mult)

            def stage2(q):
                blk = blk_of(q)
                ws = (q // SUB) % 2
                p2 = q % 2
                for c in range(4):
                    b.transpose(pth[:, c, :], hh.s(p2)[:, c * 128:(c + 1) * 128], ident_b[:])
                b.copy(hT[:], pth[:, 0:4, :], eng="act")
                for hf in range(2):
                    for c in range(4):
                        b.matmul(pyy.s(hf), hT[:, c, :], wdb.s(ws)[:, c, hf * 512:(hf + 1) * 512], start=(c == 0), stop=(c == 3))
                b.copy(ysb.s(p2)[:, 0:512], pyy.s(0), eng="act")
                b.copy(ysb.s(p2)[:, 512:1024], pyy.s(1))
                b.load(ys_d[blk * 128:(blk + 1) * 128, :], ysb.s(p2))

            fetch_w(0)
            fetch_x(0)
            stage1(0)
            for q in range(NSEQ):
                if q % SUB == 0 and q // SUB + 1 < NB:
                    fetch_w(q // SUB + 1)
                if q + 1 < NSEQ:
                    stage1(q + 1)
                stage2(q)
        b.stack = st0

        b.mark('moe')
        with ExitStack() as stf:
            b.stack = stf
            fgr = b.sbuf("fgr", [128, D], F32)
            b.bcast_load(fgr[:], fgain)
            y1 = b.sbuf("y1", [128, 2, D], F32, nslots=2); y2 = b.sbuf("y2", [128, 2, D], F32, nslots=2)
            hh1 = b.sbuf("hh1", [128, 2, D], F32, nslots=2)
            acc = b.sbuf("acc", [128, D], F32); acc2 = b.sbuf("acc2", [128, 2, D], F32, nslots=2)
            ot = b.sbuf("ot", [128, 2, D], F32, nslots=2)
            jk = b.sbuf("jk_f", [128, D], BF16)
            sf = b.sbuf("sf", [128, 2, 4], F32, nslots=2)
            outs = []

            def fetch_f(i):
                s = i % 2
                b.gather(y1.s(s), ys_d, dest_i[:, i, 0:1])
                b.gather(y2.s(s), ys_d, dest_i[:, i, 1:2])
                b.load(hh1.s(s), h1_d[i * 128:(i + 1) * 128, :])

            def combine(i):
                s = i % 2
                if i + 1 < NT:
                    fetch_f(i + 1)
                b.stt(acc[:], y1.s(s), gates[:, i, 0:1], hh1.s(s), ALU.mult, ALU.add)
                b.stt(acc2.s(s), y2.s(s), gates[:, i, 1:2], acc[:], ALU.mult, ALU.add)
                sfs = sf.s(s)
                b.act(jk[:], acc2.s(s), AF.Square, accum_out=sfs[:, 0:1])
                b.act(sfs[:, 1:2], sfs[:, 0:1], AF.Ln, bias=EPS, scale=1.0 / D)
                b.act(sfs[:, 2:3], sfs[:, 1:2], AF.Exp, scale=-0.5)

            def finish(i):
                s = i % 2
                b.stt(ot.s(s), acc2.s(s), sf.s(s)[:, 2:3], fgr[:], ALU.mult, ALU.mult)
                outs.append(b.load(out[i * 128:(i + 1) * 128, :], ot.s(s)))

            fetch_f(0)
            combine(0)
            for i in range(NT):
                if i + 1 < NT:
                    combine(i + 1)
                finish(i)
            b.wait_all("sp", outs)

        def tail(bb):
            bb.waited = {e: {} for e in bb.prog}
            bb.latest = {}
            bb.lastw = {}
            bb.readers = {}
            for e, lst in bb.prog.items():
                for idx, it in enumerate(lst):
                    if it[0] == "dma":
                        bb.latest[it[3]] = max(bb.latest.get(it[3], 0), 0)
            toks = []
            cnt = {}
            for e, lst in bb.prog.items():
                for idx, it in enumerate(lst):
                    if it[0] == "dma":
                        cnt[it[3]] = cnt.get(it[3], 0) + 16
                        bb.latest[it[3]] = cnt[it[3]]
                    elif it[2] is not None and e in COMPUTE:
                        bb.latest[("e", e)] = idx
            for q in bb.dma_cnt:
                for i in range(bb.n_dma):
                    bb.dma_cnt[q][i] = cnt.get(("d", q, i), 0) // 16
            bb.barrier()
            for i in range(NT):
                toks.append(bb.load(out[i * 128:(i + 1) * 128, :], zeros_f[:]))
            bb.wait_all("sp", toks)
        b.emit(trunc=trunc, tail=tail)
    return nc


GRID_W = 64
ROPE_THETA = 10000.0
_NC_CACHE = {}


def _consts(T):
    NT = T // 128
    RB = 256
    NB = 2 * T // RB + 63
    s = np.arange(128)[:, None]
    c = np.arange(128)[None, :]
    maskU = (s <= c).astype(np.float32)
    maskL = (s >= c).astype(np.float32)
    t = np.arange(T)
    row = (t // GRID_W).astype(np.float32)
    col = (t % GRID_W).astype(np.float32)
    axis_dim = 32
    inv_freq = (ROPE_THETA ** (-np.arange(0, axis_dim, 2, dtype=np.float32) / axis_dim)).astype(np.float32)
    ang = np.concatenate([row[:, None] * inv_freq, col[:, None] * inv_freq], axis=-1).astype(np.float32)
    cos = np.cos(ang).astype(np.float32).reshape(NT, 128, 32).transpose(1, 0, 2)
    sin = np.sin(ang).astype(np.float32).reshape(NT, 128, 32).transpose(1, 0, 2)
    return dict(
        c_ident=np.eye(128, dtype=np.float32),
        c_mask4=np.ascontiguousarray(np.concatenate([maskU, maskU, maskL, maskL], axis=1)),
        c_lstrict=(s < c).astype(np.float32),
        c_cos=np.ascontiguousarray(cos), c_sin=np.ascontiguousarray(sin),
        c_pidx=np.arange(128, dtype=np.float32).reshape(128, 1),
        c_thr=np.ascontiguousarray(np.broadcast_to((float(RB) * np.arange(NB, dtype=np.float32))[None, :], (128, NB))),
        c_bd=np.ascontiguousarray(((np.arange(128)[:, None] // 64) == (np.arange(256)[None, :] // 128)).astype(np.float32)),
    )


def _shared_inputs(T, norm1_gain, w_in, gla_up_fwd, gla_up_fwd_bias, gla_up_bwd, gla_up_bwd_bias, gla_out_gain,
                   q_norm_gain, k_norm_gain, att_out_gain, w_out, norm2_gain, w_group, b_group, w_expert, b_expert,
                   w_gate, w_up, w_down, final_gain):
    f = lambda a: np.ascontiguousarray(np.asarray(a, dtype=np.float32))
    d = dict(
        w_in=f(w_in[0]), g1pk=f(np.asarray(norm1_gain[0]).reshape(8, 128).T),
        up_f=f(gla_up_fwd[0]), up_b=f(gla_up_bwd[0]),
        bias_f=f(np.asarray(gla_up_fwd_bias[0]).reshape(2, 128).T), bias_b=f(np.asarray(gla_up_bwd_bias[0]).reshape(2, 128).T),
        gla_gain=f(gla_out_gain[0]), q_gain=f(q_norm_gain[0]), k_gain=f(k_norm_gain[0]), att_gain=f(att_out_gain[0]),
        w_out=f(w_out[0]), g2=f(norm2_gain[0]), w_group=f(w_group[0]), b_group=f(b_group[0]),
        w_expert=f(w_expert[0]), b_expert=f(b_expert[0]),
        w_gate=f(w_gate[0]), w_up=f(w_up[0]), w_down=f(w_down[0]), fgain=f(final_gain),
    )
    d.update(_consts(T))
    return d


def kernel(x, **params):
    x = np.asarray(x, dtype=np.float32)
    B, T, _ = x.shape
    if T not in _NC_CACHE:
        _NC_CACHE[T] = build(T)
    nc = _NC_CACHE[T]
    shared = _shared_inputs(T, **params)
    in_maps = []
    for bi in range(B):
        m = dict(shared)
        m["x"] = np.ascontiguousarray(x[bi])
        in_maps.append(m)
    res = run_bass_kernel_spmd(nc, in_maps, core_ids=list(range(B)))
    return np.stack([np.asarray(r["out"], dtype=np.float32) for r in res.results], axis=0)
```

```python
import numpy as np
from contextlib import ExitStack
import concourse.bass as bass
import concourse.mybir as mybir
from concourse.bass_utils import run_bass_kernel_spmd

F32 = mybir.dt.float32
BF16 = mybir.dt.bfloat16
I32 = mybir.dt.int32
AF = mybir.ActivationFunctionType
ALU = mybir.AluOpType
AX = mybir.AxisListType

SAME_ENGINE_SYNC = True
COMPUTE = ("pe", "act", "dve", "pool")


class V:
    __slots__ = ("ap", "keys")

    def __init__(self, ap, keys):
        self.ap = ap
        self.keys = tuple(keys)

    def __getitem__(self, idx):
        return V(self.ap[idx], self.keys)

    def re(self, s, **kw):
        return V(self.ap.rearrange(s, **kw), self.keys)

    def bc(self, shape):
        return V(self.ap.to_broadcast(list(shape)), self.keys)

    def bitcast(self, dt):
        return V(self.ap.bitcast(dt), self.keys)


class Tile:
    def __init__(self, b, name, handle, nslots):
        self.b = b
        self.name = name
        self.h = handle
        self.nslots = nslots

    def all(self):
        if self.nslots:
            return V(self.h[:], [(self.name, i) for i in range(self.nslots)])
        return V(self.h[:], [(self.name, None)])

    def s(self, i):
        assert self.nslots and 0 <= i < self.nslots
        return V(self.h[:, i], [(self.name, i)])

    def __getitem__(self, idx):
        return self.all()[idx]


class Builder:
    def __init__(self, nc, n_dma_sems=24):
        self.nc = nc
        self.prog = {e: [] for e in ("pe", "act", "dve", "pool", "sp")}
        self.waited = {e: {} for e in self.prog}
        self.lastw = {}
        self.readers = {}
        self.n_dma = n_dma_sems
        self.dma_cnt = {"sp": [0] * n_dma_sems, "pool": [0] * n_dma_sems, "act": [0] * n_dma_sems}
        self.dma_rr = {"sp": 0, "pool": 0, "act": 0}
        self.stack = None
        self.uid = 0
        self.out_tokens = []
        self.latest = {}
        self.bounds_reg = None
        self.marks = {}

    def sbuf(self, name, shape, dtype, nslots=0):
        h = self.stack.enter_context(self.nc.sbuf_tensor(name, list(shape), dtype))
        return Tile(self, name, h, nslots)

    def psum(self, name, shape, dtype, nslots=0):
        h = self.stack.enter_context(self.nc.psum_tensor(name, list(shape), dtype))
        return Tile(self, name, h, nslots)

    def dram(self, name, shape, dtype, kind="Internal"):
        t = self.nc.dram_tensor(name, list(shape), dtype, kind=kind)
        return V(t.ap(), [(name, None)])

    def _deps(self, eng, reads, writes, skip_self):
        toks = []
        for v in reads:
            for k in v.keys:
                toks += list(self.lastw.get(k, {}).items())
        for v in writes:
            for k in v.keys:
                toks += list(self.lastw.get(k, {}).items())
                toks += list(self.readers.get(k, {}).items())
        need = {}
        for src, val in toks:
            if src == ("e", eng) and (skip_self or not SAME_ENGINE_SYNC or eng == "pe"):
                continue
            if self.waited[eng].get(src, -1) >= val:
                continue
            if need.get(src, -1) < val:
                need[src] = val
        for src, val in need.items():
            self.waited[eng][src] = val
        return list(need.items())

    def _commit(self, tok, reads, writes, partial):
        src, val = tok
        if self.latest.get(src, -1) < val:
            self.latest[src] = val
        for v in reads:
            for k in v.keys:
                d = self.readers.setdefault(k, {})
                if d.get(src, -1) < val:
                    d[src] = val
        for v in writes:
            for k in v.keys:
                d = self.lastw.setdefault(k, {})
                if d.get(src, -1) < val:
                    d[src] = val

    def op(self, eng, fn, reads=(), writes=(), skip_self=False, partial=False):
        reads = [r for r in reads if isinstance(r, V)]
        writes = [w for w in writes if isinstance(w, V)]
        waits = self._deps(eng, reads, writes, skip_self)
        idx = len(self.prog[eng])
        tok = (("e", eng), idx)
        self.prog[eng].append(["op", waits, fn, None])
        self._commit(tok, reads, writes, partial)
        return tok

    def dma(self, q, fn, reads=(), writes=(), partial=False):
        reads = [r for r in reads if isinstance(r, V)]
        writes = [w for w in writes if isinstance(w, V)]
        i = self.dma_rr[q]
        self.dma_rr[q] = (i + 1) % self.n_dma
        src = ("d", q, i)
        waits = self._deps(q, reads, writes, False)
        prev = self.dma_cnt[q][i]
        if prev > 0 and self.waited[q].get(src, -1) < prev * 16:
            waits.append((src, prev * 16))
            self.waited[q][src] = prev * 16
        self.dma_cnt[q][i] += 1
        tok = (src, self.dma_cnt[q][i] * 16)
        self.prog[q].append(["dma", waits, fn, src])
        self._commit(tok, reads, writes, partial)
        return tok

    def mark(self, name):
        if name in self.marks:
            return
        self.barrier()
        self.marks[name] = {e: len(v) for e, v in self.prog.items()}

    def barrier(self):
        for eng in self.prog:
            need = {}
            for src, val in self.latest.items():
                if src == ("e", eng) and (eng == "pe" or not SAME_ENGINE_SYNC):
                    continue
                if self.waited[eng].get(src, -1) >= val:
                    continue
                need[src] = val
            for src, val in need.items():
                self.waited[eng][src] = val
            if need:
                self.prog[eng].append(["op", list(need.items()), None, None])

    def wait_all(self, eng, toks):
        need = {}
        for src, val in toks:
            if self.waited[eng].get(src, -1) >= val:
                continue
            if need.get(src, -1) < val:
                need[src] = val
        self.prog[eng].append(["op", list(need.items()), None, None])

    def emit(self, trunc=None, tail=None):
        nc = self.nc
        if trunc is not None:
            self.prog = {e: v[:self.marks[trunc][e]] for e, v in self.prog.items()}
            tail(self)
        signal = {e: set() for e in COMPUTE}
        for e, lst in self.prog.items():
            for item in lst:
                for src, val in item[1]:
                    if src[0] == "e":
                        signal[src[1]].add(val)
        rank = {}
        for e in COMPUTE:
            r = {}
            c = 0
            for idx in sorted(signal[e]):
                c += 1
                r[idx] = c
            rank[e] = r
        from contextlib import ExitStack
        with ExitStack() as st:
            esem = {e: st.enter_context(nc.semaphore("sem_" + e)) for e in COMPUTE}
            dsem = {}
            for q in ("sp", "pool", "act"):
                for i in range(self.n_dma):
                    if self.dma_cnt[q][i] > 0:
                        dsem[("d", q, i)] = st.enter_context(nc.semaphore("dsem_%s_%d" % (q, i)))
            block = st.enter_context(nc.Block())

            def run(ename, eng):
                for idx, (kind, waits, fn, dsrc) in enumerate(self.prog[ename]):
                    for src, val in waits:
                        if src[0] == "e":
                            eng.wait_ge(esem[src[1]], rank[src[1]][val])
                        else:
                            eng.wait_ge(dsem[src], val)
                    if fn is None:
                        continue
                    ins = fn(eng)
                    if kind == "dma":
                        ins.then_inc(dsem[dsrc], 16)
                    elif ename in COMPUTE and idx in rank[ename]:
                        ins.then_inc(esem[ename], 1)

            @block.tensor
            def _(t):
                run("pe", t)

            @block.scalar
            def _(a):
                run("act", a)

            @block.vector
            def _(v):
                run("dve", v)

            @block.gpsimd
            def _(g):
                run("pool", g)

            @block.sync
            def _(s):
                run("sp", s)

    def matmul(self, out, lhsT, rhs, start=True, stop=True):
        return self.op("pe", lambda e: e.matmul(out.ap, lhsT=lhsT.ap, rhs=rhs.ap, start=start, stop=stop),
                       reads=[lhsT, rhs], writes=[out], partial=not start)

    def transpose(self, out, in_, ident):
        return self.op("pe", lambda e: e.transpose(out=out.ap, in_=in_.ap, identity=ident.ap),
                       reads=[in_, ident], writes=[out], partial=True)

    def act(self, out, in_, func, bias=0.0, scale=1.0, accum_out=None, eng="act"):
        ba = bias.ap if isinstance(bias, V) else bias
        sa = scale.ap if isinstance(scale, V) else scale
        kw = {}
        if accum_out is not None:
            kw["accum_out"] = accum_out.ap
        return self.op("act", lambda e: e.activation(out=out.ap, in_=in_.ap, func=func, bias=ba, scale=sa, **kw),
                       reads=[in_, bias, scale], writes=[out] + ([accum_out] if accum_out is not None else []))

    def ts(self, out, in0, s1, s2=None, op0=ALU.mult, op1=None, eng="dve", accum_out=None):
        a1 = s1.ap if isinstance(s1, V) else s1
        a2 = s2.ap if isinstance(s2, V) else s2
        kw = {}
        if op1 is not None:
            kw["op1"] = op1
        if accum_out is not None:
            kw["accum_out"] = accum_out.ap
        return self.op(eng, lambda e: e.tensor_scalar(out=out.ap, in0=in0.ap, scalar1=a1, scalar2=a2, op0=op0, **kw),
                       reads=[in0, s1, s2], writes=[out] + ([accum_out] if accum_out is not None else []))

    def tt(self, out, in0, in1, op, eng="dve"):
        return self.op(eng, lambda e: e.tensor_tensor(out=out.ap, in0=in0.ap, in1=in1.ap, op=op),
                       reads=[in0, in1], writes=[out])

    def stt(self, out, in0, scalar, in1, op0, op1):
        sa = scalar.ap if isinstance(scalar, V) else scalar
        return self.op("dve", lambda e: e.scalar_tensor_tensor(out=out.ap, in0=in0.ap, scalar=sa, in1=in1.ap, op0=op0, op1=op1),
                       reads=[in0, scalar, in1], writes=[out])

    def copy(self, out, in_, eng="dve"):
        if eng == "act":
            return self.op("act", lambda e: e.copy(out=out.ap, in_=in_.ap), reads=[in_], writes=[out])
        return self.op(eng, lambda e: e.tensor_copy(out=out.ap, in_=in_.ap), reads=[in_], writes=[out])

    def reduce(self, out, in_, op, axis=AX.X):
        return self.op("dve", lambda e: e.tensor_reduce(out=out.ap, in_=in_.ap, axis=axis, op=op),
                       reads=[in_], writes=[out])

    def recip(self, out, in_):
        return self.op("dve", lambda e: e.reciprocal(out=out.ap, in_=in_.ap), reads=[in_], writes=[out])

    def max8(self, out, in_):
        return self.op("dve", lambda e: e.max(out=out.ap, in_=in_.ap), reads=[in_], writes=[out])

    def scan(self, out, d0, d1, initial, op0, op1):
        ia = initial.ap if isinstance(initial, V) else initial
        return self.op("dve", lambda e: e.tensor_tensor_scan(out=out.ap, data0=d0.ap, data1=d1.ap, initial=ia, op0=op0, op1=op1),
                       reads=[d0, d1, initial], writes=[out])

    def memset(self, out, val, eng="dve"):
        return self.op(eng, lambda e: e.memset(out.ap, val), writes=[out])

    def iota(self, out, pattern, base=0, channel_multiplier=0, allow=False):
        return self.op("pool", lambda e: e.iota(out.ap, pattern=pattern, base=base, channel_multiplier=channel_multiplier,
                                                allow_small_or_imprecise_dtypes=allow), writes=[out])

    def load(self, out, in_, q="sp", partial=False):
        return self.dma(q, lambda e: e.dma_start(out=out.ap, in_=in_.ap), reads=[in_], writes=[out], partial=partial)

    def bcast_load(self, out, vec):
        return self.dma("sp", lambda e: e.dma_start(out=out.ap, in_=vec.ap.partition_broadcast(128)), reads=[vec], writes=[out])

    def gather(self, out, in_, idx, bounds=None, partial=False):
        def fn(e):
            kw = {}
            if bounds is not None:
                if self.bounds_reg is None:
                    self.bounds_reg = (e.alloc_register("gather_bound"), bounds)
                    e.reg_mov(self.bounds_reg[0], bounds)
                assert self.bounds_reg[1] == bounds
                kw["bounds_check"] = self.bounds_reg[0]
                kw["oob_is_err"] = False
            return e.indirect_dma_start(out=out.ap, out_offset=None, in_=in_.ap,
                                        in_offset=bass.IndirectOffsetOnAxis(ap=idx.ap, axis=0), **kw)
        return self.dma("pool", fn, reads=[in_, idx], writes=[out], partial=partial)

    def scatter(self, out, in_, idx, bounds=None):
        def fn(e):
            kw = {}
            if bounds is not None:
                kw["bounds_check"] = bounds
                kw["oob_is_err"] = False
            return e.indirect_dma_start(out=out.ap, out_offset=bass.IndirectOffsetOnAxis(ap=idx.ap, axis=0),
                                        in_=in_.ap, in_offset=None, **kw)
        return self.dma("pool", fn, reads=[in_, idx], writes=[out], partial=True)


D = 1024
NK = 8
EPS = 1e-6
BIG = 1.0e4


def build(T, debug=False, trunc=None):
    NT = T // 128
    NG = T // 512
    RB = 256
    SUB = RB // 128
    NB = 2 * T // RB + 63
    NBMAX = 2 * T // RB
    nc = bass.Bass("TRN2", target_bir_lowering=False)
    b = Builder(nc)

    def din(name, shape, dt=F32):
        return V(nc.dram_tensor(name, list(shape), dt, kind="ExternalInput").ap(), [(name, None)])

    def dout(name, shape, dt=F32, kind="ExternalOutput"):
        return V(nc.dram_tensor(name, list(shape), dt, kind=kind).ap(), [(name, None)])

    x = din("x", [T, D])
    w_in = din("w_in", [D, 2336])
    g1pk = din("g1pk", [128, 8])
    up_f = din("up_f", [16, 256]); up_b = din("up_b", [16, 256])
    bias_f = din("bias_f", [128, 2]); bias_b = din("bias_b", [128, 2])
    gla_gain = din("gla_gain", [128])
    q_gain = din("q_gain", [64]); k_gain = din("k_gain", [64]); att_gain = din("att_gain", [512])
    w_out = din("w_out", [D, D]); g2 = din("g2", [D])
    w_group = din("w_group", [D, 8]); b_group = din("b_group", [8])
    w_expert = din("w_expert", [D, 64]); b_expert = din("b_expert", [64])
    w_gate = din("w_gate", [64, D, 512]); w_up = din("w_up", [64, D, 512]); w_down = din("w_down", [64, 512, D])
    fgain = din("fgain", [D])
    c_ident = din("c_ident", [128, 128]); c_mask4 = din("c_mask4", [128, 512]); c_lstrict = din("c_lstrict", [128, 128])
    c_cos = din("c_cos", [128, NT, 32]); c_sin = din("c_sin", [128, NT, 32])
    c_pidx = din("c_pidx", [128, 1]); c_thr = din("c_thr", [128, NB]); c_bd = din("c_bd", [128, 256])
    out = dout("out", [T, D])
    dk = "ExternalOutput" if debug else "Internal"
    h1_d = dout("h1_d", [T, D], F32, dk)
    xs_d = dout("xs_d", [NB * RB, D], BF16, dk)
    ys_d = dout("ys_d", [NB * RB, D], F32, dk)
    mp_d = dout("mp_d", [128, NT * 192], F32, "Internal")
    xn2_d = dout("xn2_d", [T, D], BF16, "Internal")
    if debug:
        dbg_mixg = dout("dbg_mixg", [128, 4, T], BF16)
        dbg_mixa = dout("dbg_mixa", [128, 4, T], BF16)
        dbg_route = dout("dbg_route", [128, NT, 4])
        dbg_be = dout("dbg_be", [128, NB])

    with ExitStack() as st0:
        b.stack = st0
        ident_f = b.sbuf("ident_f", [128, 128], F32); ident_b = b.sbuf("ident_b", [128, 128], BF16)
        mask4 = b.sbuf("mask4", [128, 512], F32)
        lstrict_f = b.sbuf("lstrict_f", [128, 128], F32); lstrict_b = b.sbuf("lstrict_b", [128, 128], BF16)
        ones_b = b.sbuf("ones_b", [128, 128], BF16); ones_f = b.sbuf("ones_f", [128, 128], F32)
        zeros_b = b.sbuf("zeros_b", [128, 1024], BF16)
        zeros_f = b.sbuf("zeros_f", [128, 1024], F32)
        b.memset(zeros_f[:], 0.0, eng="pool")
        pidx = b.sbuf("pidx", [128, 1], F32); thr = b.sbuf("thr", [128, NB], F32)
        g1 = b.sbuf("g1", [128, 8], F32)
        b.load(ident_f[:], c_ident); b.load(mask4[:], c_mask4); b.load(lstrict_f[:], c_lstrict)
        bdm = b.sbuf("bdm", [128, 256], F32)
        b.load(bdm[:], c_bd)
        b.load(pidx[:], c_pidx); b.load(thr[:], c_thr); b.load(g1[:], g1pk)
        b.copy(ident_b[:], ident_f[:]); b.copy(lstrict_b[:], lstrict_f[:])
        b.memset(ones_b[:], 1.0); b.memset(ones_f[:], 1.0); b.memset(zeros_b[:], 0.0, eng="pool")
        ones512 = b.sbuf("ones512", [128, 512], F32)
        b.memset(ones512[:], 1.0)
        gates = b.sbuf("gates", [128, NT, 2], F32)
        dest_i = b.sbuf("dest_i", [128, NT, 2], I32)
        idxw_i = b.sbuf("idxw_i", [128, NB], I32)
        run = b.sbuf("run", [128, 64], F32)
        b.memset(run[:], 0.0)
        stS1 = ExitStack(); stS1.__enter__(); b.stack = stS1
        mixg = b.sbuf("mixg", [128, 4, T], BF16)
        STK = stS1

        def xn_group_factory(stk, psum_tr):
            xt = b.sbuf("xt" + stk, [128, 2, D], F32, nslots=2)
            junk = b.sbuf("junk" + stk, [128, D], BF16)
            xnb = b.sbuf("xnb" + stk, [128, 2, D], BF16, nslots=2)
            xnT = b.sbuf("xnT" + stk, [128, 2, NK, 512], BF16, nslots=2)
            st1 = b.sbuf("st1" + stk, [128, 2, 4], F32, nslots=2)

            def load_tile(i):
                b.load(xt.s(i % 2), x[i * 128:(i + 1) * 128, :])

            def group(g):
                gs = g % 2
                for j in range(4):
                    i = g * 4 + j
                    s = i % 2
                    if i == 0:
                        load_tile(0)
                    if i + 1 < NT:
                        load_tile(i + 1)
                    ss = st1.s(s)
                    b.act(junk[:], xt.s(s), AF.Square, accum_out=ss[:, 0:1])
                    b.act(ss[:, 1:2], ss[:, 0:1], AF.Ln, bias=EPS, scale=1.0 / D)
                    b.act(ss[:, 2:3], ss[:, 1:2], AF.Exp, scale=-0.5)
                    b.ts(xnb.s(s), xt.s(s), ss[:, 2:3], None, op0=ALU.mult)
                    ptr_s = psum_tr.s(s)
                    for k in range(NK):
                        b.transpose(ptr_s[:, k, :], xnb.s(s)[:, k * 128:(k + 1) * 128], ident_b[:])
                    b.copy(xnT.s(gs)[:, :, j * 128:(j + 1) * 128], ptr_s, eng="act")
                return xnT.s(gs)
            return group

        def load_w_cols(Wb, stage, cols):
            c0 = 0
            for (a, z) in cols:
                n = z - a
                for k in range(NK):
                    sl = stage.s(k % 2)
                    b.load(sl[:, 0:n], w_in[k * 128:(k + 1) * 128, a:z])
                    b.ts(Wb[:, k, c0:c0 + n], sl[:, 0:n], g1[:, k:k + 1], None, op0=ALU.mult, eng="dve")
                c0 += n
        for dt in range(2):
            with ExitStack() as stg:
                b.stack = stg
                stg.callback(b.barrier)
                sfx = "g%d" % dt
                Wb = b.sbuf("Wb" + sfx, [128, NK, 800], BF16)
                wst = b.sbuf("wst" + sfx, [128, 2, 256], F32, nslots=2)
                load_w_cols(Wb, wst, [(dt * 128, dt * 128 + 128), (256 + dt * 128, 256 + dt * 128 + 128),
                                      (512 + dt * 256, 512 + dt * 256 + 256), (1024 + dt * 256, 1024 + dt * 256 + 256),
                                      (1536, 1552), (1552, 1568)])
                upf = b.sbuf("upf" + sfx, [16, 128], F32); upb = b.sbuf("upb" + sfx, [16, 128], F32)
                b.load(upf[:], up_f[:, dt * 128:(dt + 1) * 128]); b.load(upb[:], up_b[:, dt * 128:(dt + 1) * 128])
                nbias = b.sbuf("nbias" + sfx, [128, 2], F32)
                bst = b.sbuf("bst" + sfx, [128, 2], F32)
                bst2 = b.sbuf("bst2" + sfx, [128, 2], F32)
                b.load(bst[:], bias_f); b.load(bst2[:], bias_b)
                b.ts(nbias[:, 0:1], bst[:, dt:dt + 1], -1.0, None, op0=ALU.mult)
                b.ts(nbias[:, 1:2], bst2[:, dt:dt + 1], -1.0, None, op0=ALU.mult)
                ggain = b.sbuf("ggain" + sfx, [128, 128], F32)
                b.bcast_load(ggain[:], gla_gain)
                qef = b.sbuf("qef" + sfx, [128, T], BF16); kef = b.sbuf("kef" + sfx, [128, T], BF16)
                qeb = b.sbuf("qeb" + sfx, [128, T], BF16); keb = b.sbuf("keb" + sfx, [128, T], BF16)
                decf = b.sbuf("decf" + sfx, [128, NT], F32); decb = b.sbuf("decb" + sfx, [128, NT], F32)
                v_tm = b.sbuf("v_tm" + sfx, [128, NT, 256], BF16)
                G2 = b.sbuf("G2" + sfx, [128, NT, 256], BF16)
                zT = b.sbuf("zT" + sfx, [16, 2, 512], F32)
                qk32 = b.sbuf("qk32" + sfx, [128, 2, 512], F32)
                Pex = b.sbuf("Pex" + sfx, [128, 513], F32)
                Dd = b.sbuf("Dd" + sfx, [128, 512], F32)
                Ee = b.sbuf("Ee" + sfx, [128, 2, 512], F32)
                lap = b.sbuf("lap" + sfx, [128, 512], F32)
                gt = b.sbuf("gt" + sfx, [128, 3, 256], F32)
                with ExitStack() as stx:
                    b.stack = stx
                    stx.callback(b.barrier)
                    ptr = b.psum("ptr" + sfx, [128, 2, NK, 128], BF16, nslots=2)
                    pfm = b.psum("pfm" + sfx, [128, 2, 512], F32, nslots=2)
                    ptm = b.psum("ptm" + sfx, [128, 2, 512], F32, nslots=2)
                    pz = b.psum("pz" + sfx, [16, 2, 512], F32, nslots=2)
                    xgroup = xn_group_factory(sfx, ptr)
                    b.memset(Pex[:, 0:1], 0.0)
                    for g in range(NG):
                        xg = xgroup(g)
                        tok = slice(g * 512, (g + 1) * 512)
                        for qi in range(2):
                            for k in range(NK):
                                b.matmul(pfm.s(qi), Wb[:, k, qi * 128:(qi + 1) * 128], xg[:, k, :], start=(k == 0), stop=(k == NK - 1))
                            b.op("act", (lambda o_, i_, m_: (lambda e: e.mul(out=o_.ap, in_=i_.ap, mul=m_)))(qk32[:, qi, :], pfm.s(qi), (0.125 if qi == 0 else 1.0)), reads=[pfm.s(qi)], writes=[qk32[:, qi, :]])
                        for zi in range(2):
                            for k in range(NK):
                                b.matmul(pz.s(zi), Wb[:, k, 768 + zi * 16:768 + zi * 16 + 16], xg[:, k, :], start=(k == 0), stop=(k == NK - 1))
                            b.copy(zT[:, zi, :], pz.s(zi))
                        for di in range(2):
                            up = upf if di == 0 else upb
                            pl = pfm.s(di)
                            b.matmul(pl, up[:], zT[:, di, :])
                            b.act(lap[:], pl, AF.Exp, bias=nbias[:, di:di + 1], scale=-1.0)
                            b.act(lap[:], lap[:], AF.Ln, bias=1.0)
                            b.scan(Pex[:, 1:513], ones512[:], lap[:], 0.0, ALU.mult, ALU.add)
                            Pc = Pex[:, 1:513].re("p (n c) -> p n c", c=128)
                            Pe = Pex[:, 0:512].re("p (n c) -> p n c", c=128)
                            D3 = Dd[:].re("p (n c) -> p n c", c=128)
                            if di == 0:
                                b.tt(D3, Pc, Pe[:, :, 0:1].bc([128, 4, 128]), ALU.subtract)
                            else:
                                b.tt(D3, Pc[:, :, 127:128].bc([128, 4, 128]), Pe, ALU.subtract)
                            b.act(Ee[:, 0, :], Dd[:], AF.Exp, scale=-1.0 / 16.0)
                            b.act(Ee[:, 1, :], Dd[:], AF.Exp, scale=1.0 / 16.0)
                            qe, ke, dec = (qef, kef, decf) if di == 0 else (qeb, keb, decb)
                            b.tt(qe[:, tok], qk32[:, 0, :], Ee[:, 0, :], ALU.mult)
                            b.tt(ke[:, tok], qk32[:, 1, :], Ee[:, 1, :], ALU.mult)
                            E3 = Ee[:, 0, :].re("p (n c) -> p n c", c=128)
                            col = 127 if di == 0 else 0
                            b.copy(dec[:, g * 4:(g + 1) * 4].re("p (n o) -> p n o", o=1), E3[:, :, col:col + 1])
                        for j in range(4):
                            i = g * 4 + j
                            xl = [xg[:, k, j * 128:(j + 1) * 128] for k in range(NK)]
                            pv = ptm.s(0)
                            for k in range(NK):
                                b.matmul(pv[:, 0:256], xl[k], Wb[:, k, 256:512], start=(k == 0), stop=(k == NK - 1))
                            b.copy(v_tm[:, i, :], pv[:, 0:256], eng="act")
                            pg = ptm.s(1)
                            for k in range(NK):
                                b.matmul(pg[:, 0:256], xl[k], Wb[:, k, 512:768], start=(k == 0), stop=(k == NK - 1))
                            b.act(gt[:, 0, :], pg[:, 0:256], AF.Exp, scale=-1.0)
                            b.ts(gt[:, 0, :], gt[:, 0, :], 1.0, None, op0=ALU.add)
                            b.recip(gt[:, 1, :], gt[:, 0, :])
                            b.tt(gt[:, 2, :], gt[:, 1, :], pg[:, 0:256], ALU.mult)
                            b.tt(G2[:, i, :].re("p (h e) -> p h e", h=2), gt[:, 2, :].re("p (h e) -> p h e", h=2),
                                 ggain[:].re("p (o e) -> p o e", o=1).bc([128, 2, 128]), ALU.mult)
                b.mark('glaprep%d' % dt)
                with ExitStack() as stc:
                    b.stack = stc
                    stc.callback(b.barrier)
                    ketm = b.sbuf("ketm" + sfx, [128, 2, 128], BF16, nslots=2)
                    Sf = b.sbuf("Sf" + sfx, [128, NT, 256], BF16)
                    Sb = b.sbuf("Sb" + sfx, [128, 2, 256], BF16, nslots=2)
                    Tst = b.sbuf("Tst" + sfx, [128, 256], F32)
                    Am = b.sbuf("Am" + sfx, [128, 4, 128], BF16)
                    osb = b.sbuf("osb" + sfx, [128, 256], F32)
                    omx = b.sbuf("omx" + sfx, [128, 256], BF16)
                    jk = b.sbuf("jk" + sfx, [128, 128], BF16)
                    stt_ = b.sbuf("stt" + sfx, [128, 8], F32)
                    pkv_ = b.psum("pkv" + sfx, [128, 512], F32); pkv = pkv_[:, 0:256]
                    pa0 = b.psum("pa0" + sfx, [128, 4, 128], F32)
                    pa1 = b.psum("pa1" + sfx, [128, 4, 128], F32)
                    po_ = b.psum("po" + sfx, [128, 512], F32); po = po_[:, 0:256]
                    ptk = b.psum("ptk" + sfx, [128, 8, 128], BF16)
                    for n in range(NT):
                        ck = slice(n * 128, (n + 1) * 128)
                        if n >= 1:
                            b.stt(Sf[:, n, :], Tst[:], decf[:, n - 1:n], bdm[:], ALU.mult, ALU.mult)
                        if n == NT - 1:
                            break
                        b.transpose(ptk[:, 0, :], kef[:, ck], ident_b[:])
                        b.copy(ketm.s(n % 2), ptk[:, 0, :], eng="act")
                        b.matmul(pkv, ketm.s(n % 2), v_tm[:, n, :])
                        if n == 0:
                            b.copy(Tst[:], pkv)
                        else:
                            b.stt(Tst[:], Tst[:], decf[:, n - 1:n], pkv, ALU.mult, ALU.add)
                    for n in range(NT - 1, -1, -1):
                        ck = slice(n * 128, (n + 1) * 128)
                        sbc = Sb.s(n % 2)
                        if n < NT - 1:
                            b.stt(sbc, Tst[:], decb[:, n + 1:n + 2], bdm[:], ALU.mult, ALU.mult)
                        for hl in range(2):
                            pr = slice(hl * 64, (hl + 1) * 64)
                            pah = pa0 if hl == 0 else pa1
                            b.matmul(pah[:, 0, :], kef[pr, ck], qef[pr, ck])
                            b.matmul(pah[:, 1, :], keb[pr, ck], qeb[pr, ck])
                        for hl in range(2):
                            pah = pa0 if hl == 0 else pa1
                            b.tt(Am[:, 2 * hl:2 * hl + 2, :].re("p a c -> p (a c)"), pah[:, 0:2, :].re("p a c -> p (a c)"), mask4[:, 128:384], ALU.mult)
                        for hl in range(2):
                            pr = slice(hl * 64, (hl + 1) * 64)
                            es = slice(hl * 128, (hl + 1) * 128)
                            mms = [(Am[:, 2 * hl, :], v_tm[:, n, es]), (Am[:, 2 * hl + 1, :], v_tm[:, n, es])]
                            if n > 0:
                                mms.append((qef[:, ck], Sf[:, n, es]))
                            if n < NT - 1:
                                mms.append((qeb[:, ck], sbc[:, es]))
                            for mi, (l_, r_) in enumerate(mms):
                                b.matmul(po[:, es], l_, r_, start=(mi == 0), stop=(mi == len(mms) - 1))
                        if n > 0:
                            b.transpose(ptk[:, 1, :], keb[:, ck], ident_b[:])
                            b.copy(ketm.s(n % 2), ptk[:, 1, :], eng="act")
                            b.matmul(pkv, ketm.s(n % 2), v_tm[:, n, :])
                            if n == NT - 1:
                                b.copy(Tst[:], pkv)
                            else:
                                b.stt(Tst[:], Tst[:], decb[:, n + 1:n + 2], pkv, ALU.mult, ALU.add)
                        for hl in range(2):
                            es = slice(hl * 128, (hl + 1) * 128)
                            b.act(jk[:], po[:, es], AF.Square, accum_out=stt_[:, hl:hl + 1])
                        b.act(stt_[:, 2:4], stt_[:, 0:2], AF.Ln, bias=EPS, scale=1.0 / 128)
                        b.act(stt_[:, 4:6], stt_[:, 2:4], AF.Exp, scale=-0.5)
                        b.tt(osb[:].re("p (h e) -> p h e", h=2), po.re("p (h e) -> p h e", h=2),
                             stt_[:, 4:6].re("p (h o) -> p h o", o=1).bc([128, 2, 128]), ALU.mult)
                        b.tt(omx[:], osb[:], G2[:, n, :], ALU.mult)
                        for hl in range(2):
                            b.transpose(ptk[:, hl, :], omx[:, hl * 128:(hl + 1) * 128], ident_b[:])
                        b.copy(mixg[:, 2 * dt:2 * dt + 2, ck], ptk[:, 0:2, :])
            b.stack = STK
            b.mark('gla%d' % dt)
        with ExitStack() as sta:
            b.stack = sta
            sta.callback(b.barrier)
            mixa = b.sbuf("mixa", [128, 4, T], BF16)
            qT = b.sbuf("qT_att", [128, 4, T], BF16)
            kT = b.sbuf("kT_att", [128, T], BF16)
            Vaug = b.sbuf("Vaug", [128, NT, 2, 65], BF16)
            b.memset(Vaug[:, :, :, 64:65], 1.0)
            qg = b.sbuf("qg_row", [128, 64], F32); kg = b.sbuf("kg_row", [128, 64], F32)
            b.bcast_load(qg[:], q_gain)
            b.bcast_load(kg[:], k_gain)
            b.ts(qg[:], qg[:], 0.125, None, op0=ALU.mult)
            agr = b.sbuf("agr", [128, 512], F32)
            b.bcast_load(agr[:], att_gain)
            cosb = b.sbuf("cosb", [128, NT, 32], F32); sinb = b.sbuf("sinb", [128, NT, 32], F32)
            b.load(cosb[:], c_cos); b.load(sinb[:], c_sin)
            with ExitStack() as stx:
                b.stack = stx
                stx.callback(b.barrier)
                ptr = b.psum("ptr_a", [128, 2, NK, 128], BF16, nslots=2)
                ptm = b.psum("ptm_a", [128, 2, 512], F32, nslots=2)
                ptq = b.psum("ptq_a", [128, 8, 128], BF16)
                Wb = b.sbuf("Wb_a", [128, NK, 768], BF16)
                wst = b.sbuf("wst_a", [128, 2, 512], F32, nslots=2)
                load_w_cols(Wb, wst, [(1568, 2080), (2080, 2336)])
                sq = b.sbuf("sq_a", [128, 512], F32)
                s8 = b.sbuf("s8_a", [128, 24], F32)
                qn = b.sbuf("qn_a", [128, 512], F32)
                t1 = b.sbuf("t1_a", [128, 256], F32); t2 = b.sbuf("t2_a", [128, 256], F32)
                qr = b.sbuf("qr_a", [128, 512], BF16)
                kr = b.sbuf("kr_a", [128, 128], BF16)
                xgroup = xn_group_factory("a", ptr)

                def norm_rope(src_ps, hd, grow, i, dst_even, dst_odd):
                    nh = 1
                    for z_ in hd:
                        nh *= z_
                    w = nh * 64
                    b.act(sq[:, 0:w], src_ps, AF.Square)
                    b.reduce(s8[:, 0:nh], sq[:, 0:w].re("p (h d) -> p h d", d=64), ALU.add)
                    b.act(s8[:, 8:8 + nh], s8[:, 0:nh], AF.Ln, bias=EPS, scale=1.0 / 64)
                    b.act(s8[:, 16:16 + nh], s8[:, 8:8 + nh], AF.Exp, scale=-0.5)
                    b.tt(qn[:, 0:w].re("p (h d) -> p h d", d=64), src_ps.re("p (h d) -> p h d", d=64),
                         s8[:, 16:16 + nh].re("p (h o) -> p h o", o=1).bc([128, nh, 64]), ALU.mult)
                    b.tt(qn[:, 0:w].re("p (h d) -> p h d", d=64), qn[:, 0:w].re("p (h d) -> p h d", d=64),
                         grow[:].re("p (o d) -> p o d", o=1).bc([128, nh, 64]), ALU.mult)
                    if len(hd) == 2:
                        pat = "p (k g i two) -> p k g i two"; kw = dict(k=hd[0], g=hd[1], i=32, two=2)
                        pat3 = "p (k g i) -> p k g i"; kw3 = dict(k=hd[0], g=hd[1], i=32)
                        patc = "p (a c i) -> p a c i"; kwc = dict(a=1, c=1)
                        shp = [128, hd[0], hd[1], 32]
                        q4 = qn[:, 0:w].re(pat, **kw)
                        x0 = q4[:, :, :, :, 0]; x1 = q4[:, :, :, :, 1]
                    else:
                        pat = "p (k i two) -> p k i two"; kw = dict(k=hd[0], i=32, two=2)
                        pat3 = "p (k i) -> p k i"; kw3 = dict(k=hd[0], i=32)
                        patc = "p (a i) -> p a i"; kwc = dict(a=1)
                        shp = [128, hd[0], 32]
                        q4 = qn[:, 0:w].re(pat, **kw)
                        x0 = q4[:, :, :, 0]; x1 = q4[:, :, :, 1]
                    cb = cosb[:, i, :].re(patc, **kwc).bc(shp)
                    sb_ = sinb[:, i, :].re(patc, **kwc).bc(shp)
                    hw = nh * 32
                    a1 = t1[:, 0:hw].re(pat3, **kw3); a2 = t2[:, 0:hw].re(pat3, **kw3)
                    b.tt(a1, x0, cb, ALU.mult); b.tt(a2, x1, sb_, ALU.mult)
                    b.tt(dst_even, a1, a2, ALU.subtract)
                    b.tt(a1, x0, sb_, ALU.mult); b.tt(a2, x1, cb, ALU.mult)
                    b.tt(dst_odd, a1, a2, ALU.add)

                for g in range(NG):
                    xg = xgroup(g)
                    for j in range(4):
                        i = g * 4 + j
                        tk = slice(i * 128, (i + 1) * 128)
                        xl = [xg[:, k, j * 128:(j + 1) * 128] for k in range(NK)]
                        pq = ptm.s(0)
                        for k in range(NK):
                            b.matmul(pq, xl[k], Wb[:, k, 0:512], start=(k == 0), stop=(k == NK - 1))
                        pk = ptm.s(1)
                        for k in range(NK):
                            b.matmul(pk[:, 0:256], xl[k], Wb[:, k, 512:768], start=(k == 0), stop=(k == NK - 1))
                        qr5 = qr[:].re("p (g k i two) -> p k g i two", g=4, k=2, i=32, two=2)
                        norm_rope(pq, (2, 4), qg, i, qr5[:, :, :, :, 0], qr5[:, :, :, :, 1])
                        for g4 in range(4):
                            b.transpose(ptq[:, g4, :], qr[:, g4 * 128:(g4 + 1) * 128], ident_b[:])
                        b.copy(qT[:, :, tk], ptq[:, 0:4, :], eng="act")
                        kr4 = kr[:].re("p (h i two) -> p h i two", i=32, two=2)
                        norm_rope(pk[:, 0:128], (2,), kg, i, kr4[:, :, :, 0], kr4[:, :, :, 1])
                        b.transpose(ptq[:, 4, :], kr[:], ident_b[:])
                        b.copy(kT[:, tk], ptq[:, 4, :], eng="act")
                        b.copy(Vaug[:, i, :, 0:64], pk[:, 128:256].re("p (h d) -> p h d", d=64), eng="act")
            b.mark('attproj')
            for blk in range(NB * SUB):
                b.load(xs_d[blk * 128:(blk + 1) * 128, :], zeros_b[:], q="sp")
            with ExitStack() as stx:
                b.stack = stx
                stx.callback(b.barrier)
                ps = b.psum("ps_att", [128, 4, 512], F32, nslots=4)
                pov = b.psum("po_att", [128, 2, 512], F32, nslots=2)
                ptk = b.psum("ptk_att", [128, 8, 128], BF16)
                Eb = b.sbuf("Eb", [128, 4, 512], BF16, nslots=4)
                otm = b.sbuf("otm", [128, 4, 512], F32)
                rl = b.sbuf("rl", [128, 2, 4], F32, nslots=2)
                st8 = b.sbuf("st8", [128, 4], F32)
                jk = b.sbuf("jk_att", [128, 512], BF16)
                on = b.sbuf("on_att", [128, 512], F32)
                ob = b.sbuf("ob_att", [128, 512], BF16)
                steps = []
                for qc in range(NG):
                    for g4 in range(4):
                        for s_ in range(NT):
                            steps.append((qc, g4, s_))

                def emit_s(idx):
                    qc, g4, s_ = steps[idx]
                    ees = []
                    pss2 = []
                    for kv in range(2):
                        pr = slice(kv * 64, (kv + 1) * 64)
                        pss = ps.s(kv * 2 + idx % 2)
                        b.matmul(pss, kT[pr, s_ * 128:(s_ + 1) * 128], qT[pr, g4, qc * 512:(qc + 1) * 512])
                        pss2.append(pss)
                    for kv in range(2):
                        ee = Eb.s((idx % 2) * 2 + kv)
                        b.act(ee, pss2[kv], AF.Exp)
                        ees.append(ee)
                    return ees

                def emit_pv(idx, ees):
                  qc, g4, s_ = steps[idx]
                  for kv in range(2):
                    ee = ees[kv]
                    h = kv * 4 + g4
                    po_ = pov.s(kv)
                    po3 = po_[:, 0:260].re("p (j e) -> p j e", e=65)
                    for j in range(4):
                        b.matmul(po3[:, j, :], ee[:, j * 128:(j + 1) * 128], Vaug[:, s_, kv, :],
                                 start=(s_ == 0 and j == 0), stop=(s_ == NT - 1 and j == 3))
                    if s_ == NT - 1:
                        rr = rl.s(kv)
                        b.recip(rr.re("p (j o) -> p j o", o=1), po3[:, :, 64:65])
                        b.tt(otm[:].re("p j (h d) -> p j h d", d=64)[:, :, h, :], po3[:, :, 0:64],
                             rr.re("p (j o) -> p j o", o=1).bc([128, 4, 64]), ALU.mult)
                        if g4 == 3 and kv == 1:
                            for j in range(4):
                                i = qc * 4 + j
                                tk = slice(i * 128, (i + 1) * 128)
                                b.act(jk[:], otm[:, j, :], AF.Square, accum_out=st8[:, 0:1])
                                b.act(st8[:, 1:2], st8[:, 0:1], AF.Ln, bias=EPS, scale=1.0 / 512)
                                b.act(st8[:, 2:3], st8[:, 1:2], AF.Exp, scale=-0.5)
                                b.ts(on[:], otm[:, j, :], st8[:, 2:3], None, op0=ALU.mult)
                                b.tt(ob[:], on[:], agr[:], ALU.mult)
                                for c in range(4):
                                    b.transpose(ptk[:, c, :], ob[:, c * 128:(c + 1) * 128], ident_b[:])
                                b.copy(mixa[:, :, tk], ptk[:, 0:4, :])

                pend = None
                for idx in range(len(steps)):
                    ee = emit_s(idx)
                    if pend is not None:
                        emit_pv(*pend)
                    pend = (idx, ee)
                emit_pv(*pend)
            with ExitStack() as std:
                b.stack = std
                std.callback(b.barrier)
                Wo = b.sbuf("Wo", [128, NK, D], BF16)
                wst = b.sbuf("wst_o", [128, 2, 512], F32, nslots=2)
                for k in range(NK):
                    for hf in range(2):
                        b.load(wst.s(hf), w_out[k * 128:(k + 1) * 128, hf * 512:(hf + 1) * 512])
                        b.copy(Wo[:, k, hf * 512:(hf + 1) * 512], wst.s(hf), eng=("dve" if hf == 0 else "act"))
                g2r = b.sbuf("g2r", [128, D], F32)
                b.bcast_load(g2r[:], g2)
                Wr = b.sbuf("Wr", [128, NK, 72], F32)
                with nc.allow_non_contiguous_dma(reason="small router weights"):
                    for k in range(NK):
                        b.load(Wr[:, k, 0:8], w_group[k * 128:(k + 1) * 128, :])
                        b.load(Wr[:, k, 8:72], w_expert[k * 128:(k + 1) * 128, :])
                brow = b.sbuf("brow", [1, 72], F32)
                b.load(brow[:, 0:8], b_group.re("(o n) -> o n", o=1)); b.load(brow[:, 8:72], b_expert.re("(o n) -> o n", o=1))
                xn2b = b.sbuf("xn2b", [128, 2, D], BF16, nslots=2)
                mp = b.sbuf("mp", [128, 2, 192], F32, nslots=2)
                xt = b.sbuf("xt_o", [128, 2, D], F32, nslots=2)
                h1 = b.sbuf("h1_o", [128, 2, D], F32, nslots=2)
                xn2_2 = b.sbuf("xn2_o", [128, 2, D], F32, nslots=2)
                xn2T_2 = b.sbuf("xn2T_o", [128, 2, NK, 128], F32, nslots=2)
                jk_1 = b.sbuf("jk_o", [128, D], BF16)
                sm_2 = b.sbuf("sm_o", [128, 2, 32], F32, nslots=2)
                lg_2 = b.sbuf("lg_o", [128, 2, 72], F32, nslots=2)
                m8_2 = b.sbuf("m8_o", [128, 2, 16], F32, nslots=2)
                og_2 = b.sbuf("og_o", [128, 2, 8], F32, nslots=2)
                tmp8_2 = b.sbuf("tmp8_o", [128, 2, 8], F32, nslots=2)
                ge_2 = b.sbuf("ge_o", [128, 2, 8], F32, nslots=2)
                ml_2 = b.sbuf("ml_o", [128, 2, 64], F32, nslots=2)
                Mb_2 = b.sbuf("Mb_o", [128, 2, 64], BF16, nslots=2)
                py4 = b.psum("py_o", [128, 4, 512], F32, nslots=4)
                ptf = b.psum("ptf_o", [128, 2, 512], F32, nslots=2)
                plg = b.psum("plg_o", [128, 512], F32)
                pps = b.psum("pps_o", [128, 512], F32)
                b.load(xt.s(0), x[0:128, :])
                for i in range(NT):
                    tk = slice(i * 128, (i + 1) * 128)
                    s = i % 2
                    xn2 = xn2_2.s(s); xn2T = xn2T_2.s(s); jk = jk_1[:]; sm = sm_2.s(s); lg = lg_2.s(s); m8 = m8_2.s(s)
                    og = og_2.s(s); tmp8 = tmp8_2.s(s); ge = ge_2.s(s); ml = ml_2.s(s); Mb = Mb_2.s(s)
                    if i + 1 < NT:
                        b.load(xt.s((i + 1) % 2), x[(i + 1) * 128:(i + 2) * 128, :])
                    for hf in range(2):
                        for c in range(NK):
                            b.matmul(py4.s(2 * s + hf), (mixg if c < 4 else mixa)[:, c % 4, tk], Wo[:, c, hf * 512:(hf + 1) * 512], start=(c == 0), stop=(c == NK - 1))
                        b.tt(h1.s(s)[:, hf * 512:(hf + 1) * 512], py4.s(2 * s + hf), xt.s(s)[:, hf * 512:(hf + 1) * 512], ALU.add)
                    b.load(h1_d[tk, :], h1.s(s))
                    b.act(jk, h1.s(s), AF.Square, accum_out=sm[:, 0:1])
                    b.act(sm[:, 1:2], sm[:, 0:1], AF.Ln, bias=EPS, scale=1.0 / D)
                    b.act(sm[:, 2:3], sm[:, 1:2], AF.Exp, scale=-0.5)
                    b.stt(xn2, h1.s(s), sm[:, 2:3], g2r[:], ALU.mult, ALU.mult)
                    b.copy(xn2b.s(s).re("t (c p) -> t c p", c=NK), xn2.re("t (p c) -> t c p", c=NK), eng="act")
                    b.load(xn2_d[tk, :], xn2b.s(s))
                    for c in range(NK):
                        b.transpose(ptf.s(c // 4)[:, (c % 4) * 128:(c % 4 + 1) * 128], xn2[:, c * 128:(c + 1) * 128], ident_f[:])
                    b.copy(xn2T[:, 0:4, :].re("p c t -> p (c t)"), ptf.s(0), eng="act")
                    b.copy(xn2T[:, 4:8, :].re("p c t -> p (c t)"), ptf.s(1))
                    for c in range(NK):
                        b.matmul(plg[:, 0:72], xn2T[:, c, :], Wr[:, c, :], start=(c == 0), stop=False)
                    b.matmul(plg[:, 0:72], ones_f[0:1, :], brow[:], start=False, stop=True)
                    b.copy(lg, plg[:, 0:72])
                    b.max8(m8[:, 0:8], lg[:, 0:8])
                    b.ts(og, lg[:, 0:8], m8[:, 0:1], None, op0=ALU.is_equal)
                    b.ts(sm[:, 3:4], m8[:, 0:1], -1.0, None, op0=ALU.mult)
                    b.act(ge, lg[:, 0:8], AF.Exp, bias=sm[:, 3:4], scale=1.0, accum_out=sm[:, 4:5])
                    b.recip(sm[:, 5:6], sm[:, 4:5])
                    b.ts(tmp8, og, BIG, -BIG, op0=ALU.mult, op1=ALU.add)
                    b.tt(ml.re("p (g j) -> p g j", j=8), lg[:, 8:72].re("p (g j) -> p g j", j=8),
                         tmp8.re("p (g o) -> p g o", o=1).bc([128, 8, 8]), ALU.add)
                    b.max8(m8[:, 8:16], ml)
                    b.ts(mp.s(s)[:, 0:64], ml, m8[:, 8:9], None, op0=ALU.is_equal)
                    b.ts(mp.s(s)[:, 64:128], ml, m8[:, 9:10], None, op0=ALU.is_equal)
                    b.tt(sm[:, 6:7], m8[:, 9:10], m8[:, 8:9], ALU.subtract)
                    b.act(sm[:, 7:8], sm[:, 6:7], AF.Exp)
                    b.ts(sm[:, 8:9], sm[:, 7:8], 1.0, None, op0=ALU.add)
                    b.recip(sm[:, 9:10], sm[:, 8:9])
                    b.tt(sm[:, 10:11], sm[:, 7:8], sm[:, 9:10], ALU.mult)
                    b.tt(gates[:, i, 0:1], sm[:, 9:10], sm[:, 5:6], ALU.mult)
                    b.tt(gates[:, i, 1:2], sm[:, 10:11], sm[:, 5:6], ALU.mult)
                    b.tt(Mb, mp.s(s)[:, 0:64], mp.s(s)[:, 64:128], ALU.add)
                    b.matmul(pps[:, 0:64], lstrict_b[:], Mb)
                    b.matmul(pps[:, 64:128], ones_b[:], Mb)
                    b.tt(mp.s(s)[:, 128:192], pps[:, 0:64], run[:], ALU.add)
                    b.tt(run[:], pps[:, 64:128], run[:], ALU.add)
                    b.load(mp_d[:, i * 192:(i + 1) * 192], mp.s(s))
        b.mark('router')
        stS1.__exit__(None, None, None)
        b.barrier()
        b.stack = st0
        with ExitStack() as stl:
            b.stack = stl
            stl.callback(b.barrier)
            mpa = b.sbuf("mpa", [128, NT, 192], F32)
            b.load(mpa[:].re("p n c -> p (n c)"), mp_d)
            cmpA = b.sbuf("cmpA", [128, 64, NBMAX], BF16)
            nblk = b.sbuf("nblk", [128, 64], F32)
            pend = b.sbuf("pend", [128, 64], F32)
            pstart = b.sbuf("pstart", [128, 64], F32)
            ones64 = b.sbuf("ones64", [128, 64], F32)
            b.memset(ones64[:], 1.0)
            b.tt(cmpA[:], run[:].re("p (e o) -> p e o", o=1).bc([128, 64, NBMAX]),
                 thr[:, 0:NBMAX].re("p (o n) -> p o n", o=1).bc([128, 64, NBMAX]), ALU.is_gt)
            b.reduce(nblk[:], cmpA[:], ALU.add)
            b.ts(nblk[:], nblk[:], float(RB), None, op0=ALU.mult)
            b.scan(pend[:], ones64[:], nblk[:], 0.0, ALU.mult, ALU.add)
            b.tt(pstart[:], pend[:], nblk[:], ALU.subtract)
            cmpB = b.sbuf("cmpB", [128, NB, 64], BF16)
            bef = b.sbuf("bef", [128, NB], F32)
            b.tt(cmpB[:], pend[:].re("p (o e) -> p o e", o=1).bc([128, NB, 64]),
                 thr[:].re("p (n o) -> p n o", o=1).bc([128, NB, 64]), ALU.is_le)
            b.reduce(bef[:], cmpB[:], ALU.add)
            if debug:
                b.load(dbg_be, bef[:])
            b.ts(bef[:], bef[:], 128.0, pidx[:, 0:1], op0=ALU.mult, op1=ALU.add)
            b.copy(idxw_i[:], bef[:])
            destf = b.sbuf("destf", [128, NT, 2], F32)
            tqa = b.sbuf("tqa", [128, NT, 64], F32); tqb = b.sbuf("tqb", [128, NT, 64], F32)
            b.tt(tqa[:], mpa[:, :, 128:192], pstart[:].re("p (o e) -> p o e", o=1).bc([128, NT, 64]), ALU.add)
            for k2 in range(2):
                b.tt(tqb[:], tqa[:], mpa[:, :, k2 * 64:(k2 + 1) * 64], ALU.mult)
                b.reduce(destf[:, :, k2:k2 + 1].re("p n o -> p (n o)"), tqb[:], ALU.add)
            b.copy(dest_i[:], destf[:])
            if debug:
                dr = b.sbuf("dr", [128, NT, 4], F32)
                b.copy(dr[:, :, 0:2], destf[:]); b.copy(dr[:, :, 2:4], gates[:])
                b.load(dbg_route, dr[:])
            b.mark('layout')
            xr = b.sbuf("xr", [128, 2, D], BF16, nslots=2)
            b.load(xr.s(0), xn2_d[0:128, :])
            for i in range(NT):
                if i + 1 < NT:
                    b.load(xr.s((i + 1) % 2), xn2_d[(i + 1) * 128:(i + 2) * 128, :])
                for k2 in range(2):
                    b.scatter(xs_d, xr.s(i % 2), dest_i[:, i, k2:k2 + 1])
        b.stack = st0

        b.mark('scatter')
        with ExitStack() as stm:
            b.stack = stm
            stm.callback(b.barrier)
            wstg = b.sbuf("wstg", [128, 4, 4096], F32, nslots=4)
            wgb = b.sbuf("wgb", [128, 2, NK, 512], BF16, nslots=2)
            wub = b.sbuf("wub", [128, 2, NK, 512], BF16, nslots=2)
            wdb = b.sbuf("wdb", [128, 2, 4, D], BF16, nslots=2)
            xsb = b.sbuf("xsb", [128, 2, D], BF16, nslots=2)
            xsT = b.sbuf("xsT", [128, 2, NK, 128], BF16, nslots=2)
            hh = b.sbuf("hh", [128, 2, 512], BF16, nslots=2)
            hT = b.sbuf("hT", [128, 4, 128], BF16)
            et = b.sbuf("et", [128, 2, 3, 512], F32, nslots=2)
            ysb = b.sbuf("ysb", [128, 2, D], F32, nslots=2)
            ptx = b.psum("ptx", [128, NK, 128], BF16)
            pth = b.psum("pth", [128, NK, 128], BF16)
            pg2 = b.psum("pg_m", [128, 2, 512], F32, nslots=2)
            pu2 = b.psum("pu_m", [128, 2, 512], F32, nslots=2)
            pyy = b.psum("pyy", [128, 2, 512], F32, nslots=2)
            wg_v = w_gate.re("e (p c) f -> (e p) (c f)", c=NK)
            wu_v = w_up.re("e (p c) f -> (e p) (c f)", c=NK)
            wd_v = w_down.re("e (p c) d -> (e p) (c d)", c=4)
            sc = [0]

            order = []
            lo, hi = 0, NB - 1
            while lo <= hi:
                order.append(lo); lo += 1
                if lo <= hi:
                    order.append(hi); hi -= 1
            seq = [(slot, sub) for slot in order for sub in range(SUB)]
            NSEQ = len(seq)

            def fetch_w(pos):
                slot = order[pos]
                ws = pos % 2
                for (wv, dstb, eng) in ((wg_v, wgb, "act"), (wu_v, wub, "dve"), (wd_v, wdb, "mix")):
                    sl = wstg.s(sc[0] % 4); sc[0] += 1
                    b.gather(sl, wv, idxw_i[:, slot:slot + 1], bounds=64 * 128 - 1)
                    dv = dstb.s(ws).re("p a f -> p (a f)")
                    if eng == "mix":
                        b.copy(dv[:, 0:2048], sl[:, 0:2048], eng="dve")
                        b.copy(dv[:, 2048:4096], sl[:, 2048:4096], eng="dve")
                    elif eng == "act":
                        b.copy(dv[:, 0:2048], sl[:, 0:2048], eng="act")
                        b.copy(dv[:, 2048:4096], sl[:, 2048:4096], eng="dve")
                    else:
                        b.copy(dv, sl, eng=eng)

            def blk_of(q):
                slot, sub = seq[q]
                return slot * SUB + sub

            def fetch_x(q):
                blk = blk_of(q)
                b.load(xsb.s(q % 2), xs_d[blk * 128:(blk + 1) * 128, :])

            def stage1(q):
                ws = (q // SUB) % 2
                p2 = q % 2
                if q + 1 < NSEQ:
                    fetch_x(q + 1)
                pg = pg2.s(p2); pu = pu2.s(p2); et_ = et.s(p2); xT = xsT.s(p2)
                for c in range(NK):
                    b.transpose(ptx[:, c, :], xsb.s(p2)[:, c * 128:(c + 1) * 128], ident_b[:])
                b.copy(xT, ptx[:], eng="act")
                for c in range(NK):
                    b.matmul(pg, xT[:, c, :], wgb.s(ws)[:, c, :], start=(c == 0), stop=(c == NK - 1))
                for c in range(NK):
                    b.matmul(pu, xT[:, c, :], wub.s(ws)[:, c, :], start=(c == 0), stop=(c == NK - 1))
                b.act(et_[:, 2, :], pg, AF.Silu)
                b.tt(hh.s(p2).re("s (c p) -> s c p", c=4), et_[:, 2, :].re("s (p c) -> s c p", c=4), pu.re("s (p c) -> s c p", c=4), ALU.mult)

            def stage2(q):
                blk = blk_of(q)
                ws = (q // SUB) % 2
                p2 = q % 2
                for c in range(4):
                    b.transpose(pth[:, c, :], hh.s(p2)[:, c * 128:(c + 1) * 128], ident_b[:])
                b.copy(hT[:], pth[:, 0:4, :], eng="act")
                for hf in range(2):
                    for c in range(4):
                        b.matmul(pyy.s(hf), hT[:, c, :], wdb.s(ws)[:, c, hf * 512:(hf + 1) * 512], start=(c == 0), stop=(c == 3))
                b.copy(ysb.s(p2)[:, 0:512], pyy.s(0), eng="act")
                b.copy(ysb.s(p2)[:, 512:1024], pyy.s(1))
                b.load(ys_d[blk * 128:(blk + 1) * 128, :], ysb.s(p2))

            fetch_w(0)
            fetch_x(0)
            stage1(0)
            for q in range(NSEQ):
                if q % SUB == 0 and q // SUB + 1 < NB:
                    fetch_w(q // SUB + 1)
                if q + 1 < NSEQ:
                    stage1(q + 1)
                stage2(q)
        b.stack = st0

        b.mark('moe')
        with ExitStack() as stf:
            b.stack = stf
            fgr = b.sbuf("fgr", [128, D], F32)
            b.bcast_load(fgr[:], fgain)
            y1 = b.sbuf("y1", [128, 2, D], F32, nslots=2); y2 = b.sbuf("y2", [128, 2, D], F32, nslots=2)
            hh1 = b.sbuf("hh1", [128, 2, D], F32, nslots=2)
            acc = b.sbuf("acc", [128, D], F32); acc2 = b.sbuf("acc2", [128, 2, D], F32, nslots=2)
            ot = b.sbuf("ot", [128, 2, D], F32, nslots=2)
            jk = b.sbuf("jk_f", [128, D], BF16)
            sf = b.sbuf("sf", [128, 2, 4], F32, nslots=2)
            outs = []

            def fetch_f(i):
                s = i % 2
                b.gather(y1.s(s), ys_d, dest_i[:, i, 0:1])
                b.gather(y2.s(s), ys_d, dest_i[:, i, 1:2])
                b.load(hh1.s(s), h1_d[i * 128:(i + 1) * 128, :])

            def combine(i):
                s = i % 2
                if i + 1 < NT:
                    fetch_f(i + 1)
                b.stt(acc[:], y1.s(s), gates[:, i, 0:1], hh1.s(s), ALU.mult, ALU.add)
                b.stt(acc2.s(s), y2.s(s), gates[:, i, 1:2], acc[:], ALU.mult, ALU.add)
                sfs = sf.s(s)
                b.act(jk[:], acc2.s(s), AF.Square, accum_out=sfs[:, 0:1])
                b.act(sfs[:, 1:2], sfs[:, 0:1], AF.Ln, bias=EPS, scale=1.0 / D)
                b.act(sfs[:, 2:3], sfs[:, 1:2], AF.Exp, scale=-0.5)

            def finish(i):
                s = i % 2
                b.stt(ot.s(s), acc2.s(s), sf.s(s)[:, 2:3], fgr[:], ALU.mult, ALU.mult)
                outs.append(b.load(out[i * 128:(i + 1) * 128, :], ot.s(s)))

            fetch_f(0)
            combine(0)
            for i in range(NT):
                if i + 1 < NT:
                    combine(i + 1)
                finish(i)
            b.wait_all("sp", outs)

        def tail(bb):
            bb.waited = {e: {} for e in bb.prog}
            bb.latest = {}
            bb.lastw = {}
            bb.readers = {}
            for e, lst in bb.prog.items():
                for idx, it in enumerate(lst):
                    if it[0] == "dma":
                        bb.latest[it[3]] = max(bb.latest.get(it[3], 0), 0)
            toks = []
            cnt = {}
            for e, lst in bb.prog.items():
                for idx, it in enumerate(lst):
                    if it[0] == "dma":
                        cnt[it[3]] = cnt.get(it[3], 0) + 16
                        bb.latest[it[3]] = cnt[it[3]]
                    elif it[2] is not None and e in COMPUTE:
                        bb.latest[("e", e)] = idx
            for q in bb.dma_cnt:
                for i in range(bb.n_dma):
                    bb.dma_cnt[q][i] = cnt.get(("d", q, i), 0) // 16
            bb.barrier()
            for i in range(NT):
                toks.append(bb.load(out[i * 128:(i + 1) * 128, :], zeros_f[:]))
            bb.wait_all("sp", toks)
        b.emit(trunc=trunc, tail=tail)
    return nc


GRID_W = 64
ROPE_THETA = 10000.0
_NC_CACHE = {}


def _consts(T):
    NT = T // 128
    RB = 256
    NB = 2 * T // RB + 63
    s = np.arange(128)[:, None]
    c = np.arange(128)[None, :]
    maskU = (s <= c).astype(np.float32)
    maskL = (s >= c).astype(np.float32)
    t = np.arange(T)
    row = (t // GRID_W).astype(np.float32)
    col = (t % GRID_W).astype(np.float32)
    axis_dim = 32
    inv_freq = (ROPE_THETA ** (-np.arange(0, axis_dim, 2, dtype=np.float32) / axis_dim)).astype(np.float32)
    ang = np.concatenate([row[:, None] * inv_freq, col[:, None] * inv_freq], axis=-1).astype(np.float32)
    cos = np.cos(ang).astype(np.float32).reshape(NT, 128, 32).transpose(1, 0, 2)
    sin = np.sin(ang).astype(np.float32).reshape(NT, 128, 32).transpose(1, 0, 2)
    return dict(
        c_ident=np.eye(128, dtype=np.float32),
        c_mask4=np.ascontiguousarray(np.concatenate([maskU, maskU, maskL, maskL], axis=1)),
        c_lstrict=(s < c).astype(np.float32),
        c_cos=np.ascontiguousarray(cos), c_sin=np.ascontiguousarray(sin),
        c_pidx=np.arange(128, dtype=np.float32).reshape(128, 1),
        c_thr=np.ascontiguousarray(np.broadcast_to((float(RB) * np.arange(NB, dtype=np.float32))[None, :], (128, NB))),
        c_bd=np.ascontiguousarray(((np.arange(128)[:, None] // 64) == (np.arange(256)[None, :] // 128)).astype(np.float32)),
    )


def _shared_inputs(T, norm1_gain, w_in, gla_up_fwd, gla_up_fwd_bias, gla_up_bwd, gla_up_bwd_bias, gla_out_gain,
                   q_norm_gain, k_norm_gain, att_out_gain, w_out, norm2_gain, w_group, b_group, w_expert, b_expert,
                   w_gate, w_up, w_down, final_gain):
    f = lambda a: np.ascontiguousarray(np.asarray(a, dtype=np.float32))
    d = dict(
        w_in=f(w_in[0]), g1pk=f(np.asarray(norm1_gain[0]).reshape(8, 128).T),
        up_f=f(gla_up_fwd[0]), up_b=f(gla_up_bwd[0]),
        bias_f=f(np.asarray(gla_up_fwd_bias[0]).reshape(2, 128).T), bias_b=f(np.asarray(gla_up_bwd_bias[0]).reshape(2, 128).T),
        gla_gain=f(gla_out_gain[0]), q_gain=f(q_norm_gain[0]), k_gain=f(k_norm_gain[0]), att_gain=f(att_out_gain[0]),
        w_out=f(w_out[0]), g2=f(norm2_gain[0]), w_group=f(w_group[0]), b_group=f(b_group[0]),
        w_expert=f(w_expert[0]), b_expert=f(b_expert[0]),
        w_gate=f(w_gate[0]), w_up=f(w_up[0]), w_down=f(w_down[0]), fgain=f(final_gain),
    )
    d.update(_consts(T))
    return d


def kernel(x, **params):
    x = np.asarray(x, dtype=np.float32)
    B, T, _ = x.shape
    if T not in _NC_CACHE:
        _NC_CACHE[T] = build(T)
    nc = _NC_CACHE[T]
    shared = _shared_inputs(T, **params)
    in_maps = []
    for bi in range(B):
        m = dict(shared)
        m["x"] = np.ascontiguousarray(x[bi])
        in_maps.append(m)
    res = run_bass_kernel_spmd(nc, in_maps, core_ids=list(range(B)))
    return np.stack([np.asarray(r["out"], dtype=np.float32) for r in res.results], axis=0)
```
